# Optimizing a Trainium2 kernel written in Bass

```python
import math
import jax
import jax.numpy as jnp
from jax import lax
import numpy as np

D_MODEL = 1024
BATCH = 16
SEQ = 4096
DEPTH = 4

GRID_W = 64
CTX_LEN = 256
N_MIXERS = 2
N_ADA = 6
NORM_EPS = 1e-6
GDN_HEADS = 8
GDN_DK = 128
GDN_DV = 128
GDN_CONV = 5
GDN_CHUNK = 64
GDN_QK_W = GDN_HEADS * GDN_DK
GDN_V_W = GDN_HEADS * GDN_DV
GDN_IN_W = 2 * GDN_QK_W + 2 * GDN_V_W + 4 * GDN_HEADS
GMLP_CHUNK = 128
GMLP_GROUPS = 8
GMLP_WIDTH = 2 * D_MODEL
ROWS_PER_CHUNK = GMLP_CHUNK // GRID_W
N_EXPERTS = 32
TOP_K = 4
D_EXPERT = D_MODEL
SWIGLU_ALPHA = 1.702
SWIGLU_LIMIT = 7.0

kernel_name = 'hybrid_gdn_chunkgmlp_moe_prefix_dit'


def _n_layers_of(m):
    return (DEPTH - m + N_MIXERS - 1) // N_MIXERS


def rmsnorm(x, g):
    xf = x.astype(jnp.float32)
    y = xf * lax.rsqrt(jnp.mean(xf * xf, axis=-1, keepdims=True) + NORM_EPS)
    return y.astype(x.dtype) * g


def layernorm(x, g, bias):
    xf = x.astype(jnp.float32)
    mu = jnp.mean(xf, axis=-1, keepdims=True)
    xc = xf - mu
    y = xc * lax.rsqrt(jnp.mean(xc * xc, axis=-1, keepdims=True) + NORM_EPS)
    return y.astype(x.dtype) * g + bias


def l2norm(x):
    return x * lax.rsqrt(jnp.sum(x * x, axis=-1, keepdims=True) + NORM_EPS)


def modulate(h, shift, scale):
    return h * (1.0 + scale) + shift


def centred_dwconv(x, w):
    k, ch = w.shape
    return lax.conv_general_dilated(x, w[:, None, :].astype(x.dtype), window_strides=(1,),
                                    padding=[(k // 2, k // 2)], dimension_numbers=('NWC', 'WIO', 'NWC'),
                                    feature_group_count=ch)


def chunk_gated_delta(q, k, v, g, beta, s0):
    b, h, t, dk = k.shape
    cs = GDN_CHUNK
    n = t // cs
    q = q * (dk ** -0.5)
    k_beta = k * beta[..., None]
    v_beta = v * beta[..., None]
    chunk = lambda a: a.reshape(b, h, n, cs, a.shape[-1])
    q, k, k_beta, v_beta = chunk(q), chunk(k), chunk(k_beta), chunk(v_beta)
    g = jnp.cumsum(g.reshape(b, h, n, cs), axis=-1)
    idx = jnp.arange(cs)
    incl = idx[:, None] >= idx[None, :]
    strict = idx[:, None] > idx[None, :]
    decay = jnp.exp(jnp.where(incl, g[..., :, None] - g[..., None, :], -jnp.inf))
    lmat = jnp.where(strict, jnp.einsum('bhnid,bhnjd->bhnij', k_beta, k) * decay, 0.0)
    eye = jnp.eye(cs, dtype=lmat.dtype)
    t_inv = lax.linalg.triangular_solve(eye + lmat, jnp.broadcast_to(eye, lmat.shape),
                                        left_side=True, lower=True)
    u = t_inv @ v_beta
    w = t_inv @ (k_beta * jnp.exp(g)[..., None])
    a_intra = jnp.einsum('bhnid,bhnjd->bhnij', q, k) * decay
    q_dec = q * jnp.exp(g)[..., None]
    g_last = g[..., -1]
    k_tail = k * jnp.exp(g_last[..., None] - g)[..., None]

    def step(s, xs):
        q_i, k_i, u_i, w_i, a_i, d_i = xs
        v_new = u_i - w_i @ s
        o_i = q_i @ s + a_i @ v_new
        s = s * d_i[..., None, None] + jnp.swapaxes(k_i, -1, -2) @ v_new
        return s, o_i

    xs = tuple(jnp.moveaxis(a, 2, 0) for a in (q_dec, k_tail, u, w, a_intra, jnp.exp(g_last)))
    s_final, o = lax.scan(step, s0, xs)
    o = jnp.moveaxis(o, 0, 2).reshape(b, h, t, -1)
    return o, s_final


def bidir_gated_delta(q, k, v, g, beta, s0_fwd, s0_bwd):
    o_f, s_f = chunk_gated_delta(q, k, v, g[0], beta[0], s0_fwd)
    rev = lambda a: jnp.flip(a, axis=2)
    o_b, s_b = chunk_gated_delta(rev(q), rev(k), rev(v), rev(g[1]), rev(beta[1]), s0_bwd)
    return o_f + rev(o_b), s_f, s_b


def gdn_features(h, w_in, conv_w, a_log, dt_bias):
    b, t, _ = h.shape
    proj = h @ w_in
    o1 = 2 * GDN_QK_W + GDN_V_W
    qkv = jax.nn.silu(centred_dwconv(proj[..., :o1], conv_w))

    def heads(a, dh):
        return a.reshape(b, t, GDN_HEADS, dh).transpose(0, 2, 1, 3).astype(jnp.float32)

    q = l2norm(heads(qkv[..., :GDN_QK_W], GDN_DK))
    k = l2norm(heads(qkv[..., GDN_QK_W:2 * GDN_QK_W], GDN_DK))
    v = heads(qkv[..., 2 * GDN_QK_W:], GDN_DV)
    z = proj[..., o1:o1 + GDN_V_W]
    o2 = o1 + GDN_V_W
    a = proj[..., o2:o2 + 2 * GDN_HEADS].astype(jnp.float32).reshape(b, t, 2, GDN_HEADS)
    bb = proj[..., o2 + 2 * GDN_HEADS:].astype(jnp.float32).reshape(b, t, 2, GDN_HEADS)
    g = -jnp.exp(a_log.astype(jnp.float32)) * jax.nn.softplus(a + dt_bias.astype(jnp.float32))
    beta = jax.nn.sigmoid(bb)
    to_dir = lambda x: x.transpose(2, 0, 3, 1)
    return q, k, v, z, to_dir(g), to_dir(beta)


def gdn_output(o, z, norm_g, w_out):
    b, hh, t, dv = o.shape
    o = jnp.swapaxes(o, 1, 2)
    o = o * lax.rsqrt(jnp.mean(o * o, axis=-1, keepdims=True) + NORM_EPS)
    o = o * norm_g.astype(jnp.float32) * jax.nn.silu(z.reshape(b, t, hh, dv).astype(jnp.float32))
    return o.reshape(b, t, hh * dv).astype(w_out.dtype) @ w_out


def chunk_gmlp(h, n_chunks, w_in, b_in, ln_g, ln_b, w_s, b_s, w_out):
    b, t, _ = h.shape
    zz = jax.nn.gelu(h @ w_in + b_in, approximate=False)
    u, v = zz[..., :GMLP_WIDTH], zz[..., GMLP_WIDTH:]
    v = layernorm(v, ln_g, ln_b)
    v = v.reshape(b, n_chunks, GMLP_CHUNK, GMLP_GROUPS, GMLP_WIDTH // GMLP_GROUPS)
    s = jnp.einsum('gij,bnjgc->bnigc', w_s, v) + b_s.T[:, :, None]
    return (u * s.reshape(b, t, GMLP_WIDTH)) @ w_out


def clamped_swiglu_expert(h, w_gu, b_gu, w_dn, b_dn):
    gu = h @ w_gu + b_gu
    gate = jnp.minimum(gu[..., :D_EXPERT], SWIGLU_LIMIT)
    up = jnp.clip(gu[..., D_EXPERT:], -SWIGLU_LIMIT, SWIGLU_LIMIT)
    glu = gate * jax.nn.sigmoid(gate * SWIGLU_ALPHA)
    return ((up + 1.0) * glu) @ w_dn + b_dn


def moe(h, w_r, b_r, w_gu, b_gu, w_dn, b_dn):
    logits = (h @ w_r + b_r).astype(jnp.float32)
    top_v, top_i = lax.top_k(logits, TOP_K)
    probs = jax.nn.softmax(top_v, axis=-1)
    comb = jnp.einsum('nke,nk->ne', jax.nn.one_hot(top_i, N_EXPERTS, dtype=jnp.float32), probs).astype(h.dtype)
    y = jnp.zeros_like(h)
    for e in range(N_EXPERTS):
        y = y + comb[:, e:e + 1] * clamped_swiglu_expert(h, w_gu[e], b_gu[e], w_dn[e], b_dn[e])
    return y


def setup_inputs(seed: int = 0) -> dict:
    key = jax.random.key(seed)
    ks = iter(jax.random.split(key, 40))
    f32 = jnp.float32
    D = D_MODEL
    na, nb = _n_layers_of(0), _n_layers_of(1)

    def nrm(shape, scale):
        return jax.random.normal(next(ks), shape, f32) * scale

    def gain(shape):
        return 1.0 + nrm(shape, 0.05)

    x = nrm((BATCH, SEQ, D), 1.0)
    c = nrm((BATCH, D), 1.0)
    ctx = nrm((BATCH, CTX_LEN, D), 1.0)
    c_ctx = nrm((D,), 1.0)
    ada_w = nrm((DEPTH, D, N_ADA * D), 0.5 * D ** -0.5)
    ada_b = nrm((DEPTH, N_ADA * D), 0.02)
    norm1_g = gain((DEPTH, D))
    norm2_g = gain((DEPTH, D))
    gdn_w_in = nrm((na, D, GDN_IN_W), D ** -0.5)
    gdn_conv_w = nrm((na, GDN_CONV, 2 * GDN_QK_W + GDN_V_W), GDN_CONV ** -0.5)
    gdn_a_log = jnp.log(jax.random.uniform(next(ks), (na, 2, GDN_HEADS), f32, 1.0, 16.0))
    dt = jnp.exp(jax.random.uniform(next(ks), (na, 2, GDN_HEADS), f32, math.log(1e-3), math.log(1e-1)))
    gdn_dt_bias = dt + jnp.log(-jnp.expm1(-dt))
    gdn_norm_g = gain((na, GDN_DV))
    gdn_w_out = nrm((na, GDN_V_W, D), GDN_V_W ** -0.5)
    gmlp_w_in = nrm((nb, D, 2 * GMLP_WIDTH), D ** -0.5)
    gmlp_b_in = nrm((nb, 2 * GMLP_WIDTH), 0.02)
    gmlp_ln_g = gain((nb, GMLP_WIDTH))
    gmlp_ln_b = nrm((nb, GMLP_WIDTH), 0.02)
    gmlp_w_s = nrm((nb, GMLP_GROUPS, GMLP_CHUNK, GMLP_CHUNK), 0.5 * GMLP_CHUNK ** -0.5)
    gmlp_b_s = gain((nb, GMLP_GROUPS, GMLP_CHUNK))
    gmlp_w_out = nrm((nb, GMLP_WIDTH, D), GMLP_WIDTH ** -0.5)
    router_w = nrm((DEPTH, D, N_EXPERTS), D ** -0.5)
    router_b = nrm((DEPTH, N_EXPERTS), 0.01)
    moe_w_gu = nrm((DEPTH, N_EXPERTS, D, 2 * D_EXPERT), D ** -0.5)
    moe_b_gu = nrm((DEPTH, N_EXPERTS, 2 * D_EXPERT), 0.01)
    moe_w_dn = nrm((DEPTH, N_EXPERTS, D_EXPERT, D), D_EXPERT ** -0.5)
    moe_b_dn = nrm((DEPTH, N_EXPERTS, D), 0.01)
    final_g = gain((D,))
    return {'x': x, 'c': c, 'ctx': ctx, 'c_ctx': c_ctx, 'ada_w': ada_w, 'ada_b': ada_b,
            'norm1_g': norm1_g, 'norm2_g': norm2_g, 'gdn_w_in': gdn_w_in, 'gdn_conv_w': gdn_conv_w,
            'gdn_a_log': gdn_a_log, 'gdn_dt_bias': gdn_dt_bias, 'gdn_norm_g': gdn_norm_g, 'gdn_w_out': gdn_w_out,
            'gmlp_w_in': gmlp_w_in, 'gmlp_b_in': gmlp_b_in, 'gmlp_ln_g': gmlp_ln_g, 'gmlp_ln_b': gmlp_ln_b,
            'gmlp_w_s': gmlp_w_s, 'gmlp_b_s': gmlp_b_s, 'gmlp_w_out': gmlp_w_out,
            'router_w': router_w, 'router_b': router_b, 'moe_w_gu': moe_w_gu, 'moe_b_gu': moe_b_gu,
            'moe_w_dn': moe_w_dn, 'moe_b_dn': moe_b_dn, 'final_g': final_g}


def reference(x, c, ctx, c_ctx, ada_w, ada_b, norm1_g, norm2_g, gdn_w_in, gdn_conv_w, gdn_a_log, gdn_dt_bias,
              gdn_norm_g, gdn_w_out, gmlp_w_in, gmlp_b_in, gmlp_ln_g, gmlp_ln_b, gmlp_w_s, gmlp_b_s, gmlp_w_out,
              router_w, router_b, moe_w_gu, moe_b_gu, moe_w_dn, moe_b_dn, final_g):
    b, t, d = x.shape
    rows = t // GRID_W
    n_lat_chunks = rows // ROWS_PER_CHUNK
    n_ctx_chunks = ctx.shape[1] // GMLP_CHUNK
    last_ctx_reader = max([i for i in range(DEPTH) if i % N_MIXERS == 0])
    silu_c = jax.nn.silu(c)
    silu_cc = jax.nn.silu(c_ctx)
    zero_state = jnp.zeros((b, GDN_HEADS, GDN_DK, GDN_DV), jnp.float32)
    for i in range(DEPTH):
        j = i // N_MIXERS
        ctx_in = i <= last_ctx_reader
        ctx_out = i < last_ctx_reader
        mx = [m[:, None, :] for m in jnp.split(silu_c @ ada_w[i] + ada_b[i], N_ADA, axis=-1)]
        hx = modulate(rmsnorm(x, norm1_g[i]), mx[0], mx[1])
        if ctx_in:
            mc = jnp.split(silu_cc @ ada_w[i] + ada_b[i], N_ADA, axis=-1)
            hc = modulate(rmsnorm(ctx, norm1_g[i]), mc[0], mc[1])
        if i % N_MIXERS == 0:
            p = (gdn_w_in[j], gdn_conv_w[j], gdn_a_log[j], gdn_dt_bias[j])
            qc, kc, vc, zc, gc, bc = gdn_features(hc, *p)
            o_c, s_f, s_b = bidir_gated_delta(qc, kc, vc, gc, bc, zero_state, zero_state)
            qx, kx, vx, zx, gx, bx = gdn_features(hx, *p)
            o_x, _, _ = bidir_gated_delta(qx, kx, vx, gx, bx, s_f, s_b)
            yx = gdn_output(o_x, zx, gdn_norm_g[j], gdn_w_out[j])
            if ctx_out:
                yc = gdn_output(o_c, zc, gdn_norm_g[j], gdn_w_out[j])
        else:
            gp = (gmlp_w_in[j], gmlp_b_in[j], gmlp_ln_g[j], gmlp_ln_b[j], gmlp_w_s[j], gmlp_b_s[j], gmlp_w_out[j])
            yx = chunk_gmlp(hx, n_lat_chunks, *gp)
            if ctx_out:
                yc = chunk_gmlp(hc, n_ctx_chunks, *gp)
        x = x + mx[2] * yx
        mp = (router_w[i], router_b[i], moe_w_gu[i], moe_b_gu[i], moe_w_dn[i], moe_b_dn[i])
        hx2 = modulate(rmsnorm(x, norm2_g[i]), mx[3], mx[4]).reshape(-1, d)
        if ctx_out:
            ctx = ctx + mc[2] * yc
            hc2 = modulate(rmsnorm(ctx, norm2_g[i]), mc[3], mc[4]).reshape(-1, d)
            y2 = moe(jnp.concatenate([hx2, hc2], axis=0), *mp)
            x = x + mx[5] * y2[:b * t].reshape(x.shape)
            ctx = ctx + mc[5] * y2[b * t:].reshape(ctx.shape)
        else:
            x = x + mx[5] * moe(hx2, *mp).reshape(x.shape)
    return rmsnorm(x, final_g)
```

```python
import numpy as np
from contextlib import ExitStack
import concourse.bass as bass
import concourse.mybir as mybir
from concourse.bass_utils import run_bass_kernel_spmd

F32 = mybir.dt.float32
BF16 = mybir.dt.bfloat16
AF = mybir.ActivationFunctionType
ALU = mybir.AluOpType
AX = mybir.AxisListType

D = 1024
EPS = 1e-6
H = 8
CONVK = 5
GW = 2048
LIMIT = 7.0
ALPHA = 1.702
TOPK = 4


class Cfg:
    def __init__(self, NB=2, T=4096, TC=256, DEPTH=4, NE=32, mixers=(True, True), moe=True):
        self.NB, self.T, self.TC, self.DEPTH, self.NE = NB, T, TC, DEPTH, NE
        self.mixers = mixers
        self.moe = moe
        self.NX = NB * T
        self.NTOK = NB * T + NB * TC
        self.last_ctx_reader = max(i for i in range(DEPTH) if i % 2 == 0)
        self.o_adab = 0
        self.o_n1 = 48
        self.o_n2 = 56
        self.o_bgu = 64
        self.o_mix = 64 + NE * 16
        self.NCOL = self.o_mix + 128


class Prog:
    ENG = ("pe", "act", "dve", "pool", "sp")

    def __init__(self, nc, es):
        self.nc, self.es = nc, es
        self.q = {e: [] for e in self.ENG}
        self.cnt, self.sems = {}, {}
        self.lastw, self.readers = {}, {}
        self.seen = {e: {} for e in self.ENG}
        for e in self.ENG:
            self._sem("E_" + e)

    def _sem(self, name):
        if name not in self.sems:
            self.sems[name] = self.es.enter_context(self.nc.semaphore(name))
            self.cnt[name] = 0
        return name

    def _deps(self, eng, r, w):
        waits = {}

        def add(s, v):
            if eng == "pe" and s == "E_pe":
                return
            if waits.get(s, 0) < v:
                waits[s] = v

        for k in r:
            t = self.lastw.get(k)
            if t:
                add(*t)
        for k in w:
            t = self.lastw.get(k)
            if t:
                add(*t)
            for s, v in self.readers.get(k, {}).items():
                add(s, v)
        out = []
        for s, v in waits.items():
            if self.seen[eng].get(s, 0) < v:
                self.seen[eng][s] = v
                out.append((s, v))
        return out

    def _commit(self, tok, r, w):
        for k in r:
            d = self.readers.setdefault(k, {})
            if d.get(tok[0], 0) < tok[1]:
                d[tok[0]] = tok[1]
        for k in w:
            self.lastw[k] = tok
            self.readers[k] = {}

    def op(self, eng, fn, r=(), w=()):
        waits = self._deps(eng, r, w)
        s = "E_" + eng
        self.cnt[s] += 1
        self.q[eng].append((fn, waits, s, 1))
        self._commit((s, self.cnt[s]), r, w)

    def dma(self, eng, sem, out, in_, r=(), w=()):
        self._sem(sem)
        waits = self._deps(eng, r, w)
        self.cnt[sem] += 16
        self.q[eng].append((lambda e, o=out, i=in_: e.dma_start(out=o, in_=i), waits, sem, 16))
        self._commit((sem, self.cnt[sem]), r, w)

    def barrier(self):
        for e in self.ENG:
            waits = []
            for s, v in self.cnt.items():
                if v > 0 and self.seen[e].get(s, 0) < v and not (s == "E_" + e):
                    self.seen[e][s] = v
                    waits.append((s, v))
            if waits:
                self.q[e].append((None, waits, None, 0))
        self.lastw, self.readers = {}, {}

    def emit(self):
        nc = self.nc
        names = {"pe": "tensor", "act": "scalar", "dve": "vector", "pool": "gpsimd", "sp": "sync"}
        self.barrier()
        with nc.Block() as block:
            for e in self.ENG:
                def body(eng, e=e):
                    for fn, waits, s, inc in self.q[e]:
                        for ws, wv in waits:
                            eng.wait_ge(self.sems[ws], wv)
                        if fn is not None:
                            ins = fn(eng)
                            ins.then_inc(self.sems[s], inc)
                getattr(block, names[e])(body)


def build(cfg):
    nc = bass.Bass("TRN2", target_bir_lowering=False)
    NB, T, TC, DEPTH, NE = cfg.NB, cfg.T, cfg.TC, cfg.DEPTH, cfg.NE
    NX, NTOK = cfg.NX, cfg.NTOK
    na = (DEPTH + 1) // 2
    nb_ = DEPTH // 2

    def din(name, shape, dt=F32):
        return nc.dram_tensor(name, list(shape), dt, kind="ExternalInput").ap()

    xT_in = din("xT", [D, NX])
    cxT_in = din("cxT", [D, NB * TC])
    cT_in = din("cT", [128, 8 * (NB + 1)])
    cols_in = din("cols", [DEPTH, 128, cfg.NCOL])
    consts_in = din("consts", [128, 6 * 128])
    ada_w = din("ada_w", [DEPTH, D, 6 * D])
    router_w = din("router_w", [DEPTH, D, NE])
    router_bb = din("router_bb", [DEPTH, 128, NE])
    sel_in = din("sel", [NE, NE * 128])
    moe_w_gu = din("moe_w_gu", [DEPTH, NE, D, 2 * D])
    moe_w_dn = din("moe_w_dn", [DEPTH, NE, D, D])
    moe_b_dn = din("moe_b_dn", [DEPTH, NE, D])
    final_g = din("final_g", [128, 8])
    gm_w_in = gm_w_out = gm_w_sT = gm_rows = gd_w_in = gd_w_out = None
    if nb_ > 0 and cfg.mixers[1]:
        gm_w_in = din("gm_w_in", [nb_, D, 2 * GW])
        gm_w_out = din("gm_w_out", [nb_, GW, D])
        gm_w_sT = din("gm_w_sT", [nb_, 8, 128, 128])
        gm_rows = din("gm_rows", [nb_, 128, 3 * GW + 8 * 128])
    if na > 0 and cfg.mixers[0]:
        gd_w_in = din("gd_w_in", [na, D, 4128])
        gd_w_out = din("gd_w_out", [na, D, D])
    y_out = nc.dram_tensor("yT", [D, NX], F32, kind="ExternalOutput").ap()
    cfg.dbg = None
    if getattr(cfg, "debug", False):
        cfg.dbg = (nc.dram_tensor("dbg_acc", [D, 512], F32, kind="ExternalOutput").ap(),
                   nc.dram_tensor("dbg_comb", [NE, 512], F32, kind="ExternalOutput").ap(),
                   nc.dram_tensor("dbg_h", [D, 512], BF16, kind="ExternalOutput").ap())
    xs = nc.dram_tensor("xs", [D, NTOK], F32).ap()
    gscr = None
    if na > 0 and cfg.mixers[0]:
        gscr = (nc.dram_tensor("g_pj", [4096, NTOK], F32).ap(), nc.dram_tensor("g_gb", [32, NTOK], F32).ap(),
                nc.dram_tensor("g_qk", [3072, NTOK], F32).ap(), nc.dram_tensor("g_ot", [2, NTOK, 1024], F32).ap())

    es = ExitStack()
    P = Prog(nc, es)

    uid = [0]

    def sb(name, shape, dt=F32, st=es):
        uid[0] += 1
        return st.enter_context(nc.sbuf_tensor("s%d_%s" % (uid[0], name), list(shape), dt))

    PS = [es.enter_context(nc.psum_tensor("ps%d" % i, [128, 512], F32)) for i in range(8)]
    psn = [0]

    def nextps():
        i = psn[0] % 8
        psn[0] += 1
        return i

    consts = sb("consts", [128, 6 * 128])
    ident = consts[:, 0:128]
    onesm = consts[:, 128:256]
    ones1 = consts[:, 256:384]
    epsc = consts[:, 384:385]
    P.dma("sp", "d_const", consts[:], consts_in[:, :], w=["consts"])
    cT = sb("cT", [128, 8 * (NB + 1)])
    sT = sb("sT", [128, 8 * (NB + 1)], BF16)
    P.dma("sp", "d_const", cT[:], cT_in[:, :], w=["cT"])
    P.op("act", lambda e: e.activation(out=sT[:], in_=cT[:], func=AF.Silu), r=["cT"], w=["sT"])
    cols = sb("cols", [128, cfg.NCOL])
    modT = sb("modT", [128, 48 * (NB + 1)])
    modA = sb("modA", [128, 2 * 8 * (NB + 1)])
    fing = sb("fing", [128, 8])
    P.dma("sp", "d_const", fing[:], final_g[:, :], w=["fing"])
    selt = sb("selt", [NE, NE * 128])
    P.dma("sp", "d_const", selt[:], sel_in[:, :], w=["selt"])

    def mcol(j, b):
        return modT[:, j * (NB + 1) + b: j * (NB + 1) + b + 1]

    def acol(which, c, b):
        i = (which * 8 + c) * (NB + 1) + b
        return modA[:, i:i + 1]

    def tiles(seq_len, base, nseq, tile, midx_fn):
        out = []
        for s in range(nseq):
            for t0 in range(0, seq_len, tile):
                n = min(tile, seq_len - t0)
                out.append((base + s * seq_len + t0, n, midx_fn(s)))
        return out

    x_tiles = tiles(T, 0, NB, 512, lambda s: s)
    c_tiles = tiles(TC, NX, NB, 512, lambda s: NB)

    for (src, base, n) in ((xT_in, 0, NX), (cxT_in, NX, NB * TC)):
        for t0 in range(0, n, 2048):
            nn = min(2048, n - t0)
            P.dma("sp", "d_cp", xs[:, base + t0: base + t0 + nn], src[:, t0:t0 + nn], w=[("xs", base + t0 + i) for i in range(0, nn, 128)])

    def xs_keys(c0, n):
        return [("xs", c0 + i) for i in range(0, n, 128)]

    xs_v = xs.rearrange("(c p) t -> p c t", p=128)

    def norm_mod(st, xt, n, which, mi, h16, h32=None, tag="nm"):
        sq = st["sq"]
        xk = [("xt", c) for c in range(8)]
        P.op("act", lambda e: e.activation(out=sq[:, :, :n], in_=xt[:, :, :n], func=AF.Square), r=xk, w=["sq"] + [("tmp", c) for c in range(8)])
        pi = nextps()
        ps = PS[pi]

        def mm(e):
            for c in range(8):
                ins = e.matmul(ps[:, :n], onesm, sq[:, c, :n], start=(c == 0), stop=(c == 7))
            return ins
        P.op("pe", mm, r=["sq", "consts"], w=[("ps", pi)])
        rstd = st["rstd"]
        P.op("act", lambda e: e.activation(out=rstd[:, :n], in_=ps[:, :n], func=AF.Sqrt, bias=epsc, scale=1.0), r=[("ps", pi), "consts"], w=["rstd"])
        P.op("dve", lambda e: e.reciprocal(out=rstd[:, :n], in_=rstd[:, :n]), r=["rstd"], w=["rstd"])
        tmp = st["tmp"]
        for c in range(8):
            P.op("dve", lambda e, c=c: e.tensor_tensor(out=tmp[:, c, :n], in0=xt[:, c, :n], in1=rstd[:, :n], op=ALU.mult),
                 r=[("xt", c), "rstd", "sq"], w=[("tmp", c)])
            dst = h32 if h32 is not None else h16
            P.op("act", lambda e, c=c, dst=dst: e.activation(out=dst[:, c, :n], in_=tmp[:, c, :n], func=AF.Identity,
                                                              scale=acol(which, c, mi), bias=mcol((0 if which == 0 else 3) * 8 + c, mi)),
                 r=[("tmp", c), "modA", "modT"], w=[("xt", c)] if h32 is not None else [("h16", c)])
            if h32 is not None:
                P.op("pool", lambda e, c=c: e.tensor_copy(out=h16[:, c, :n], in_=h32[:, c, :n]), r=[("xt", c)], w=[("h16", c)])

    for l in range(DEPTH):
        j = l // 2
        ctx_in = l <= cfg.last_ctx_reader
        ctx_out = l < cfg.last_ctx_reader
        P.barrier()
        P.dma("sp", "d_cols", cols[:], cols_in[l, :, :], w=["cols"])
        with ExitStack() as st_:
            aw = [sb("aw%d" % i, [128, 8, 1024], BF16, st_) for i in range(2)]
            awv = ada_w[l].rearrange("(k p) n -> p k n", p=128)
            for g6 in range(6):
                a = aw[g6 % 2]
                ak = "aw%d" % (g6 % 2)
                P.dma("pool", "d_" + ak, a[:], awv[:, :, g6 * 1024:(g6 + 1) * 1024], w=[ak])
                for jj in range(8):
                    jg = g6 * 8 + jj
                    pi = nextps()
                    ps = PS[pi]

                    def mm(e, a=a, jj=jj, ps=ps):
                        for k in range(8):
                            ins = e.matmul(ps[:, :NB + 1], a[:, k, jj * 128:(jj + 1) * 128], sT[:, k * (NB + 1):(k + 1) * (NB + 1)],
                                           start=(k == 0), stop=(k == 7))
                        return ins
                    P.op("pe", mm, r=[ak, "sT"], w=[("ps", pi)])
                    P.op("dve", lambda e, jg=jg, ps=ps: e.tensor_scalar(out=modT[:, jg * (NB + 1):(jg + 1) * (NB + 1)], in0=ps[:, :NB + 1],
                                                                         scalar1=cols[:, cfg.o_adab + jg: cfg.o_adab + jg + 1], scalar2=None, op0=ALU.add),
                         r=[("ps", pi), "cols"], w=["modT"])
            for which, grp, og in ((0, 1, cfg.o_n1), (1, 4, cfg.o_n2)):
                for c in range(8):
                    jg = grp * 8 + c
                    i0 = (which * 8 + c) * (NB + 1)
                    P.op("dve", lambda e, jg=jg, i0=i0, og=og, c=c: e.tensor_scalar(
                        out=modA[:, i0:i0 + NB + 1], in0=modT[:, jg * (NB + 1):(jg + 1) * (NB + 1)],
                        scalar1=1.0, scalar2=cols[:, og + c: og + c + 1], op0=ALU.add, op1=ALU.mult),
                        r=["modT", "cols"], w=["modA"])
        P.barrier()

        if l % 2 == 1 and cfg.mixers[1]:
            gmlp_layer(nc, cfg, P, sb, PS, nextps, l, j, xs_v, xs_keys, norm_mod, mcol, cols,
                       gm_w_in, gm_w_out, gm_w_sT, gm_rows, x_tiles, c_tiles, ctx_out, consts)
            P.barrier()
        if l % 2 == 0 and cfg.mixers[0]:
            gdn_layer(nc, cfg, P, sb, PS, nextps, l, j, xs_v, xs_keys, norm_mod, mcol, cols, consts,
                      gd_w_in, gd_w_out, None, x_tiles, c_tiles, ctx_out, gscr)
            P.barrier()

        if cfg.moe:
            toks = list(x_tiles) + (list(c_tiles) if ctx_out else [])
            moe_layer(nc, cfg, P, sb, PS, nextps, l, xs_v, xs_keys, norm_mod, mcol, cols, consts, selt,
                      router_w, router_bb, moe_w_gu, moe_w_dn, moe_b_dn, toks)
            P.barrier()

    with ExitStack() as st_:
        xt2 = [sb("fx%d" % i, [128, 8, 512], F32, st_) for i in range(2)]
        sq = sb("fsq", [128, 8, 512], F32, st_)
        rstd = sb("frstd", [128, 512], F32, st_)
        yo = [sb("fy%d" % i, [128, 8, 512], F32, st_) for i in range(2)]
        yv = y_out.rearrange("(c p) t -> p c t", p=128)
        for ti, (c0, n, mi) in enumerate(x_tiles):
            xt = xt2[ti % 2]
            xk = "fx%d" % (ti % 2)
            yk = "fy%d" % (ti % 2)
            yt = yo[ti % 2]
            P.dma("sp", "d_" + xk, xt[:, :, :n], xs_v[:, :, c0:c0 + n], r=xs_keys(c0, n), w=[xk])
            P.op("act", lambda e, xt=xt, n=n: e.activation(out=sq[:, :, :n], in_=xt[:, :, :n], func=AF.Square), r=[xk], w=["fsq"])
            pi = nextps()
            ps = PS[pi]

            def mm(e, ps=ps, n=n):
                for c in range(8):
                    ins = e.matmul(ps[:, :n], onesm, sq[:, c, :n], start=(c == 0), stop=(c == 7))
                return ins
            P.op("pe", mm, r=["fsq", "consts"], w=[("ps", pi)])
            P.op("act", lambda e, ps=ps, n=n: e.activation(out=rstd[:, :n], in_=ps[:, :n], func=AF.Sqrt, bias=epsc, scale=1.0), r=[("ps", pi), "consts"], w=["frstd"])
            P.op("dve", lambda e, n=n: e.reciprocal(out=rstd[:, :n], in_=rstd[:, :n]), r=["frstd"], w=["frstd"])
            for c in range(8):
                P.op("dve", lambda e, c=c, xt=xt, yt=yt, n=n: e.scalar_tensor_tensor(
                    out=yt[:, c, :n], in0=xt[:, c, :n], scalar=fing[:, c:c + 1], in1=rstd[:, :n], op0=ALU.mult, op1=ALU.mult),
                    r=[xk, "frstd", "fing"], w=[yk])
            P.dma("sp", "d_" + yk, yv[:, :, c0:c0 + n], yt[:, :, :n], r=[yk], w=[("y", c0)])
    P.emit()
    es.close()
    return nc


def moe_layer(nc, cfg, P, sb, PS, nextps, l, xs_v, xs_keys, norm_mod, mcol, cols, consts, selt,
              router_w, router_bb, moe_w_gu, moe_w_dn, moe_b_dn, toks):
    NE, NB = cfg.NE, cfg.NB
    ident = consts[:, 0:128]
    GT = 1024
    groups, cur, curn = [], [], 0
    for t in toks:
        if curn + t[1] > GT:
            groups.append(cur)
            cur, curn = [], 0
        cur.append(t)
        curn += t[1]
    if cur:
        groups.append(cur)
    with ExitStack() as st_:
        h16 = sb("m_h16", [128, 8, GT], BF16, st_)
        acc = sb("m_acc", [128, 8, GT], F32, st_)
        combT = sb("m_combT", [NE, GT], F32, st_)
        wgu = [sb("m_wgu%d" % i, [128, 8, 2048], BF16, st_) for i in range(2)]
        wdn = [sb("m_wdn%d" % i, [128, 8, 1024], BF16, st_) for i in range(1)]
        NWDN = 1
        with ExitStack() as tmp_:
            big = sb("m_big", [128, 8, 512], F32, tmp_)
            sq = sb("m_sq", [128, 8, 512], F32, tmp_)
        hmid = [sb("m_hmid%d" % i, [128, 8, 512], BF16, st_) for i in range(2)]
        gg = [sb("m_g%d" % i, [128, 512], F32, st_) for i in range(2)]
        sg = [sb("m_s%d" % i, [128, 512], F32, st_) for i in range(2)]
        uu = [sb("m_u%d" % i, [128, 512], F32, st_) for i in range(2)]
        cbc = sb("m_cbc", [128, 512], F32, st_)
        stn = {"sq": sq, "rstd": sb("m_rstd", [128, 512], F32, st_), "tmp": sq}
        h32 = big
        rw = sb("m_rw", [128, 8, NE], F32, st_)
        rbb = sb("m_rbb", [128, NE], F32, st_)
        bdn = sb("m_bdn", [NE, 1024], F32, st_)
        lg = sb("m_lg", [128, 4, NE], F32, st_)
        top8 = sb("m_top8", [128, 4, 8], F32, st_)
        ex = sb("m_ex", [128, 4, NE], F32, st_)
        ssum = sb("m_ssum", [128, 4], F32, st_)
        bu1 = sb("m_bu1", [128, NE * 8], F32, st_)
        P.dma("sp", "d_mrw", rw[:], router_w[l].rearrange("(k p) n -> p k n", p=128), w=["rw"])
        P.dma("sp", "d_mrbb", rbb[:], router_bb[l, :, :], w=["rbb"])
        P.dma("sp", "d_mbdn", bdn[:], moe_b_dn[l, :, :], w=["bdn"])
        for e_ in range(NE):
            P.op("dve", lambda e, e_=e_: e.tensor_scalar(out=bu1[:, e_ * 8:(e_ + 1) * 8],
                                                         in0=cols[:, cfg.o_bgu + e_ * 16 + 8: cfg.o_bgu + e_ * 16 + 16],
                                                         scalar1=1.0, scalar2=None, op0=ALU.add), r=["cols"], w=["bu1"])
        wload = [0]

        def load_gu(e_):
            s = wload[0] % 2
            wload[0] += 1
            for half in range(2):
                P.dma("pool", "d_wgu%d" % s, wgu[s][:, half * 4:(half + 1) * 4, :],
                      moe_w_gu[l, e_].rearrange("(k p) n -> p k n", p=128)[:, half * 4:(half + 1) * 4, :], w=[("wgu", s, half)])
            return s

        def load_dn(e_):
            P.dma("pool", "d_wdn0", wdn[0][:], moe_w_dn[l, e_].rearrange("(k p) n -> p k n", p=128), w=[("wdn", 0)])

        for grp in groups:
            P.barrier()
            off = 0
            offs = []
            for (c0, n, mi) in grp:
                offs.append(off)
                nblk = n // 128
                P.dma("sp", "d_mbig", big[:, :, :n], xs_v[:, :, c0:c0 + n], r=xs_keys(c0, n), w=[("xt", c) for c in range(8)])
                norm_mod(stn, big, n, 1, mi, h16[:, :, off:off + n], h32=h32)
                pi = nextps()
                ps = PS[pi]

                def mm(e, ps=ps, nblk=nblk):
                    for b in range(nblk):
                        for k in range(8):
                            ins = e.matmul(ps[:, b * NE:(b + 1) * NE], h32[:, k, b * 128:(b + 1) * 128], rw[:, k, :], start=(k == 0), stop=(k == 7))
                    return ins
                P.op("pe", mm, r=[("xt", c) for c in range(8)] + ["rw"], w=[("ps", pi)])
                for b in range(nblk):
                    P.op("dve", lambda e, b=b, ps=ps: e.tensor_tensor(out=lg[:, b, :], in0=ps[:, b * NE:(b + 1) * NE], in1=rbb[:], op=ALU.add),
                         r=[("ps", pi), "rbb"], w=[("lg", b)])
                    P.op("dve", lambda e, b=b: e.max(out=top8[:, b, :], in_=lg[:, b, :]), r=[("lg", b)], w=[("top8", b)])
                    P.op("dve", lambda e, b=b: e.tensor_scalar(out=ex[:, b, :], in0=lg[:, b, :], scalar1=top8[:, b, 0:1], scalar2=None, op0=ALU.subtract),
                         r=[("lg", b), ("top8", b)], w=[("ex", b)])
                    P.op("act", lambda e, b=b: e.activation(out=ex[:, b, :], in_=ex[:, b, :], func=AF.Exp), r=[("ex", b)], w=[("ex", b)])
                    P.op("dve", lambda e, b=b: e.scalar_tensor_tensor(out=ex[:, b, :], in0=lg[:, b, :], scalar=top8[:, b, TOPK - 1:TOPK], in1=ex[:, b, :],
                                                                      op0=ALU.is_ge, op1=ALU.mult), r=[("lg", b), ("top8", b), ("ex", b)], w=[("ex", b)])
                    P.op("dve", lambda e, b=b: e.tensor_reduce(out=ssum[:, b:b + 1], in_=ex[:, b, :], axis=AX.X, op=ALU.add), r=[("ex", b)], w=[("ssum", b)])
                    P.op("dve", lambda e, b=b: e.reciprocal(out=ssum[:, b:b + 1], in_=ssum[:, b:b + 1]), r=[("ssum", b)], w=[("ssum", b)])
                    P.op("dve", lambda e, b=b: e.tensor_scalar(out=ex[:, b, :], in0=ex[:, b, :], scalar1=ssum[:, b:b + 1], scalar2=None, op0=ALU.mult),
                         r=[("ex", b), ("ssum", b)], w=[("ex", b)])
                pi2 = nextps()
                ps2 = PS[pi2]

                def tr(e, ps2=ps2, nblk=nblk):
                    for b in range(nblk):
                        ins = e.transpose(ps2[:NE, b * 128:(b + 1) * 128], ex[:, b, :], ident)
                    return ins
                P.op("pe", tr, r=[("ex", b) for b in range(nblk)] + ["consts"], w=[("ps", pi2)])
                P.op("act", lambda e, ps2=ps2, off=off, n=n: e.copy(out=combT[:, off:off + n], in_=ps2[:NE, :n]), r=[("ps", pi2)], w=[("combT", off)])
                for m in range(8):
                    pi3 = nextps()
                    ps3 = PS[pi3]
                    P.op("pe", lambda e, ps3=ps3, m=m, off=off, n=n: e.matmul(ps3[:, :n], bdn[:, m * 128:(m + 1) * 128], combT[:, off:off + n], start=True, stop=True),
                         r=["bdn", ("combT", off)], w=[("ps", pi3)])
                    P.op("act", lambda e, ps3=ps3, m=m, off=off, n=n: e.copy(out=acc[:, m, off:off + n], in_=ps3[:, :n]), r=[("ps", pi3)], w=[("acc", m, off)])
                off += n
            P.barrier()
            s_next = load_gu(0)
            load_dn(0)
            for e_ in range(NE):
                s = s_next
                if e_ + 1 < NE:
                    s_next = load_gu(e_ + 1)
                for ti, (c0, n, mi) in enumerate(grp):
                    off = offs[ti]
                    hm = hmid[ti % 2]
                    hk = "hmid%d" % (ti % 2)
                    pi = nextps()
                    ps = PS[pi]
                    P.op("pe", lambda e, ps=ps, e_=e_, off=off, n=n: e.matmul(ps[:, :n], selt[:, e_ * 128:(e_ + 1) * 128], combT[:, off:off + n], start=True, stop=True),
                         r=["selt", ("combT", off)], w=[("ps", pi)])
                    P.op("act", lambda e, ps=ps, n=n: e.copy(out=cbc[:, :n], in_=ps[:, :n]), r=[("ps", pi)], w=["cbc"])
                    for jp in range(8):
                        pg, pu = nextps(), nextps()
                        psg, psu = PS[pg], PS[pu]

                        def mmgu(e, psg=psg, psu=psu, jp=jp, s=s, off=off, n=n):
                            for k in range(8):
                                e.matmul(psg[:, :n], wgu[s][:, k, jp * 128:(jp + 1) * 128], h16[:, k, off:off + n], start=(k == 0), stop=(k == 7))
                            for k in range(8):
                                ins = e.matmul(psu[:, :n], wgu[s][:, k, 1024 + jp * 128:1024 + (jp + 1) * 128], h16[:, k, off:off + n], start=(k == 0), stop=(k == 7))
                            return ins
                        P.op("pe", mmgu, r=[("wgu", s, 0), ("wgu", s, 1)] + [("h16", c) for c in range(8)], w=[("ps", pg), ("ps", pu)])
                        g_, s_, u_ = gg[jp % 2], sg[jp % 2], uu[jp % 2]
                        gk, sk, uk = "gg%d" % (jp % 2), "sg%d" % (jp % 2), "uu%d" % (jp % 2)
                        bcol = cols[:, cfg.o_bgu + e_ * 16 + jp: cfg.o_bgu + e_ * 16 + jp + 1]
                        P.op("dve", lambda e, g_=g_, psg=psg, bcol=bcol, n=n: e.tensor_scalar(out=g_[:, :n], in0=psg[:, :n], scalar1=bcol, scalar2=LIMIT, op0=ALU.add, op1=ALU.min),
                             r=[("ps", pg), "cols"], w=[gk])
                        P.op("act", lambda e, g_=g_, s_=s_, n=n: e.activation(out=s_[:, :n], in_=g_[:, :n], func=AF.Sigmoid, scale=ALPHA), r=[gk], w=[sk])
                        P.op("dve", lambda e, u_=u_, psu=psu, e_=e_, jp=jp, n=n: e.tensor_scalar(out=u_[:, :n], in0=psu[:, :n], scalar1=bu1[:, e_ * 8 + jp: e_ * 8 + jp + 1],
                                                                                         scalar2=LIMIT + 1.0, op0=ALU.add, op1=ALU.min), r=[("ps", pu), "bu1"], w=[uk])
                        P.op("pool", lambda e, g_=g_, s_=s_, n=n: e.tensor_tensor(out=s_[:, :n], in0=g_[:, :n], in1=s_[:, :n], op=ALU.mult), r=[gk, sk], w=[sk])
                        P.op("pool", lambda e, s_=s_, n=n: e.tensor_tensor(out=s_[:, :n], in0=s_[:, :n], in1=cbc[:, :n], op=ALU.mult), r=[sk, "cbc"], w=[sk])
                        P.op("dve", lambda e, u_=u_, s_=s_, hm=hm, jp=jp, n=n: e.scalar_tensor_tensor(out=hm[:, jp, :n], in0=u_[:, :n], scalar=1.0 - LIMIT, in1=s_[:, :n],
                                                                                               op0=ALU.max, op1=ALU.mult), r=[uk, sk], w=[(hk, jp)])
                    for m in range(8):
                        pi = nextps()
                        ps = PS[pi]

                        def mmdn(e, ps=ps, m=m, s=s, hm=hm, n=n):
                            for k in range(8):
                                ins = e.matmul(ps[:, :n], wdn[s % NWDN][:, k, m * 128:(m + 1) * 128], hm[:, k, :n], start=(k == 0), stop=(k == 7))
                            return ins
                        P.op("pe", mmdn, r=[("wdn", s % NWDN)] + [(hk, k) for k in range(8)], w=[("ps", pi)])
                        P.op("dve", lambda e, ps=ps, m=m, off=off, n=n: e.tensor_tensor(out=acc[:, m, off:off + n], in0=acc[:, m, off:off + n], in1=ps[:, :n], op=ALU.add),
                             r=[("ps", pi), ("acc", m, off)], w=[("acc", m, off)])
                if e_ + 1 < NE:
                    load_dn(e_ + 1)
            P.barrier()
            if cfg.dbg is not None and grp is groups[0]:
                P.dma("sp", "d_dbg", cfg.dbg[0].rearrange("(c p) t -> p c t", p=128), acc[:, :, 0:512], w=["dbg0"])
                P.dma("sp", "d_dbg", cfg.dbg[1][:, :], combT[:, 0:512], w=["dbg1"])
                P.dma("sp", "d_dbg", cfg.dbg[2].rearrange("(c p) t -> p c t", p=128), h16[:, :, 0:512], w=["dbg2"])
                P.barrier()
            for ti, (c0, n, mi) in enumerate(grp):
                off = offs[ti]
                P.dma("sp", "d_mbig", big[:, :, :n], xs_v[:, :, c0:c0 + n], r=xs_keys(c0, n), w=[("xt", c) for c in range(8)])
                for m in range(8):
                    P.op("dve", lambda e, m=m, off=off, n=n, mi=mi: e.scalar_tensor_tensor(out=big[:, m, :n], in0=acc[:, m, off:off + n], scalar=mcol(5 * 8 + m, mi),
                                                                                         in1=big[:, m, :n], op0=ALU.mult, op1=ALU.add),
                         r=[("acc", m, off), ("xt", m), "modT"], w=[("xt", m)])
                P.dma("sp", "d_mst", xs_v[:, :, c0:c0 + n], big[:, :, :n], r=[("xt", c) for c in range(8)], w=xs_keys(c0, n))


def gdn_layer(nc, cfg, P, sb, PS, nextps, l, j, xs_v, xs_keys, norm_mod, mcol, cols, consts,
              gd_w_in, gd_w_out, gd_rows, x_tiles, c_tiles, ctx_out, scr):
    NB, T, TC = cfg.NB, cfg.T, cfg.TC
    NX, NTOK = cfg.NX, cfg.NTOK
    ident = consts[:, 0:128]
    ones1 = consts[:, 256:384]
    epsc = consts[:, 384:385]
    om = cfg.o_mix
    pj, gb, qkvn, oTok = scr
    pj_v = pj.rearrange("(c p) t -> p c t", p=128)
    qk_v = qkvn.rearrange("(c p) t -> p c t", p=128)
    toks = list(x_tiles) + list(c_tiles)
    with ExitStack() as st_:
        w16 = sb("g_w", [128, 8, 4128], BF16, st_)
        wv = gd_w_in[j].rearrange("(k p) n -> p k n", p=128)
        for q4 in range(4):
            P.dma("pool", "d_gw%d" % q4, w16[:, :, q4 * 1032:(q4 + 1) * 1032], wv[:, :, q4 * 1032:(q4 + 1) * 1032], w=[("gw", q4)])
        gwk = [("gw", q4) for q4 in range(4)]
        big = sb("g_big", [128, 8, 512], F32, st_)
        sq = sb("g_sq", [128, 8, 512], F32, st_)
        stn = {"sq": sq, "rstd": sb("g_rstd", [128, 512], F32, st_), "tmp": sq}
        h16 = sb("g_h16", [128, 8, 512], BF16, st_)
        ob = [sb("g_ob%d" % i, [128, 512], F32, st_) for i in range(4)]
        gt = sb("g_gt", [32, 512], F32, st_)
        gt2 = sb("g_gt2", [32, 512], F32, st_)
        nega = sb("g_nega", [32, 1], F32, st_)
        P.op("act", lambda e: e.activation(out=nega[:], in_=cols[:32, om + 121:om + 122], func=AF.Exp), r=["cols"], w=["nega"])
        P.op("dve", lambda e: e.tensor_scalar(out=nega[:], in0=nega[:], scalar1=cols[:32, om + 124:om + 125], scalar2=-1.0, op0=ALU.mult, op1=ALU.mult), r=["nega", "cols"], w=["nega"])
        for (c0, n, mi) in toks:
            P.dma("sp", "d_gbig", big[:, :, :n], xs_v[:, :, c0:c0 + n], r=xs_keys(c0, n), w=[("xt", c) for c in range(8)])
            norm_mod(stn, big, n, 0, mi, h16)
            for jc in range(32):
                pi = nextps()
                ps = PS[pi]

                def mm(e, ps=ps, jc=jc, n=n):
                    for k in range(8):
                        ins = e.matmul(ps[:, :n], w16[:, k, jc * 128:(jc + 1) * 128], h16[:, k, :n], start=(k == 0), stop=(k == 7))
                    return ins
                P.op("pe", mm, r=gwk + [("h16", c) for c in range(8)], w=[("ps", pi)])
                o_ = ob[jc % 4]
                okk = "gob%d" % (jc % 4)
                if jc < 24:
                    P.op("act", lambda e, o_=o_, ps=ps, n=n: e.copy(out=o_[:, :n], in_=ps[:, :n]), r=[("ps", pi)], w=[okk])
                else:
                    P.op("act", lambda e, o_=o_, ps=ps, n=n: e.activation(out=o_[:, :n], in_=ps[:, :n], func=AF.Silu), r=[("ps", pi)], w=[okk])
                P.dma("sp", "d_" + okk, pj[jc * 128:(jc + 1) * 128, c0:c0 + n], o_[:, :n], r=[okk], w=[("pj", jc, c0)])
            pi = nextps()
            ps = PS[pi]

            def mm2(e, ps=ps, n=n):
                for k in range(8):
                    ins = e.matmul(ps[:32, :n], w16[:, k, 4096:4128], h16[:, k, :n], start=(k == 0), stop=(k == 7))
                return ins
            P.op("pe", mm2, r=gwk + [("h16", c) for c in range(8)], w=[("ps", pi)])
            P.op("act", lambda e, ps=ps, n=n: e.activation(out=gt[:, :n], in_=ps[:32, :n], func=AF.Exp, bias=cols[:32, om + 122:om + 123], scale=1.0), r=[("ps", pi), "cols"], w=["gt"])
            P.op("act", lambda e, n=n: e.activation(out=gt[:, :n], in_=gt[:, :n], func=AF.Ln, bias=ones1[:32, 0:1], scale=1.0), r=["gt", "consts"], w=["gt"])
            P.op("act", lambda e, ps=ps, n=n: e.activation(out=gt2[:, :n], in_=ps[:32, :n], func=AF.Sigmoid), r=[("ps", pi)], w=["gt2"])
            P.op("dve", lambda e, n=n: e.tensor_scalar(out=gt[:, :n], in0=gt[:, :n], scalar1=nega[:, 0:1], scalar2=None, op0=ALU.mult), r=["gt", "nega"], w=["gt"])
            P.op("dve", lambda e, n=n: e.scalar_tensor_tensor(out=gt2[:, :n], in0=gt2[:, :n], scalar=cols[:32, om + 123:om + 124], in1=gt[:, :n], op0=ALU.mult, op1=ALU.add),
                 r=["gt", "gt2", "cols"], w=["gt2"])
            P.dma("sp", "d_ggb", gb[:, c0:c0 + n], gt2[:, :n], r=["gt2"], w=[("gb", c0)])
    P.barrier()
    with ExitStack() as st_:
        buf = [sb("c_buf%d" % i, [128, 516], F32, st_) for i in range(2)]
        ac = [sb("c_ac%d" % i, [128, 512], F32, st_) for i in range(2)]
        sqq = sb("c_sq", [128, 512], F32, st_)
        rr = sb("c_rr", [128, 512], F32, st_)
        it = 0
        for (c0, n, mi) in toks:
            seq0 = (c0 // T) * T if c0 < NX else NX + ((c0 - NX) // TC) * TC
            seqn = T if c0 < NX else TC
            first = (c0 == seq0)
            last = (c0 + n == seq0 + seqn)
            for jc in range(24):
                b_ = buf[it % 2]
                bk = "cbuf%d" % (it % 2)
                a_ = ac[it % 2]
                ak = "cac%d" % (it % 2)
                it += 1
                lo = 0 if first else 2
                hi = 0 if last else 2
                if first:
                    P.op("pool", lambda e, b_=b_: e.memset(b_[:, 0:2], 0.0), w=[bk])
                if last:
                    P.op("pool", lambda e, b_=b_, n=n: e.memset(b_[:, n + 2:n + 4], 0.0), w=[bk])
                P.dma("sp", "d_" + bk, b_[:, 2 - lo:n + 2 + hi], pj[jc * 128:(jc + 1) * 128, c0 - lo:c0 + n + hi], r=[("pj", jc, c0)], w=[bk])
                for tp in range(5):
                    wc = cols[:, om + tp * 24 + jc: om + tp * 24 + jc + 1]
                    if tp == 0:
                        P.op("dve", lambda e, a_=a_, b_=b_, wc=wc, n=n: e.tensor_scalar(out=a_[:, :n], in0=b_[:, 0:n], scalar1=wc, scalar2=None, op0=ALU.mult), r=[bk, "cols"], w=[ak])
                    else:
                        P.op("dve", lambda e, a_=a_, b_=b_, wc=wc, n=n, tp=tp: e.scalar_tensor_tensor(out=a_[:, :n], in0=b_[:, tp:tp + n], scalar=wc, in1=a_[:, :n], op0=ALU.mult, op1=ALU.add),
                             r=[bk, ak, "cols"], w=[ak])
                P.op("act", lambda e, a_=a_, n=n: e.activation(out=a_[:, :n], in_=a_[:, :n], func=AF.Silu), r=[ak], w=[ak])
                if jc < 16:
                    P.op("act", lambda e, a_=a_, n=n: e.activation(out=sqq[:, :n], in_=a_[:, :n], func=AF.Square), r=[ak], w=["csq"])
                    pi = nextps()
                    ps = PS[pi]
                    P.op("pe", lambda e, ps=ps, n=n: e.matmul(ps[:, :n], ones1, sqq[:, :n], start=True, stop=True), r=["csq", "consts"], w=[("ps", pi)])
                    P.op("act", lambda e, ps=ps, n=n: e.activation(out=rr[:, :n], in_=ps[:, :n], func=AF.Sqrt, bias=epsc, scale=1.0), r=[("ps", pi), "consts"], w=["crr"])
                    P.op("dve", lambda e, n=n: e.reciprocal(out=rr[:, :n], in_=rr[:, :n]), r=["crr"], w=["crr"])
                    sc = (128.0 ** -0.5) if jc < 8 else 1.0
                    P.op("dve", lambda e, a_=a_, n=n, sc=sc: e.scalar_tensor_tensor(out=a_[:, :n], in0=a_[:, :n], scalar=sc, in1=rr[:, :n], op0=ALU.mult, op1=ALU.mult), r=[ak, "crr"], w=[ak])
                P.dma("sp", "d_" + ak, qkvn[jc * 128:(jc + 1) * 128, c0:c0 + n], a_[:, :n], r=[ak], w=[("qk", jc, c0)])
    P.barrier()
    with ExitStack() as st_:
        S = sb("s_S", [128, NB * 8 * 128], F32, st_)
        qkv = [sb("s_qkv%d" % i, [128, 24, 64], F32, st_) for i in range(2)]
        gbr = sb("s_gbr", [32, 64], F32, st_)
        gtok = sb("s_gtok", [64, 32], F32, st_)
        gc = sb("s_gc", [64, 8], F32, st_)
        egc = sb("s_egc", [64, 8], F32, st_)
        ekt = sb("s_ekt", [64, 8], F32, st_)
        egl = sb("s_egl", [128, 8], F32, st_)
        glb = sb("s_glb", [128, 8], F32, st_)
        negb = sb("s_negb", [64, 8], F32, st_)
        obuf = [sb("s_obuf%d" % i, [64, 1024], F32, st_) for i in range(2)]
        W = {}
        for nm, shp in (("kbg", [64, 128]), ("ktl", [64, 128]), ("vt", [64, 128]), ("dg", [64, 64]), ("E", [64, 64]), ("Ei", [64, 64]), ("Es", [64, 64]),
                        ("aT", [64, 64]), ("Pa", [64, 64]), ("PaT", [64, 64]), ("Pb", [64, 64]), ("PbT", [64, 64]), ("Y", [64, 64]),
                        ("ub", [64, 128]), ("wT", [128, 64]), ("vn", [64, 128]), ("o1", [64, 128])):
            W[nm] = sb("s_" + nm, shp, F32, st_)
        def maskI(d):
            return consts[:64, 512 + d * 128: 512 + d * 128 + 64]

        def maskS(d):
            return consts[:64, 512 + d * 128 + 64: 512 + d * 128 + 128]

        def chunk_list(s, d):
            cx = [(NX + s * TC + i * 64) for i in range(TC // 64)]
            xx = [(s * T + i * 64) for i in range(T // 64)]
            return (cx + xx) if d == 0 else (cx[::-1] + xx[::-1])

        def ev(eng, out, in_, r, w):
            if eng == "act":
                P.op("act", lambda e: e.copy(out=out, in_=in_), r=r, w=w)
            else:
                P.op("dve", lambda e: e.tensor_copy(out=out, in_=in_), r=r, w=w)

        ci = 0
        for d in range(2):
            P.op("pool", lambda e: e.memset(S[:], 0.0), w=[("S", s, h) for s in range(NB) for h in range(8)])
            for s in range(NB):
                for c0 in chunk_list(s, d):
                    qb = qkv[ci % 2]
                    qkk = "qkv%d" % (ci % 2)
                    ob_ = obuf[ci % 2]
                    obk = "obuf%d" % (ci % 2)
                    ci += 1
                    P.dma("sp", "d_" + qkk, qb[:], qk_v[:, :, c0:c0 + 64], r=[("qk", jc, (c0 // 512) * 512 if False else None) for jc in range(0)], w=[qkk])
                    P.dma("sp", "d_gbr", gbr[:], gb[:, c0:c0 + 64], w=["gbr"])
                    pi = nextps()
                    ps = PS[pi]
                    P.op("pe", lambda e, ps=ps: e.transpose(ps[:64, :32], gbr[:, :], ident[:32, :32]), r=["gbr", "consts"], w=[("ps", pi)])
                    ev("act", gtok[:], ps[:64, :32], [("ps", pi)], ["gtok"])
                    P.op("dve", lambda e, d=d: e.tensor_scalar(out=negb[:], in0=gtok[:, 16 + d * 8:24 + d * 8], scalar1=-1.0, scalar2=None, op0=ALU.mult), r=["gtok"], w=["negb"])
                    pi = nextps()
                    ps = PS[pi]
                    P.op("pe", lambda e, ps=ps, d=d: e.matmul(ps[:64, :8], maskI(d), gtok[:, d * 8:d * 8 + 8], start=True, stop=True), r=["gtok", "consts"], w=[("ps", pi)])
                    ev("dve", gc[:], ps[:64, :8], [("ps", pi)], ["gc"])
                    pi = nextps()
                    ps = PS[pi]
                    P.op("pe", lambda e, ps=ps, d=d: e.matmul(ps[:, :8], ones1[:64, :], gtok[:, d * 8:d * 8 + 8], start=True, stop=True), r=["gtok", "consts"], w=[("ps", pi)])
                    ev("dve", glb[:], ps[:, :8], [("ps", pi)], ["glb"])
                    P.op("act", lambda e: e.activation(out=egl[:], in_=glb[:], func=AF.Exp), r=["glb"], w=["egl"])
                    P.op("act", lambda e: e.activation(out=egc[:], in_=gc[:], func=AF.Exp), r=["gc"], w=["egc"])
                    P.op("dve", lambda e: e.tensor_tensor(out=ekt[:], in0=glb[:64, :], in1=gc[:], op=ALU.subtract), r=["glb", "gc"], w=["ekt"])
                    P.op("act", lambda e: e.activation(out=ekt[:], in_=ekt[:], func=AF.Exp), r=["ekt"], w=["ekt"])
                    for h in range(8):
                        qT, kT, vT = qb[:, h, :], qb[:, 8 + h, :], qb[:, 16 + h, :]
                        Sh = S[:, (s * 8 + h) * 128:(s * 8 + h + 1) * 128]
                        Sk = ("S", s, h)
                        pk, pv = nextps(), nextps()
                        P.op("pe", lambda e, kT=kT, pk=pk: e.transpose(PS[pk][:64, :128], kT, ident), r=[qkk, "consts"], w=[("ps", pk)])
                        P.op("pe", lambda e, vT=vT, pv=pv: e.transpose(PS[pv][:64, :128], vT, ident), r=[qkk, "consts"], w=[("ps", pv)])
                        P.op("dve", lambda e, pk=pk, h=h: e.tensor_scalar(out=W["kbg"][:], in0=PS[pk][:64, :128], scalar1=egc[:, h:h + 1], scalar2=None, op0=ALU.mult), r=[("ps", pk), "egc"], w=["kbg"])
                        P.op("dve", lambda e, pk=pk, h=h: e.tensor_scalar(out=W["ktl"][:], in0=PS[pk][:64, :128], scalar1=ekt[:, h:h + 1], scalar2=None, op0=ALU.mult), r=[("ps", pk), "ekt"], w=["ktl"])
                        ev("act", W["vt"][:], PS[pv][:64, :128], [("ps", pv)], ["vt"])
                        P.op("dve", lambda e, h=h: e.tensor_scalar(out=W["dg"][:], in0=ident[:64, :64], scalar1=gc[:, h:h + 1], scalar2=None, op0=ALU.mult), r=["gc", "consts"], w=["dg"])
                        pr = nextps()
                        P.op("pe", lambda e, pr=pr: e.matmul(PS[pr][:64, :64], ones1[:64, :64], W["dg"][:], start=True, stop=True), r=["dg", "consts"], w=[("ps", pr)])
                        P.op("dve", lambda e, pr=pr, h=h: e.tensor_scalar(out=W["E"][:], in0=PS[pr][:64, :64], scalar1=gc[:, h:h + 1], scalar2=0.0, op0=ALU.subtract, op1=ALU.min), r=[("ps", pr), "gc"], w=["E"])
                        P.op("act", lambda e: e.activation(out=W["E"][:], in_=W["E"][:], func=AF.Exp), r=["E"], w=["E"])
                        P.op("pool", lambda e, d=d: e.tensor_tensor(out=W["Ei"][:], in0=W["E"][:], in1=maskI(d), op=ALU.mult), r=["E", "consts"], w=["Ei"])
                        P.op("pool", lambda e, d=d: e.tensor_tensor(out=W["Es"][:], in0=W["E"][:], in1=maskS(d), op=ALU.mult), r=["E", "consts"], w=["Es"])
                        pkk, pqk = nextps(), nextps()
                        P.op("pe", lambda e, kT=kT, pkk=pkk: e.matmul(PS[pkk][:64, :64], kT, kT, start=True, stop=True), r=[qkk], w=[("ps", pkk)])
                        P.op("pe", lambda e, kT=kT, qT=qT, pqk=pqk: e.matmul(PS[pqk][:64, :64], kT, qT, start=True, stop=True), r=[qkk], w=[("ps", pqk)])
                        P.op("dve", lambda e, pkk=pkk, h=h: e.scalar_tensor_tensor(out=W["PaT"][:], in0=PS[pkk][:64, :64], scalar=negb[:, h:h + 1], in1=W["Es"][:], op0=ALU.mult, op1=ALU.mult),
                             r=[("ps", pkk), "negb", "Es"], w=["PaT"])
                        P.op("dve", lambda e, pqk=pqk: e.tensor_tensor(out=W["aT"][:], in0=PS[pqk][:64, :64], in1=W["Ei"][:], op=ALU.mult), r=[("ps", pqk), "Ei"], w=["aT"])
                        pt = nextps()
                        P.op("pe", lambda e, pt=pt: e.transpose(PS[pt][:64, :64], W["PaT"][:], ident[:64, :64]), r=["PaT", "consts"], w=[("ps", pt)])
                        ev("act", W["Pa"][:], PS[pt][:64, :64], [("ps", pt)], ["Pa"])
                        P.op("dve", lambda e: e.tensor_tensor(out=W["Y"][:], in0=W["PaT"][:], in1=ident[:64, :64], op=ALU.add), r=["PaT", "consts"], w=["Y"])
                        cur, nxt = ("Pa", "PaT"), ("Pb", "PbT")
                        for lev in range(1, 6):
                            p1 = nextps()
                            P.op("pe", lambda e, p1=p1, cur=cur: e.matmul(PS[p1][:64, :64], W[cur[1]][:], W[cur[0]][:], start=True, stop=True), r=[cur[0], cur[1]], w=[("ps", p1)])
                            ev("act", W[nxt[0]][:], PS[p1][:64, :64], [("ps", p1)], [nxt[0]])
                            if lev < 5:
                                p2 = nextps()
                                P.op("pe", lambda e, p2=p2, cur=cur: e.matmul(PS[p2][:64, :64], W[cur[0]][:], W[cur[1]][:], start=True, stop=True), r=[cur[0], cur[1]], w=[("ps", p2)])
                                ev("dve", W[nxt[1]][:], PS[p2][:64, :64], [("ps", p2)], [nxt[1]])
                            p3 = nextps()
                            P.op("pe", lambda e, p3=p3, nxt=nxt: e.matmul(PS[p3][:64, :64], W[nxt[0]][:], W["Y"][:], start=True, stop=True), r=[nxt[0], "Y"], w=[("ps", p3)])
                            P.op("dve", lambda e, p3=p3: e.tensor_tensor(out=W["Y"][:], in0=W["Y"][:], in1=PS[p3][:64, :64], op=ALU.add), r=[("ps", p3), "Y"], w=["Y"])
                            cur, nxt = nxt, cur
                        pu, pw = nextps(), nextps()
                        P.op("pe", lambda e, pu=pu: e.matmul(PS[pu][:64, :128], W["Y"][:], W["vt"][:], start=True, stop=True), r=["Y", "vt"], w=[("ps", pu)])
                        P.op("pe", lambda e, pw=pw: e.matmul(PS[pw][:, :64], W["kbg"][:], W["Y"][:], start=True, stop=True), r=["Y", "kbg"], w=[("ps", pw)])
                        P.op("dve", lambda e, pu=pu, h=h, d=d: e.tensor_scalar(out=W["ub"][:], in0=PS[pu][:64, :128], scalar1=gtok[:, 16 + d * 8 + h:17 + d * 8 + h], scalar2=None, op0=ALU.mult),
                             r=[("ps", pu), "gtok"], w=["ub"])
                        ev("act", W["wT"][:], PS[pw][:, :64], [("ps", pw)], ["wT"])
                        p1, p2 = nextps(), nextps()
                        P.op("pe", lambda e, p1=p1, Sh=Sh: e.matmul(PS[p1][:64, :128], W["wT"][:], Sh, start=True, stop=True), r=["wT", Sk], w=[("ps", p1)])
                        P.op("pe", lambda e, p2=p2, Sh=Sh, qT=qT: e.matmul(PS[p2][:64, :128], qT, Sh, start=True, stop=True), r=[qkk, Sk], w=[("ps", p2)])
                        P.op("dve", lambda e, p1=p1, h=h: e.scalar_tensor_tensor(out=W["vn"][:], in0=PS[p1][:64, :128], scalar=negb[:, h:h + 1], in1=W["ub"][:], op0=ALU.mult, op1=ALU.add),
                             r=[("ps", p1), "negb", "ub"], w=["vn"])
                        P.op("act", lambda e, p2=p2, h=h: e.activation(out=W["o1"][:], in_=PS[p2][:64, :128], func=AF.Copy, scale=egc[:, h:h + 1]), r=[("ps", p2), "egc"], w=["o1"])
                        p3, p4 = nextps(), nextps()
                        P.op("pe", lambda e, p3=p3: e.matmul(PS[p3][:64, :128], W["aT"][:], W["vn"][:], start=True, stop=True), r=["aT", "vn"], w=[("ps", p3)])
                        P.op("pe", lambda e, p4=p4: e.matmul(PS[p4][:, :128], W["ktl"][:], W["vn"][:], start=True, stop=True), r=["ktl", "vn"], w=[("ps", p4)])
                        P.op("dve", lambda e, p3=p3, h=h, ob_=ob_: e.tensor_tensor(out=ob_[:, h * 128:(h + 1) * 128], in0=W["o1"][:], in1=PS[p3][:64, :128], op=ALU.add),
                             r=[("ps", p3), "o1"], w=[obk])
                        P.op("dve", lambda e, p4=p4, h=h, Sh=Sh: e.scalar_tensor_tensor(out=Sh, in0=Sh, scalar=egl[:, h:h + 1], in1=PS[p4][:, :128], op0=ALU.mult, op1=ALU.add),
                             r=[("ps", p4), "egl", Sk], w=[Sk])
                    P.dma("sp", "d_" + obk, oTok[d, c0:c0 + 64, :], ob_[:], r=[obk], w=[("oT", d, c0)])
    P.barrier()
    with ExitStack() as st_:
        wo = sb("o_w", [128, 8, 1024], BF16, st_)
        P.dma("pool", "d_ow", wo[:], gd_w_out[j].rearrange("(k p) n -> p k n", p=128), w=["ow"])
        of_ = sb("o_f", [128, 1024], F32, st_)
        obb = sb("o_b", [128, 1024], F32, st_)
        sqj = sb("o_sq", [128, 128], F32, st_)
        ssq = sb("o_ss", [128, 8], F32, st_)
        szt = sb("o_sz", [128, 8, 128], F32, st_)
        og = sb("o_g", [128, 8, 128], BF16, st_)
        xb = sb("o_x", [128, 8, 128], F32, st_)
        outt = list(x_tiles) + (list(c_tiles) if ctx_out else [])
        for (t0, n, mi) in outt:
            for c0 in range(t0, t0 + n, 128):
                P.dma("sp", "d_of", of_[:], oTok[0, c0:c0 + 128, :], w=["of"])
                P.dma("sp", "d_ob", obb[:], oTok[1, c0:c0 + 128, :], w=["obb"])
                P.dma("sp", "d_osz", szt[:], pj_v[:, 24:32, c0:c0 + 128], w=["szt"])
                P.dma("sp", "d_ox", xb[:], xs_v[:, :, c0:c0 + 128], r=xs_keys(c0, 128), w=["oxb"])
                P.op("dve", lambda e: e.tensor_tensor(out=of_[:], in0=of_[:], in1=obb[:], op=ALU.add), r=["of", "obb"], w=["of"])
                for h in range(8):
                    P.op("act", lambda e, h=h: e.activation(out=sqj[:], in_=of_[:, h * 128:(h + 1) * 128], func=AF.Square, accum_out=ssq[:, h:h + 1]), r=["of"], w=["sqj", ("ssq", h)])
                P.op("act", lambda e: e.activation(out=ssq[:], in_=ssq[:], func=AF.Sqrt, bias=epsc, scale=1.0 / 128.0), r=[("ssq", h) for h in range(8)] + ["consts"], w=[("ssq", h) for h in range(8)])
                P.op("dve", lambda e: e.reciprocal(out=ssq[:], in_=ssq[:]), r=[("ssq", h) for h in range(8)], w=[("ssq", h) for h in range(8)])
                for h in range(8):
                    P.op("dve", lambda e, h=h: e.tensor_scalar(out=of_[:, h * 128:(h + 1) * 128], in0=of_[:, h * 128:(h + 1) * 128], scalar1=ssq[:, h:h + 1], scalar2=None, op0=ALU.mult),
                         r=["of", ("ssq", h)], w=["of"])
                    pi = nextps()
                    P.op("pe", lambda e, h=h, pi=pi: e.transpose(PS[pi][:, :128], of_[:, h * 128:(h + 1) * 128], ident), r=["of", "consts"], w=[("ps", pi)])
                    P.op("dve", lambda e, h=h, pi=pi: e.scalar_tensor_tensor(out=og[:, h, :], in0=PS[pi][:, :128], scalar=cols[:, om + 120:om + 121], in1=szt[:, h, :], op0=ALU.mult, op1=ALU.mult),
                         r=[("ps", pi), "cols", "szt"], w=[("og", h)])
                for m in range(8):
                    pi = nextps()

                    def mm(e, pi=pi, m=m):
                        for k in range(8):
                            ins = e.matmul(PS[pi][:, :128], wo[:, k, m * 128:(m + 1) * 128], og[:, k, :], start=(k == 0), stop=(k == 7))
                        return ins
                    P.op("pe", mm, r=["ow"] + [("og", h) for h in range(8)], w=[("ps", pi)])
                    P.op("dve", lambda e, pi=pi, m=m, mi=mi: e.scalar_tensor_tensor(out=xb[:, m, :], in0=PS[pi][:, :128], scalar=mcol(2 * 8 + m, mi), in1=xb[:, m, :], op0=ALU.mult, op1=ALU.add),
                         r=[("ps", pi), "oxb", "modT"], w=["oxb"])
                P.dma("sp", "d_oxs", xs_v[:, :, c0:c0 + 128], xb[:], r=["oxb"], w=xs_keys(c0, 128))


def gmlp_layer(nc, cfg, P, sb, PS, nextps, l, j, xs_v, xs_keys, norm_mod, mcol, cols,
               gm_w_in, gm_w_out, gm_w_sT, gm_rows, x_tiles, c_tiles, ctx_out, consts):
    om = cfg.o_mix
    epsc = consts[:, 384:385]
    with ExitStack() as st_:
        wi = sb("m_wi", [128, 8, 4096], BF16, st_)
        wiv = gm_w_in[j].rearrange("(k p) n -> p k n", p=128)
        for q4 in range(4):
            P.dma("pool", "d_mwi%d" % q4, wi[:, :, q4 * 1024:(q4 + 1) * 1024], wiv[:, :, q4 * 1024:(q4 + 1) * 1024], w=[("wi", q4)])
        wik = [("wi", q4) for q4 in range(4)]
        wo = sb("m_wo", [128, 16, 1024], BF16, st_)
        P.dma("pool", "d_mwo", wo[:], gm_w_out[j].rearrange("(k p) n -> p k n", p=128), w=["wo"])
        ws = sb("m_ws", [128, 8, 128], BF16, st_)
        P.dma("pool", "d_mws", ws[:], gm_w_sT[j].rearrange("g j i -> j g i"), w=["ws"])
        rows = sb("m_rows", [128, 3 * GW + 1024], F32, st_)
        P.dma("sp", "d_mrows", rows[:], gm_rows[j, :, :], w=["rows"])
        big = sb("m_big", [128, 8, 128], F32, st_)
        sq = sb("m_sq", [128, 8, 128], F32, st_)
        stn = {"sq": sq, "rstd": sb("m_rstd", [128, 128], F32, st_), "tmp": sq}
        h16 = sb("m_h16", [128, 8, 128], BF16, st_)
        v = sb("m_v", [128, GW], F32, st_)
        vn = sb("m_vn", [128, GW], BF16, st_)
        u = sb("m_u", [128, 16, 128], F32, st_)
        gt = sb("m_gt", [128, 16, 128], BF16, st_)
        tt = [sb("m_tt%d" % i, [128, 128], F32, st_) for i in range(2)]
        st1 = sb("m_st", [128, 4], F32, st_)
        xb = sb("m_xb", [128, 8, 128], F32, st_)
        outt = list(x_tiles) + (list(c_tiles) if ctx_out else [])
        for (t0, n, mi) in outt:
            for c0 in range(t0, t0 + n, 128):
                P.dma("sp", "d_mgbig", big[:], xs_v[:, :, c0:c0 + 128], r=xs_keys(c0, 128), w=[("xt", c) for c in range(8)])
                P.dma("sp", "d_mgx", xb[:], xs_v[:, :, c0:c0 + 128], r=xs_keys(c0, 128), w=["mxb"])
                norm_mod(stn, big, 128, 0, mi, h16)
                hk = [("h16", c) for c in range(8)]
                for jc in range(16):
                    pi = nextps()

                    def mm(e, pi=pi, jc=jc):
                        for k in range(8):
                            ins = e.matmul(PS[pi][:, :128], wi[:, k, jc * 128:(jc + 1) * 128], h16[:, k, :], start=(k == 0), stop=(k == 7))
                        return ins
                    P.op("pe", mm, r=wik + hk, w=[("ps", pi)])
                    P.op("act", lambda e, pi=pi, jc=jc: e.activation(out=u[:, jc, :], in_=PS[pi][:, :128], func=AF.Gelu, bias=cols[:, om + jc:om + jc + 1], scale=1.0),
                         r=[("ps", pi), "cols"], w=[("u", jc)])
                for q4 in range(4):
                    pi = nextps()

                    def mm(e, pi=pi, q4=q4):
                        for k in range(8):
                            ins = e.matmul(PS[pi][:, :512], h16[:, k, :], wi[:, k, GW + q4 * 512:GW + (q4 + 1) * 512], start=(k == 0), stop=(k == 7))
                        return ins
                    P.op("pe", mm, r=wik + hk, w=[("ps", pi)])
                    P.op("dve", lambda e, pi=pi, q4=q4: e.tensor_tensor(out=v[:, q4 * 512:(q4 + 1) * 512], in0=PS[pi][:, :512], in1=rows[:, q4 * 512:(q4 + 1) * 512], op=ALU.add),
                         r=[("ps", pi), "rows"], w=["v"])
                P.op("act", lambda e: e.activation(out=v[:], in_=v[:], func=AF.Gelu), r=["v"], w=["v"])
                P.op("dve", lambda e: e.tensor_reduce(out=st1[:, 0:1], in_=v[:], axis=AX.X, op=ALU.add), r=["v"], w=["st1"])
                P.op("dve", lambda e: e.tensor_scalar(out=st1[:, 0:1], in0=st1[:, 0:1], scalar1=-1.0 / GW, scalar2=None, op0=ALU.mult), r=["st1"], w=["st1"])
                P.op("dve", lambda e: e.tensor_scalar(out=v[:], in0=v[:], scalar1=st1[:, 0:1], scalar2=None, op0=ALU.add), r=["v", "st1"], w=["v"])
                P.op("act", lambda e: e.activation(out=vn[:], in_=v[:], func=AF.Square, accum_out=st1[:, 1:2]), r=["v"], w=["vn", "st2"])
                P.op("act", lambda e: e.activation(out=st1[:, 1:2], in_=st1[:, 1:2], func=AF.Sqrt, bias=epsc, scale=1.0 / GW), r=["st2", "consts"], w=["st2"])
                P.op("dve", lambda e: e.reciprocal(out=st1[:, 1:2], in_=st1[:, 1:2]), r=["st2"], w=["st2"])
                P.op("dve", lambda e: e.scalar_tensor_tensor(out=v[:], in0=v[:], scalar=st1[:, 1:2], in1=rows[:, GW:2 * GW], op0=ALU.mult, op1=ALU.mult), r=["v", "st2", "rows"], w=["v"])
                P.op("dve", lambda e: e.tensor_tensor(out=vn[:], in0=v[:], in1=rows[:, 2 * GW:3 * GW], op=ALU.add), r=["v", "rows", "vn"], w=["vn"])
                for fc in range(16):
                    g = fc // 2
                    pi = nextps()
                    P.op("pe", lambda e, pi=pi, fc=fc, g=g: e.matmul(PS[pi][:, :128], vn[:, fc * 128:(fc + 1) * 128], ws[:, g, :], start=True, stop=True), r=["vn", "ws"], w=[("ps", pi)])
                    t_ = tt[fc % 2]
                    tk = "mtt%d" % (fc % 2)
                    P.op("dve", lambda e, pi=pi, g=g, t_=t_: e.tensor_tensor(out=t_[:], in0=PS[pi][:, :128], in1=rows[:, 3 * GW + g * 128:3 * GW + (g + 1) * 128], op=ALU.add),
                         r=[("ps", pi), "rows"], w=[tk])
                    P.op("pool", lambda e, fc=fc, t_=t_: e.tensor_tensor(out=gt[:, fc, :], in0=t_[:], in1=u[:, fc, :], op=ALU.mult), r=[tk, ("u", fc)], w=[("gt", fc)])
                for m in range(8):
                    pi = nextps()

                    def mm(e, pi=pi, m=m):
                        for k in range(16):
                            ins = e.matmul(PS[pi][:, :128], wo[:, k, m * 128:(m + 1) * 128], gt[:, k, :], start=(k == 0), stop=(k == 15))
                        return ins
                    P.op("pe", mm, r=["wo"] + [("gt", k) for k in range(16)], w=[("ps", pi)])
                    P.op("dve", lambda e, pi=pi, m=m, mi=mi: e.scalar_tensor_tensor(out=xb[:, m, :], in0=PS[pi][:, :128], scalar=mcol(2 * 8 + m, mi), in1=xb[:, m, :], op0=ALU.mult, op1=ALU.add),
                         r=[("ps", pi), "mxb", "modT"], w=["mxb"])
                P.dma("sp", "d_mgxs", xs_v[:, :, c0:c0 + 128], xb[:], r=["mxb"], w=xs_keys(c0, 128))


def _prep_inputs(cfg, inp, core, ncores):
    NB, T, TC, DEPTH, NE = cfg.NB, cfg.T, cfg.TC, cfg.DEPTH, cfg.NE
    f = np.float32
    b0 = core * NB
    x = inp["x"][b0:b0 + NB].reshape(NB * T, D)
    cx = inp["ctx"][b0:b0 + NB].reshape(NB * TC, D)
    cc = np.concatenate([inp["c"][b0:b0 + NB], inp["c_ctx"][None, :]], 0)
    cT = np.ascontiguousarray(cc.reshape(NB + 1, 8, 128).transpose(2, 1, 0).reshape(128, 8 * (NB + 1)))
    m = {"xT": np.ascontiguousarray(x.T), "cxT": np.ascontiguousarray(cx.T), "cT": cT}
    return m


def _colmat(v):
    v = np.asarray(v, np.float32).reshape(-1, 128)
    return v.T


def _shared_inputs(cfg, inp):
    NB, T, TC, DEPTH, NE = cfg.NB, cfg.T, cfg.TC, cfg.DEPTH, cfg.NE
    f = np.float32
    cols = np.zeros((DEPTH, 128, cfg.NCOL), f)
    for l in range(DEPTH):
        cols[l, :, cfg.o_adab:cfg.o_adab + 48] = _colmat(inp["ada_b"][l])
        cols[l, :, cfg.o_n1:cfg.o_n1 + 8] = _colmat(inp["norm1_g"][l])
        cols[l, :, cfg.o_n2:cfg.o_n2 + 8] = _colmat(inp["norm2_g"][l])
        cols[l, :, cfg.o_bgu:cfg.o_bgu + NE * 16] = _colmat(inp["moe_b_gu"][l])
    for l in range(DEPTH):
        if l % 2 == 0 and cfg.mixers[0]:
            jj = l // 2
            o = cfg.o_mix
            cw = np.asarray(inp["gdn_conv_w"][jj], f)
            for tp in range(5):
                cols[l, :, o + tp * 24:o + tp * 24 + 24] = _colmat(cw[tp])
            cols[l, :, o + 120] = np.asarray(inp["gdn_norm_g"][jj], f)
            cols[l, :16, o + 121] = np.asarray(inp["gdn_a_log"][jj], f).reshape(16)
            cols[l, :16, o + 122] = np.asarray(inp["gdn_dt_bias"][jj], f).reshape(16)
            cols[l, 16:32, o + 123] = 1.0
            cols[l, :16, o + 124] = 1.0
    for l in range(DEPTH):
        if l % 2 == 1 and cfg.mixers[1]:
            jj = l // 2
            cols[l, :, cfg.o_mix:cfg.o_mix + 16] = _colmat(np.asarray(inp["gmlp_b_in"][jj], f)[:GW])
    consts = np.zeros((128, 6 * 128), f)
    consts[:, 0:128] = np.eye(128, dtype=f)
    consts[:, 128:256] = 1.0 / 1024.0
    consts[:, 256:384] = 1.0
    consts[:, 384:512] = EPS
    jj_, ii_ = np.meshgrid(np.arange(64), np.arange(64), indexing="ij")
    consts[:64, 512:576] = (jj_ <= ii_)
    consts[:64, 576:640] = (jj_ < ii_)
    consts[:64, 640:704] = (jj_ >= ii_)
    consts[:64, 704:768] = (jj_ > ii_)
    sel = np.zeros((NE, NE * 128), f)
    for e in range(NE):
        sel[e, e * 128:(e + 1) * 128] = 1.0
    m = {
        "cols": cols, "consts": consts, "sel": sel,
        "ada_w": np.asarray(inp["ada_w"], f), "router_w": np.asarray(inp["router_w"], f),
        "router_bb": np.ascontiguousarray(np.broadcast_to(np.asarray(inp["router_b"], f)[:, None, :], (DEPTH, 128, NE))),
        "moe_w_gu": np.asarray(inp["moe_w_gu"], f), "moe_w_dn": np.asarray(inp["moe_w_dn"], f), "moe_b_dn": np.asarray(inp["moe_b_dn"], f),
        "final_g": np.ascontiguousarray(_colmat(inp["final_g"])),
    }
    if cfg.mixers[1] and DEPTH >= 2:
        nb_ = DEPTH // 2
        m["gm_w_in"] = np.asarray(inp["gmlp_w_in"], f)
        m["gm_w_out"] = np.asarray(inp["gmlp_w_out"], f)
        m["gm_w_sT"] = np.ascontiguousarray(np.asarray(inp["gmlp_w_s"], f).transpose(0, 1, 3, 2))
        rows = np.concatenate([np.asarray(inp["gmlp_b_in"], f)[:, GW:], np.asarray(inp["gmlp_ln_g"], f), np.asarray(inp["gmlp_ln_b"], f),
                               np.asarray(inp["gmlp_b_s"], f).reshape(nb_, 1024)], axis=1)
        m["gm_rows"] = np.ascontiguousarray(np.broadcast_to(rows[:, None, :], (nb_, 128, rows.shape[1])))
    if cfg.mixers[0]:
        m["gd_w_in"] = np.asarray(inp["gdn_w_in"], f)
        m["gd_w_out"] = np.asarray(inp["gdn_w_out"], f)
    return m


def run(cfg, inp, ncores, trace=False):
    nc = build(cfg)
    shared = _shared_inputs(cfg, inp)
    in_maps = []
    for c in range(ncores):
        m = dict(shared)
        m.update(_prep_inputs(cfg, inp, c, ncores))
        in_maps.append(m)
    res = run_bass_kernel_spmd(nc, in_maps, core_ids=list(range(ncores)), trace=trace)
    outs = []
    run.last = res
    for c in range(ncores):
        yT = res.results[c]["yT"]
        outs.append(np.ascontiguousarray(yT.T).reshape(cfg.NB, cfg.T, D))
    return np.concatenate(outs, 0), res


def kernel(**inputs):
    cfg = Cfg()
    out, _ = run(cfg, inputs, 8)
    return out.astype(np.float32)
```

```python
import numpy as np
from contextlib import ExitStack
import concourse.bass as bass
import concourse.mybir as mybir
from concourse.bass_utils import run_bass_kernel_spmd

F32 = mybir.dt.float32
BF16 = mybir.dt.bfloat16
AF = mybir.ActivationFunctionType
ALU = mybir.AluOpType
AX = mybir.AxisListType

D = 1024
EPS = 1e-6
H = 8
CONVK = 5
GW = 2048
LIMIT = 7.0
ALPHA = 1.702
TOPK = 4


class Cfg:
    def __init__(self, NB=2, T=4096, TC=256, DEPTH=4, NE=32, mixers=(True, True), moe=True):
        self.NB, self.T, self.TC, self.DEPTH, self.NE = NB, T, TC, DEPTH, NE
        self.mixers = mixers
        self.moe = moe
        self.NX = NB * T
        self.NTOK = NB * T + NB * TC
        self.last_ctx_reader = max(i for i in range(DEPTH) if i % 2 == 0)
        self.o_adab = 0
        self.o_n1 = 48
        self.o_n2 = 56
        self.o_bgu = 64
        self.o_mix = 64 + NE * 16
        self.NCOL = self.o_mix + 128


class Prog:
    ENG = ("pe", "act", "dve", "pool", "sp")

    def __init__(self, nc, es):
        self.nc, self.es = nc, es
        self.q = {e: [] for e in self.ENG}
        self.cnt, self.sems = {}, {}
        self.lastw, self.readers = {}, {}
        self.seen = {e: {} for e in self.ENG}
        for e in self.ENG:
            self._sem("E_" + e)

    def _sem(self, name):
        if name not in self.sems:
            self.sems[name] = self.es.enter_context(self.nc.semaphore(name))
            self.cnt[name] = 0
        return name

    def _deps(self, eng, r, w):
        waits = {}

        def add(s, v):
            if eng == "pe" and s == "E_pe":
                return
            if waits.get(s, 0) < v:
                waits[s] = v

        for k in r:
            t = self.lastw.get(k)
            if t:
                add(*t)
        for k in w:
            t = self.lastw.get(k)
            if t:
                add(*t)
            for s, v in self.readers.get(k, {}).items():
                add(s, v)
        out = []
        for s, v in waits.items():
            if self.seen[eng].get(s, 0) < v:
                self.seen[eng][s] = v
                out.append((s, v))
        return out

    def _commit(self, tok, r, w):
        for k in r:
            d = self.readers.setdefault(k, {})
            if d.get(tok[0], 0) < tok[1]:
                d[tok[0]] = tok[1]
        for k in w:
            self.lastw[k] = tok
            self.readers[k] = {}

    def op(self, eng, fn, r=(), w=()):
        waits = self._deps(eng, r, w)
        s = "E_" + eng
        self.cnt[s] += 1
        self.q[eng].append((fn, waits, s, 1))
        self._commit((s, self.cnt[s]), r, w)

    def dma(self, eng, sem, out, in_, r=(), w=()):
        self._sem(sem)
        waits = self._deps(eng, r, w)
        self.cnt[sem] += 16
        self.q[eng].append((lambda e, o=out, i=in_: e.dma_start(out=o, in_=i), waits, sem, 16))
        self._commit((sem, self.cnt[sem]), r, w)

    nobar = ()

    def barrier(self):
        for e in self.ENG:
            waits = []
            for s, v in self.cnt.items():
                if s in self.nobar:
                    continue
                if v > 0 and self.seen[e].get(s, 0) < v and not (s == "E_" + e):
                    self.seen[e][s] = v
                    waits.append((s, v))
            if waits:
                self.q[e].append((None, waits, None, 0))
        self.lastw = {k: t for k, t in self.lastw.items() if t[0] in self.nobar}
        self.readers = {}

    def emit(self):
        nc = self.nc
        names = {"pe": "tensor", "act": "scalar", "dve": "vector", "pool": "gpsimd", "sp": "sync"}
        self.barrier()
        with nc.Block() as block:
            for e in self.ENG:
                def body(eng, e=e):
                    for fn, waits, s, inc in self.q[e]:
                        for ws, wv in waits:
                            eng.wait_ge(self.sems[ws], wv)
                        if fn is not None:
                            ins = fn(eng)
                            ins.then_inc(self.sems[s], inc)
                getattr(block, names[e])(body)


def build(cfg):
    nc = bass.Bass("TRN2", target_bir_lowering=False)
    NB, T, TC, DEPTH, NE = cfg.NB, cfg.T, cfg.TC, cfg.DEPTH, cfg.NE
    NX, NTOK = cfg.NX, cfg.NTOK
    na = (DEPTH + 1) // 2
    nb_ = DEPTH // 2

    def din(name, shape, dt=F32):
        return nc.dram_tensor(name, list(shape), dt, kind="ExternalInput").ap()

    xT_in = din("xT", [D, NX])
    cxT_in = din("cxT", [D, NB * TC])
    cT_in = din("cT", [128, 8 * (NB + 1)])
    cols_in = din("cols", [DEPTH, 128, cfg.NCOL])
    consts_in = din("consts", [128, 6 * 128])
    ada_w = din("ada_w", [DEPTH, D, 6 * D])
    router_w = din("router_w", [DEPTH, D, NE])
    router_bb = din("router_bb", [DEPTH, 128, NE])
    sel_in = din("sel", [NE, NE * 128])
    moe_w_gu = din("moe_w_gu", [DEPTH, NE, D, 2 * D])
    moe_w_dn = din("moe_w_dn", [DEPTH, NE, D, D])
    moe_b_dn = din("moe_b_dn", [DEPTH, NE, D])
    final_g = din("final_g", [128, 8])
    gm_w_in = gm_w_out = gm_w_sT = gm_rows = gd_w_in = gd_w_out = None
    if nb_ > 0 and cfg.mixers[1]:
        gm_w_in = din("gm_w_in", [nb_, D, 2 * GW])
        gm_w_out = din("gm_w_out", [nb_, GW, D])
        gm_w_sT = din("gm_w_sT", [nb_, 8, 128, 128])
        gm_rows = din("gm_rows", [nb_, 128, 3 * GW + 8 * 128])
    if na > 0 and cfg.mixers[0]:
        gd_w_in = din("gd_w_in", [na, D, 4128])
        gd_w_out = din("gd_w_out", [na, D, D])
    y_out = nc.dram_tensor("yT", [D, NX], F32, kind="ExternalOutput").ap()
    cfg.dbg = None
    if getattr(cfg, "debug", False):
        cfg.dbg = (nc.dram_tensor("dbg_acc", [D, 512], F32, kind="ExternalOutput").ap(),
                   nc.dram_tensor("dbg_comb", [NE, 512], F32, kind="ExternalOutput").ap(),
                   nc.dram_tensor("dbg_h", [D, 512], BF16, kind="ExternalOutput").ap())
    xs = nc.dram_tensor("xs", [D, NTOK], F32).ap()
    gscr = None
    if na > 0 and cfg.mixers[0]:
        gscr = (nc.dram_tensor("g_pj", [4096, NTOK], F32).ap(), nc.dram_tensor("g_gb", [32, NTOK], F32).ap(),
                nc.dram_tensor("g_qk", [3072, NTOK], F32).ap(), nc.dram_tensor("g_ot", [2, NTOK, 1024], F32).ap())

    es = ExitStack()
    P = Prog(nc, es)

    uid = [0]

    def sb(name, shape, dt=F32, st=es):
        uid[0] += 1
        return st.enter_context(nc.sbuf_tensor("s%d_%s" % (uid[0], name), list(shape), dt))

    PS = [es.enter_context(nc.psum_tensor("ps%d" % i, [128, 512], F32)) for i in range(8)]
    psn = [0]

    def nextps():
        i = psn[0] % 8
        psn[0] += 1
        return i

    consts = sb("consts", [128, 6 * 128])
    ident = consts[:, 0:128]
    onesm = consts[:, 128:256]
    ones1 = consts[:, 256:384]
    epsc = consts[:, 384:385]
    P.dma("sp", "d_const", consts[:], consts_in[:, :], w=["consts"])
    cT = sb("cT", [128, 8 * (NB + 1)])
    sT = sb("sT", [128, 8 * (NB + 1)], BF16)
    P.dma("sp", "d_const", cT[:], cT_in[:, :], w=["cT"])
    P.op("act", lambda e: e.activation(out=sT[:], in_=cT[:], func=AF.Silu), r=["cT"], w=["sT"])
    cols = sb("cols", [128, cfg.NCOL])
    modT = sb("modT", [128, 48 * (NB + 1)])
    modA = sb("modA", [128, 2 * 8 * (NB + 1)])
    fing = sb("fing", [128, 8])
    P.dma("sp", "d_const", fing[:], final_g[:, :], w=["fing"])
    selt = None

    def mcol(j, b):
        return modT[:, j * (NB + 1) + b: j * (NB + 1) + b + 1]

    def acol(which, c, b):
        i = (which * 8 + c) * (NB + 1) + b
        return modA[:, i:i + 1]

    def tiles(seq_len, base, nseq, tile, midx_fn):
        out = []
        for s in range(nseq):
            for t0 in range(0, seq_len, tile):
                n = min(tile, seq_len - t0)
                out.append((base + s * seq_len + t0, n, midx_fn(s)))
        return out

    x_tiles = tiles(T, 0, NB, 512, lambda s: s)
    c_tiles = tiles(TC, NX, NB, 512, lambda s: NB)

    for (src, base, n) in ((xT_in, 0, NX), (cxT_in, NX, NB * TC)):
        for t0 in range(0, n, 2048):
            nn = min(2048, n - t0)
            P.dma("sp", "d_cp", xs[:, base + t0: base + t0 + nn], src[:, t0:t0 + nn], w=[("xs", base + t0 + i) for i in range(0, nn, 128)])

    def xs_keys(c0, n):
        return [("xs", c0 + i) for i in range(0, n, 128)]

    xs_v = xs.rearrange("(c p) t -> p c t", p=128)

    def norm_mod(st, xt, n, which, mi, h16, h32=None, tag="nm"):
        sq = st["sq"]
        xk = [("xt", c) for c in range(8)]
        P.op("act", lambda e: e.activation(out=sq[:, :, :n], in_=xt[:, :, :n], func=AF.Square), r=xk, w=["sq"] + [("tmp", c) for c in range(8)])
        pi = nextps()
        ps = PS[pi]

        def mm(e):
            for c in range(8):
                ins = e.matmul(ps[:, :n], onesm, sq[:, c, :n], start=(c == 0), stop=(c == 7))
            return ins
        P.op("pe", mm, r=["sq", "consts"], w=[("ps", pi)])
        rstd = st["rstd"]
        P.op("act", lambda e: e.activation(out=rstd[:, :n], in_=ps[:, :n], func=AF.Sqrt, bias=epsc, scale=1.0), r=[("ps", pi), "consts"], w=["rstd"])
        P.op("dve", lambda e: e.reciprocal(out=rstd[:, :n], in_=rstd[:, :n]), r=["rstd"], w=["rstd"])
        tmp = st["tmp"]
        for c in range(8):
            P.op("dve", lambda e, c=c: e.tensor_tensor(out=tmp[:, c, :n], in0=xt[:, c, :n], in1=rstd[:, :n], op=ALU.mult),
                 r=[("xt", c), "rstd", "sq"], w=[("tmp", c)])
            dst = h32 if h32 is not None else h16
            P.op("act", lambda e, c=c, dst=dst: e.activation(out=dst[:, c, :n], in_=tmp[:, c, :n], func=AF.Identity,
                                                              scale=acol(which, c, mi), bias=mcol((0 if which == 0 else 3) * 8 + c, mi)),
                 r=[("tmp", c), "modA", "modT"], w=[("xt", c)] if h32 is not None else [("h16", c)])
            if h32 is not None:
                P.op("pool", lambda e, c=c: e.tensor_copy(out=h16[:, c, :n], in_=h32[:, c, :n]), r=[("xt", c)], w=[("h16", c)])

    for l in range(DEPTH):
        j = l // 2
        ctx_in = l <= cfg.last_ctx_reader
        ctx_out = l < cfg.last_ctx_reader
        P.barrier()
        P.dma("sp", "d_cols", cols[:], cols_in[l, :, :], w=["cols"])
        with ExitStack() as st_:
            aw = [sb("aw%d" % i, [128, 8, 1024], BF16, st_) for i in range(2)]
            awv = ada_w[l].rearrange("(k p) n -> p k n", p=128)
            for g6 in range(6):
                a = aw[g6 % 2]
                ak = "aw%d" % (g6 % 2)
                P.dma("pool", "d_" + ak, a[:], awv[:, :, g6 * 1024:(g6 + 1) * 1024], w=[ak])
                for jj in range(8):
                    jg = g6 * 8 + jj
                    pi = nextps()
                    ps = PS[pi]

                    def mm(e, a=a, jj=jj, ps=ps):
                        for k in range(8):
                            ins = e.matmul(ps[:, :NB + 1], a[:, k, jj * 128:(jj + 1) * 128], sT[:, k * (NB + 1):(k + 1) * (NB + 1)],
                                           start=(k == 0), stop=(k == 7))
                        return ins
                    P.op("pe", mm, r=[ak, "sT"], w=[("ps", pi)])
                    P.op("dve", lambda e, jg=jg, ps=ps: e.tensor_scalar(out=modT[:, jg * (NB + 1):(jg + 1) * (NB + 1)], in0=ps[:, :NB + 1],
                                                                         scalar1=cols[:, cfg.o_adab + jg: cfg.o_adab + jg + 1], scalar2=None, op0=ALU.add),
                         r=[("ps", pi), "cols"], w=["modT"])
            for which, grp, og in ((0, 1, cfg.o_n1), (1, 4, cfg.o_n2)):
                for c in range(8):
                    jg = grp * 8 + c
                    i0 = (which * 8 + c) * (NB + 1)
                    P.op("dve", lambda e, jg=jg, i0=i0, og=og, c=c: e.tensor_scalar(
                        out=modA[:, i0:i0 + NB + 1], in0=modT[:, jg * (NB + 1):(jg + 1) * (NB + 1)],
                        scalar1=1.0, scalar2=cols[:, og + c: og + c + 1], op0=ALU.add, op1=ALU.mult),
                        r=["modT", "cols"], w=["modA"])
        P.barrier()

        if l % 2 == 1 and cfg.mixers[1]:
            gmlp_layer(nc, cfg, P, sb, PS, nextps, l, j, xs_v, xs_keys, norm_mod, mcol, cols,
                       gm_w_in, gm_w_out, gm_w_sT, gm_rows, x_tiles, c_tiles, ctx_out, consts)
            P.barrier()
        if l % 2 == 0 and cfg.mixers[0]:
            gdn_layer(nc, cfg, P, sb, PS, nextps, l, j, xs_v, xs_keys, norm_mod, mcol, cols, consts,
                      gd_w_in, gd_w_out, None, x_tiles, c_tiles, ctx_out, gscr)
            P.barrier()

        if cfg.moe:
            toks = list(x_tiles) + (list(c_tiles) if ctx_out else [])
            moe_layer(nc, cfg, P, sb, PS, nextps, l, xs_v, xs_keys, norm_mod, mcol, cols, consts, selt,
                      router_w, router_bb, moe_w_gu, moe_w_dn, moe_b_dn, toks)
            P.barrier()

    with ExitStack() as st_:
        xt2 = [sb("fx%d" % i, [128, 8, 512], F32, st_) for i in range(2)]
        sq = sb("fsq", [128, 8, 512], F32, st_)
        rstd = sb("frstd", [128, 512], F32, st_)
        yo = [sb("fy%d" % i, [128, 8, 512], F32, st_) for i in range(2)]
        yv = y_out.rearrange("(c p) t -> p c t", p=128)
        for ti, (c0, n, mi) in enumerate(x_tiles):
            xt = xt2[ti % 2]
            xk = "fx%d" % (ti % 2)
            yk = "fy%d" % (ti % 2)
            yt = yo[ti % 2]
            P.dma("sp", "d_" + xk, xt[:, :, :n], xs_v[:, :, c0:c0 + n], r=xs_keys(c0, n), w=[xk])
            P.op("act", lambda e, xt=xt, n=n: e.activation(out=sq[:, :, :n], in_=xt[:, :, :n], func=AF.Square), r=[xk], w=["fsq"])
            pi = nextps()
            ps = PS[pi]

            def mm(e, ps=ps, n=n):
                for c in range(8):
                    ins = e.matmul(ps[:, :n], onesm, sq[:, c, :n], start=(c == 0), stop=(c == 7))
                return ins
            P.op("pe", mm, r=["fsq", "consts"], w=[("ps", pi)])
            P.op("act", lambda e, ps=ps, n=n: e.activation(out=rstd[:, :n], in_=ps[:, :n], func=AF.Sqrt, bias=epsc, scale=1.0), r=[("ps", pi), "consts"], w=["frstd"])
            P.op("dve", lambda e, n=n: e.reciprocal(out=rstd[:, :n], in_=rstd[:, :n]), r=["frstd"], w=["frstd"])
            for c in range(8):
                P.op("dve", lambda e, c=c, xt=xt, yt=yt, n=n: e.scalar_tensor_tensor(
                    out=yt[:, c, :n], in0=xt[:, c, :n], scalar=fing[:, c:c + 1], in1=rstd[:, :n], op0=ALU.mult, op1=ALU.mult),
                    r=[xk, "frstd", "fing"], w=[yk])
            P.dma("sp", "d_" + yk, yv[:, :, c0:c0 + n], yt[:, :, :n], r=[yk], w=[("y", c0)])
    P.emit()
    es.close()
    return nc


def moe_layer(nc, cfg, P, sb, PS, nextps, l, xs_v, xs_keys, norm_mod, mcol, cols, consts, selt,
              router_w, router_bb, moe_w_gu, moe_w_dn, moe_b_dn, toks):
    NE, NB = cfg.NE, cfg.NB
    ident = consts[:, 0:128]
    GT = 1024
    groups, cur, curn = [], [], 0
    for t in toks:
        if curn + t[1] > GT:
            groups.append(cur)
            cur, curn = [], 0
        cur.append(t)
        curn += t[1]
    if cur:
        groups.append(cur)
    with ExitStack() as st_:
        h16 = sb("m_h16", [128, 8, GT], BF16, st_)
        acc = sb("m_acc", [128, 8, GT], F32, st_)
        combT = sb("m_combT", [NE, GT], F32, st_)
        wgu = [sb("m_wgu%d" % i, [128, 8, 2048], BF16, st_) for i in range(2)]
        wdn = [sb("m_wdn%d" % i, [128, 8, 1024], BF16, st_) for i in range(2)]
        NWDN = 2
        cm2 = [sb("m_cm%d" % i, [NE, 512], F32, st_) for i in range(2)]
        P.nobar = ()
        with ExitStack() as tmp_:
            big = sb("m_big", [128, 8, 512], F32, tmp_)
            sq = sb("m_sq", [128, 8, 512], F32, tmp_)
        hmid = [sb("m_hmid%d" % i, [128, 8, 512], BF16, st_) for i in range(2)]
        gg = [sb("m_g%d" % i, [128, 512], F32, st_) for i in range(2)]
        sg = [sb("m_s%d" % i, [128, 512], F32, st_) for i in range(2)]
        uu = [sb("m_u%d" % i, [128, 512], F32, st_) for i in range(2)]
        cbc = sb("m_cbc", [128, 512], F32, st_)
        stn = {"sq": sq, "rstd": sb("m_rstd", [128, 512], F32, st_), "tmp": sq}
        h32 = big
        rw = sb("m_rw", [128, 8, NE], F32, st_)
        rbb = sb("m_rbb", [128, NE], F32, st_)
        bdn = sb("m_bdn", [NE, 1024], F32, st_)
        lg = sb("m_lg", [128, 4, NE], F32, st_)
        top8 = sb("m_top8", [128, 4, 8], F32, st_)
        ex = sb("m_ex", [128, 4, NE], F32, st_)
        ssum = sb("m_ssum", [128, 4], F32, st_)
        bu1 = sb("m_bu1", [128, NE * 8], F32, st_)
        P.dma("sp", "d_mrw", rw[:], router_w[l].rearrange("(k p) n -> p k n", p=128), w=["rw"])
        P.dma("sp", "d_mrbb", rbb[:], router_bb[l, :, :], w=["rbb"])
        P.dma("sp", "d_mbdn", bdn[:], moe_b_dn[l, :, :], w=["bdn"])
        for e_ in range(NE):
            P.op("dve", lambda e, e_=e_: e.tensor_scalar(out=bu1[:, e_ * 8:(e_ + 1) * 8],
                                                         in0=cols[:, cfg.o_bgu + e_ * 16 + 8: cfg.o_bgu + e_ * 16 + 16],
                                                         scalar1=1.0, scalar2=None, op0=ALU.add), r=["cols"], w=["bu1"])
        wload = [0]

        def load_gu(e_):
            s = wload[0] % 2
            wload[0] += 1
            for half in range(2):
                P.dma("pool", "d_wgu%d" % s, wgu[s][:, half * 4:(half + 1) * 4, :],
                      moe_w_gu[l, e_].rearrange("(k p) n -> p k n", p=128)[:, half * 4:(half + 1) * 4, :], w=[("wgu", s, half)])
            return s

        dnload = [0]

        def load_dn(e_):
            sd = dnload[0] % 2
            dnload[0] += 1
            P.dma("pool", "d_wdn%d" % sd, wdn[sd][:], moe_w_dn[l, e_].rearrange("(k p) n -> p k n", p=128), w=[("wdn", sd)])
            return sd

        ones1 = consts[:, 256:384]
        pending = [None]
        for gi, grp in enumerate(groups):
            P.barrier()
            off = 0
            offs = []
            for (c0, n, mi) in grp:
                offs.append(off)
                nblk = n // 128
                P.dma("sp", "d_mbig", big[:, :, :n], xs_v[:, :, c0:c0 + n], r=xs_keys(c0, n), w=[("xt", c) for c in range(8)])
                norm_mod(stn, big, n, 1, mi, h16[:, :, off:off + n], h32=h32)
                pi = nextps()
                ps = PS[pi]

                def mm(e, ps=ps, nblk=nblk):
                    for b in range(nblk):
                        for k in range(8):
                            ins = e.matmul(ps[:, b * NE:(b + 1) * NE], h32[:, k, b * 128:(b + 1) * 128], rw[:, k, :], start=(k == 0), stop=(k == 7))
                    return ins
                P.op("pe", mm, r=[("xt", c) for c in range(8)] + ["rw"], w=[("ps", pi)])
                for b in range(nblk):
                    P.op("dve", lambda e, b=b, ps=ps: e.tensor_tensor(out=lg[:, b, :], in0=ps[:, b * NE:(b + 1) * NE], in1=rbb[:], op=ALU.add),
                         r=[("ps", pi), "rbb"], w=[("lg", b)])
                    P.op("dve", lambda e, b=b: e.max(out=top8[:, b, :], in_=lg[:, b, :]), r=[("lg", b)], w=[("top8", b)])
                    P.op("dve", lambda e, b=b: e.tensor_scalar(out=ex[:, b, :], in0=lg[:, b, :], scalar1=top8[:, b, 0:1], scalar2=None, op0=ALU.subtract),
                         r=[("lg", b), ("top8", b)], w=[("ex", b)])
                    P.op("act", lambda e, b=b: e.activation(out=ex[:, b, :], in_=ex[:, b, :], func=AF.Exp), r=[("ex", b)], w=[("ex", b)])
                    P.op("dve", lambda e, b=b: e.scalar_tensor_tensor(out=ex[:, b, :], in0=lg[:, b, :], scalar=top8[:, b, TOPK - 1:TOPK], in1=ex[:, b, :],
                                                                      op0=ALU.is_ge, op1=ALU.mult), r=[("lg", b), ("top8", b), ("ex", b)], w=[("ex", b)])
                    P.op("dve", lambda e, b=b: e.tensor_reduce(out=ssum[:, b:b + 1], in_=ex[:, b, :], axis=AX.X, op=ALU.add), r=[("ex", b)], w=[("ssum", b)])
                    P.op("dve", lambda e, b=b: e.reciprocal(out=ssum[:, b:b + 1], in_=ssum[:, b:b + 1]), r=[("ssum", b)], w=[("ssum", b)])
                    P.op("dve", lambda e, b=b: e.tensor_scalar(out=ex[:, b, :], in0=ex[:, b, :], scalar1=ssum[:, b:b + 1], scalar2=None, op0=ALU.mult),
                         r=[("ex", b), ("ssum", b)], w=[("ex", b)])
                pi2 = nextps()
                ps2 = PS[pi2]

                def tr(e, ps2=ps2, nblk=nblk):
                    for b in range(nblk):
                        ins = e.transpose(ps2[:NE, b * 128:(b + 1) * 128], ex[:, b, :], ident)
                    return ins
                P.op("pe", tr, r=[("ex", b) for b in range(nblk)] + ["consts"], w=[("ps", pi2)])
                P.op("act", lambda e, ps2=ps2, off=off, n=n: e.copy(out=combT[:, off:off + n], in_=ps2[:NE, :n]), r=[("ps", pi2)], w=[("combT", off)])
                for m in range(8):
                    pi3 = nextps()
                    ps3 = PS[pi3]
                    P.op("pe", lambda e, ps3=ps3, m=m, off=off, n=n: e.matmul(ps3[:, :n], bdn[:, m * 128:(m + 1) * 128], combT[:, off:off + n], start=True, stop=True),
                         r=["bdn", ("combT", off)], w=[("ps", pi3)])
                    P.op("act", lambda e, ps3=ps3, m=m, off=off, n=n: e.copy(out=acc[:, m, off:off + n], in_=ps3[:, :n]), r=[("ps", pi3)], w=[("acc", m, off)])
                off += n
            P.barrier()
            if pending[0] is None:
                pending[0] = (load_gu(0), load_dn(0))
            slots = {0: pending[0]}
            pending[0] = None
            units = [(e_, ti) for e_ in range(NE) for ti in range(len(grp))]

            def emit_gu(ui):
                e_, ti = units[ui]
                (c0, n, mi) = grp[ti]
                off = offs[ti]
                s = slots[e_][0]
                hm = hmid[ui % 2]
                hk = "hmid%d" % (ui % 2)
                def prep_cm(uj):
                    ee, tj = units[uj]
                    nj, oj = grp[tj][1], offs[tj]
                    cmj = cm2[uj % 2]
                    P.op("dve", lambda e: e.tensor_scalar(out=cmj[:, :nj], in0=combT[:, oj:oj + nj], scalar1=ident[:NE, ee:ee + 1], scalar2=None, op0=ALU.mult),
                         r=[("combT", oj), "consts"], w=["cm%d" % (uj % 2)])
                if ui == 0:
                    prep_cm(0)
                cm = cm2[ui % 2]
                pi = nextps()
                ps = PS[pi]
                P.op("pe", lambda e, ps=ps, n=n, cm=cm: e.matmul(ps[:, :n], ones1[:NE, :], cm[:, :n], start=True, stop=True), r=["cm%d" % (ui % 2), "consts"], w=[("ps", pi)])
                if ui + 1 < len(units):
                    prep_cm(ui + 1)
                P.op("act", lambda e, ps=ps, n=n: e.copy(out=cbc[:, :n], in_=ps[:, :n]), r=[("ps", pi)], w=["cbc"])
                for jp in range(8):
                    pg, pu = nextps(), nextps()
                    psg, psu = PS[pg], PS[pu]

                    def mmgu(e, psg=psg, psu=psu, jp=jp, s=s, off=off, n=n):
                        for k in range(8):
                            e.matmul(psg[:, :n], wgu[s][:, k, jp * 128:(jp + 1) * 128], h16[:, k, off:off + n], start=(k == 0), stop=(k == 7))
                        for k in range(8):
                            ins = e.matmul(psu[:, :n], wgu[s][:, k, 1024 + jp * 128:1024 + (jp + 1) * 128], h16[:, k, off:off + n], start=(k == 0), stop=(k == 7))
                        return ins
                    P.op("pe", mmgu, r=[("wgu", s, 0), ("wgu", s, 1)] + [("h16", c) for c in range(8)], w=[("ps", pg), ("ps", pu)])
                    g_, s_, u_ = gg[jp % 2], sg[jp % 2], uu[jp % 2]
                    gk, sk, uk = "gg%d" % (jp % 2), "sg%d" % (jp % 2), "uu%d" % (jp % 2)
                    bcol = cols[:, cfg.o_bgu + e_ * 16 + jp: cfg.o_bgu + e_ * 16 + jp + 1]
                    P.op("dve", lambda e, g_=g_, psg=psg, bcol=bcol, n=n: e.tensor_scalar(out=g_[:, :n], in0=psg[:, :n], scalar1=bcol, scalar2=LIMIT, op0=ALU.add, op1=ALU.min),
                         r=[("ps", pg), "cols"], w=[gk])
                    P.op("act", lambda e, g_=g_, s_=s_, n=n: e.activation(out=s_[:, :n], in_=g_[:, :n], func=AF.Sigmoid, scale=ALPHA), r=[gk], w=[sk])
                    P.op("dve", lambda e, u_=u_, psu=psu, e_=e_, jp=jp, n=n: e.tensor_scalar(out=u_[:, :n], in0=psu[:, :n], scalar1=bu1[:, e_ * 8 + jp: e_ * 8 + jp + 1],
                                                                                     scalar2=LIMIT + 1.0, op0=ALU.add, op1=ALU.min), r=[("ps", pu), "bu1"], w=[uk])
                    P.op("pool", lambda e, g_=g_, s_=s_, n=n: e.tensor_tensor(out=s_[:, :n], in0=g_[:, :n], in1=s_[:, :n], op=ALU.mult), r=[gk, sk], w=[sk])
                    P.op("pool", lambda e, s_=s_, n=n: e.tensor_tensor(out=s_[:, :n], in0=s_[:, :n], in1=cbc[:, :n], op=ALU.mult), r=[sk, "cbc"], w=[sk])
                    P.op("dve", lambda e, u_=u_, s_=s_, hm=hm, jp=jp, n=n: e.scalar_tensor_tensor(out=hm[:, jp, :n], in0=u_[:, :n], scalar=1.0 - LIMIT, in1=s_[:, :n],
                                                                                           op0=ALU.max, op1=ALU.mult), r=[uk, sk], w=[(hk, jp)])

            def emit_dn(ui):
                e_, ti = units[ui]
                (c0, n, mi) = grp[ti]
                off = offs[ti]
                sd = slots[e_][1]
                hm = hmid[ui % 2]
                hk = "hmid%d" % (ui % 2)
                for m in range(8):
                    pi = nextps()
                    ps = PS[pi]

                    def mmdn(e, ps=ps, m=m, sd=sd, hm=hm, n=n):
                        for k in range(8):
                            ins = e.matmul(ps[:, :n], wdn[sd][:, k, m * 128:(m + 1) * 128], hm[:, k, :n], start=(k == 0), stop=(k == 7))
                        return ins
                    P.op("pe", mmdn, r=[("wdn", sd)] + [(hk, k) for k in range(8)], w=[("ps", pi)])
                    P.op("dve", lambda e, ps=ps, m=m, off=off, n=n: e.tensor_tensor(out=acc[:, m, off:off + n], in0=acc[:, m, off:off + n], in1=ps[:, :n], op=ALU.add),
                         r=[("ps", pi), ("acc", m, off)], w=[("acc", m, off)])

            for ui in range(len(units)):
                e_, ti = units[ui]
                emit_gu(ui)
                if ui > 0:
                    emit_dn(ui - 1)
                if ti == 0:
                    if e_ + 1 < NE:
                        slots[e_ + 1] = (load_gu(e_ + 1), load_dn(e_ + 1))
            emit_dn(len(units) - 1)
            P.barrier()
            if cfg.dbg is not None and grp is groups[0]:
                P.dma("sp", "d_dbg", cfg.dbg[0].rearrange("(c p) t -> p c t", p=128), acc[:, :, 0:512], w=["dbg0"])
                P.dma("sp", "d_dbg", cfg.dbg[1][:, :], combT[:, 0:512], w=["dbg1"])
                P.dma("sp", "d_dbg", cfg.dbg[2].rearrange("(c p) t -> p c t", p=128), h16[:, :, 0:512], w=["dbg2"])
                P.barrier()
            for ti, (c0, n, mi) in enumerate(grp):
                off = offs[ti]
                P.dma("sp", "d_mbig", big[:, :, :n], xs_v[:, :, c0:c0 + n], r=xs_keys(c0, n), w=[("xt", c) for c in range(8)])
                for m in range(8):
                    P.op("dve", lambda e, m=m, off=off, n=n, mi=mi: e.scalar_tensor_tensor(out=big[:, m, :n], in0=acc[:, m, off:off + n], scalar=mcol(5 * 8 + m, mi),
                                                                                         in1=big[:, m, :n], op0=ALU.mult, op1=ALU.add),
                         r=[("acc", m, off), ("xt", m), "modT"], w=[("xt", m)])
                P.dma("sp", "d_mst", xs_v[:, :, c0:c0 + n], big[:, :, :n], r=[("xt", c) for c in range(8)], w=xs_keys(c0, n))


def gdn_layer(nc, cfg, P, sb, PS, nextps, l, j, xs_v, xs_keys, norm_mod, mcol, cols, consts,
              gd_w_in, gd_w_out, gd_rows, x_tiles, c_tiles, ctx_out, scr):
    NB, T, TC = cfg.NB, cfg.T, cfg.TC
    NX, NTOK = cfg.NX, cfg.NTOK
    ident = consts[:, 0:128]
    ones1 = consts[:, 256:384]
    epsc = consts[:, 384:385]
    om = cfg.o_mix
    pj, gb, qkvn, oTok = scr
    pj_v = pj.rearrange("(c p) t -> p c t", p=128)
    qk_v = qkvn.rearrange("(c p) t -> p c t", p=128)
    toks = list(x_tiles) + list(c_tiles)
    with ExitStack() as st_:
        w16 = sb("g_w", [128, 8, 4128], BF16, st_)
        wv = gd_w_in[j].rearrange("(k p) n -> p k n", p=128)
        for q4 in range(4):
            P.dma("pool", "d_gw%d" % q4, w16[:, :, q4 * 1032:(q4 + 1) * 1032], wv[:, :, q4 * 1032:(q4 + 1) * 1032], w=[("gw", q4)])
        gwk = [("gw", q4) for q4 in range(4)]
        big = sb("g_big", [128, 8, 512], F32, st_)
        sq = sb("g_sq", [128, 8, 512], F32, st_)
        stn = {"sq": sq, "rstd": sb("g_rstd", [128, 512], F32, st_), "tmp": sq}
        h16 = sb("g_h16", [128, 8, 512], BF16, st_)
        ob = [sb("g_ob%d" % i, [128, 512], F32, st_) for i in range(4)]
        gt = sb("g_gt", [32, 512], F32, st_)
        gt2 = sb("g_gt2", [32, 512], F32, st_)
        nega = sb("g_nega", [32, 1], F32, st_)
        P.op("act", lambda e: e.activation(out=nega[:], in_=cols[:32, om + 121:om + 122], func=AF.Exp), r=["cols"], w=["nega"])
        P.op("dve", lambda e: e.tensor_scalar(out=nega[:], in0=nega[:], scalar1=cols[:32, om + 124:om + 125], scalar2=-1.0, op0=ALU.mult, op1=ALU.mult), r=["nega", "cols"], w=["nega"])
        for (c0, n, mi) in toks:
            P.dma("sp", "d_gbig", big[:, :, :n], xs_v[:, :, c0:c0 + n], r=xs_keys(c0, n), w=[("xt", c) for c in range(8)])
            norm_mod(stn, big, n, 0, mi, h16)
            for jc in range(32):
                pi = nextps()
                ps = PS[pi]

                def mm(e, ps=ps, jc=jc, n=n):
                    for k in range(8):
                        ins = e.matmul(ps[:, :n], w16[:, k, jc * 128:(jc + 1) * 128], h16[:, k, :n], start=(k == 0), stop=(k == 7))
                    return ins
                P.op("pe", mm, r=gwk + [("h16", c) for c in range(8)], w=[("ps", pi)])
                o_ = ob[jc % 4]
                okk = "gob%d" % (jc % 4)
                if jc < 24:
                    P.op("act", lambda e, o_=o_, ps=ps, n=n: e.copy(out=o_[:, :n], in_=ps[:, :n]), r=[("ps", pi)], w=[okk])
                else:
                    P.op("act", lambda e, o_=o_, ps=ps, n=n: e.activation(out=o_[:, :n], in_=ps[:, :n], func=AF.Silu), r=[("ps", pi)], w=[okk])
                P.dma("sp", "d_" + okk, pj[jc * 128:(jc + 1) * 128, c0:c0 + n], o_[:, :n], r=[okk], w=[("pj", jc, c0)])
            pi = nextps()
            ps = PS[pi]

            def mm2(e, ps=ps, n=n):
                for k in range(8):
                    ins = e.matmul(ps[:32, :n], w16[:, k, 4096:4128], h16[:, k, :n], start=(k == 0), stop=(k == 7))
                return ins
            P.op("pe", mm2, r=gwk + [("h16", c) for c in range(8)], w=[("ps", pi)])
            P.op("act", lambda e, ps=ps, n=n: e.activation(out=gt[:, :n], in_=ps[:32, :n], func=AF.Exp, bias=cols[:32, om + 122:om + 123], scale=1.0), r=[("ps", pi), "cols"], w=["gt"])
            P.op("act", lambda e, n=n: e.activation(out=gt[:, :n], in_=gt[:, :n], func=AF.Ln, bias=ones1[:32, 0:1], scale=1.0), r=["gt", "consts"], w=["gt"])
            P.op("act", lambda e, ps=ps, n=n: e.activation(out=gt2[:, :n], in_=ps[:32, :n], func=AF.Sigmoid), r=[("ps", pi)], w=["gt2"])
            P.op("dve", lambda e, n=n: e.tensor_scalar(out=gt[:, :n], in0=gt[:, :n], scalar1=nega[:, 0:1], scalar2=None, op0=ALU.mult), r=["gt", "nega"], w=["gt"])
            P.op("dve", lambda e, n=n: e.scalar_tensor_tensor(out=gt2[:, :n], in0=gt2[:, :n], scalar=cols[:32, om + 123:om + 124], in1=gt[:, :n], op0=ALU.mult, op1=ALU.add),
                 r=["gt", "gt2", "cols"], w=["gt2"])
            P.dma("sp", "d_ggb", gb[:, c0:c0 + n], gt2[:, :n], r=["gt2"], w=[("gb", c0)])
    P.barrier()
    with ExitStack() as st_:
        buf = [sb("c_buf%d" % i, [128, 516], F32, st_) for i in range(2)]
        ac = [sb("c_ac%d" % i, [128, 512], F32, st_) for i in range(2)]
        sqq = sb("c_sq", [128, 512], F32, st_)
        rr = sb("c_rr", [128, 512], F32, st_)
        it = 0
        for (c0, n, mi) in toks:
            seq0 = (c0 // T) * T if c0 < NX else NX + ((c0 - NX) // TC) * TC
            seqn = T if c0 < NX else TC
            first = (c0 == seq0)
            last = (c0 + n == seq0 + seqn)
            for jc in range(24):
                b_ = buf[it % 2]
                bk = "cbuf%d" % (it % 2)
                a_ = ac[it % 2]
                ak = "cac%d" % (it % 2)
                it += 1
                lo = 0 if first else 2
                hi = 0 if last else 2
                if first:
                    P.op("pool", lambda e, b_=b_: e.memset(b_[:, 0:2], 0.0), w=[bk])
                if last:
                    P.op("pool", lambda e, b_=b_, n=n: e.memset(b_[:, n + 2:n + 4], 0.0), w=[bk])
                P.dma("sp", "d_" + bk, b_[:, 2 - lo:n + 2 + hi], pj[jc * 128:(jc + 1) * 128, c0 - lo:c0 + n + hi], r=[("pj", jc, c0)], w=[bk])
                for tp in range(5):
                    wc = cols[:, om + tp * 24 + jc: om + tp * 24 + jc + 1]
                    if tp == 0:
                        P.op("dve", lambda e, a_=a_, b_=b_, wc=wc, n=n: e.tensor_scalar(out=a_[:, :n], in0=b_[:, 0:n], scalar1=wc, scalar2=None, op0=ALU.mult), r=[bk, "cols"], w=[ak])
                    else:
                        P.op("dve", lambda e, a_=a_, b_=b_, wc=wc, n=n, tp=tp: e.scalar_tensor_tensor(out=a_[:, :n], in0=b_[:, tp:tp + n], scalar=wc, in1=a_[:, :n], op0=ALU.mult, op1=ALU.add),
                             r=[bk, ak, "cols"], w=[ak])
                P.op("act", lambda e, a_=a_, n=n: e.activation(out=a_[:, :n], in_=a_[:, :n], func=AF.Silu), r=[ak], w=[ak])
                if jc < 16:
                    P.op("act", lambda e, a_=a_, n=n: e.activation(out=sqq[:, :n], in_=a_[:, :n], func=AF.Square), r=[ak], w=["csq"])
                    pi = nextps()
                    ps = PS[pi]
                    P.op("pe", lambda e, ps=ps, n=n: e.matmul(ps[:, :n], ones1, sqq[:, :n], start=True, stop=True), r=["csq", "consts"], w=[("ps", pi)])
                    P.op("act", lambda e, ps=ps, n=n: e.activation(out=rr[:, :n], in_=ps[:, :n], func=AF.Sqrt, bias=epsc, scale=1.0), r=[("ps", pi), "consts"], w=["crr"])
                    P.op("dve", lambda e, n=n: e.reciprocal(out=rr[:, :n], in_=rr[:, :n]), r=["crr"], w=["crr"])
                    sc = (128.0 ** -0.5) if jc < 8 else 1.0
                    P.op("dve", lambda e, a_=a_, n=n, sc=sc: e.scalar_tensor_tensor(out=a_[:, :n], in0=a_[:, :n], scalar=sc, in1=rr[:, :n], op0=ALU.mult, op1=ALU.mult), r=[ak, "crr"], w=[ak])
                P.dma("sp", "d_" + ak, qkvn[jc * 128:(jc + 1) * 128, c0:c0 + n], a_[:, :n], r=[ak], w=[("qk", jc, c0)])
    P.barrier()
    with ExitStack() as st_:
        S = sb("s_S", [128, NB * 8 * 128], F32, st_)
        qkv = [sb("s_qkv%d" % i, [128, 24, 64], F32, st_) for i in range(2)]
        gbr = sb("s_gbr", [32, 64], F32, st_)
        gtok = sb("s_gtok", [64, 32], F32, st_)
        gc = sb("s_gc", [64, 8], F32, st_)
        egc = sb("s_egc", [64, 8], F32, st_)
        ekt = sb("s_ekt", [64, 8], F32, st_)
        egl = sb("s_egl", [128, 8], F32, st_)
        glb = sb("s_glb", [128, 8], F32, st_)
        negb = sb("s_negb", [64, 8], F32, st_)
        obuf = [sb("s_obuf%d" % i, [64, 1024], F32, st_) for i in range(2)]
        W = {}
        for nm, shp in (("kbg", [64, 128]), ("ktl", [64, 128]), ("vt", [64, 128]), ("dg", [64, 64]), ("E", [64, 64]), ("Ei", [64, 64]), ("Es", [64, 64]),
                        ("aT", [64, 64]), ("Pa", [64, 64]), ("PaT", [64, 64]), ("Pb", [64, 64]), ("PbT", [64, 64]), ("Y", [64, 64]),
                        ("ub", [64, 128]), ("wT", [128, 64]), ("vn", [64, 128]), ("o1", [64, 128])):
            W[nm] = sb("s_" + nm, shp, F32, st_)
        def maskI(d):
            return consts[:64, 512 + d * 128: 512 + d * 128 + 64]

        def maskS(d):
            return consts[:64, 512 + d * 128 + 64: 512 + d * 128 + 128]

        def chunk_list(s, d):
            cx = [(NX + s * TC + i * 64) for i in range(TC // 64)]
            xx = [(s * T + i * 64) for i in range(T // 64)]
            return (cx + xx) if d == 0 else (cx[::-1] + xx[::-1])

        def ev(eng, out, in_, r, w):
            if eng == "act":
                P.op("act", lambda e: e.copy(out=out, in_=in_), r=r, w=w)
            else:
                P.op("dve", lambda e: e.tensor_copy(out=out, in_=in_), r=r, w=w)

        ci = 0
        for d in range(2):
            P.op("pool", lambda e: e.memset(S[:], 0.0), w=[("S", s, h) for s in range(NB) for h in range(8)])
            for s in range(NB):
                for c0 in chunk_list(s, d):
                    qb = qkv[ci % 2]
                    qkk = "qkv%d" % (ci % 2)
                    ob_ = obuf[ci % 2]
                    obk = "obuf%d" % (ci % 2)
                    ci += 1
                    P.dma("sp", "d_" + qkk, qb[:], qk_v[:, :, c0:c0 + 64], r=[("qk", jc, (c0 // 512) * 512 if False else None) for jc in range(0)], w=[qkk])
                    P.dma("sp", "d_gbr", gbr[:], gb[:, c0:c0 + 64], w=["gbr"])
                    pi = nextps()
                    ps = PS[pi]
                    P.op("pe", lambda e, ps=ps: e.transpose(ps[:64, :32], gbr[:, :], ident[:32, :32]), r=["gbr", "consts"], w=[("ps", pi)])
                    ev("act", gtok[:], ps[:64, :32], [("ps", pi)], ["gtok"])
                    P.op("dve", lambda e, d=d: e.tensor_scalar(out=negb[:], in0=gtok[:, 16 + d * 8:24 + d * 8], scalar1=-1.0, scalar2=None, op0=ALU.mult), r=["gtok"], w=["negb"])
                    pi = nextps()
                    ps = PS[pi]
                    P.op("pe", lambda e, ps=ps, d=d: e.matmul(ps[:64, :8], maskI(d), gtok[:, d * 8:d * 8 + 8], start=True, stop=True), r=["gtok", "consts"], w=[("ps", pi)])
                    ev("dve", gc[:], ps[:64, :8], [("ps", pi)], ["gc"])
                    pi = nextps()
                    ps = PS[pi]
                    P.op("pe", lambda e, ps=ps, d=d: e.matmul(ps[:, :8], ones1[:64, :], gtok[:, d * 8:d * 8 + 8], start=True, stop=True), r=["gtok", "consts"], w=[("ps", pi)])
                    ev("dve", glb[:], ps[:, :8], [("ps", pi)], ["glb"])
                    P.op("act", lambda e: e.activation(out=egl[:], in_=glb[:], func=AF.Exp), r=["glb"], w=["egl"])
                    P.op("act", lambda e: e.activation(out=egc[:], in_=gc[:], func=AF.Exp), r=["gc"], w=["egc"])
                    P.op("dve", lambda e: e.tensor_tensor(out=ekt[:], in0=glb[:64, :], in1=gc[:], op=ALU.subtract), r=["glb", "gc"], w=["ekt"])
                    P.op("act", lambda e: e.activation(out=ekt[:], in_=ekt[:], func=AF.Exp), r=["ekt"], w=["ekt"])
                    for h in range(8):
                        qT, kT, vT = qb[:, h, :], qb[:, 8 + h, :], qb[:, 16 + h, :]
                        Sh = S[:, (s * 8 + h) * 128:(s * 8 + h + 1) * 128]
                        Sk = ("S", s, h)
                        pk, pv = nextps(), nextps()
                        P.op("pe", lambda e, kT=kT, pk=pk: e.transpose(PS[pk][:64, :128], kT, ident), r=[qkk, "consts"], w=[("ps", pk)])
                        P.op("pe", lambda e, vT=vT, pv=pv: e.transpose(PS[pv][:64, :128], vT, ident), r=[qkk, "consts"], w=[("ps", pv)])
                        P.op("dve", lambda e, pk=pk, h=h: e.tensor_scalar(out=W["kbg"][:], in0=PS[pk][:64, :128], scalar1=egc[:, h:h + 1], scalar2=None, op0=ALU.mult), r=[("ps", pk), "egc"], w=["kbg"])
                        P.op("dve", lambda e, pk=pk, h=h: e.tensor_scalar(out=W["ktl"][:], in0=PS[pk][:64, :128], scalar1=ekt[:, h:h + 1], scalar2=None, op0=ALU.mult), r=[("ps", pk), "ekt"], w=["ktl"])
                        ev("act", W["vt"][:], PS[pv][:64, :128], [("ps", pv)], ["vt"])
                        P.op("dve", lambda e, h=h: e.tensor_scalar(out=W["dg"][:], in0=ident[:64, :64], scalar1=gc[:, h:h + 1], scalar2=None, op0=ALU.mult), r=["gc", "consts"], w=["dg"])
                        pr = nextps()
                        P.op("pe", lambda e, pr=pr: e.matmul(PS[pr][:64, :64], ones1[:64, :64], W["dg"][:], start=True, stop=True), r=["dg", "consts"], w=[("ps", pr)])
                        P.op("dve", lambda e, pr=pr, h=h: e.tensor_scalar(out=W["E"][:], in0=PS[pr][:64, :64], scalar1=gc[:, h:h + 1], scalar2=0.0, op0=ALU.subtract, op1=ALU.min), r=[("ps", pr), "gc"], w=["E"])
                        P.op("act", lambda e: e.activation(out=W["E"][:], in_=W["E"][:], func=AF.Exp), r=["E"], w=["E"])
                        P.op("pool", lambda e, d=d: e.tensor_tensor(out=W["Ei"][:], in0=W["E"][:], in1=maskI(d), op=ALU.mult), r=["E", "consts"], w=["Ei"])
                        P.op("pool", lambda e, d=d: e.tensor_tensor(out=W["Es"][:], in0=W["E"][:], in1=maskS(d), op=ALU.mult), r=["E", "consts"], w=["Es"])
                        pkk, pqk = nextps(), nextps()
                        P.op("pe", lambda e, kT=kT, pkk=pkk: e.matmul(PS[pkk][:64, :64], kT, kT, start=True, stop=True), r=[qkk], w=[("ps", pkk)])
                        P.op("pe", lambda e, kT=kT, qT=qT, pqk=pqk: e.matmul(PS[pqk][:64, :64], kT, qT, start=True, stop=True), r=[qkk], w=[("ps", pqk)])
                        P.op("dve", lambda e, pkk=pkk, h=h: e.scalar_tensor_tensor(out=W["PaT"][:], in0=PS[pkk][:64, :64], scalar=negb[:, h:h + 1], in1=W["Es"][:], op0=ALU.mult, op1=ALU.mult),
                             r=[("ps", pkk), "negb", "Es"], w=["PaT"])
                        P.op("dve", lambda e, pqk=pqk: e.tensor_tensor(out=W["aT"][:], in0=PS[pqk][:64, :64], in1=W["Ei"][:], op=ALU.mult), r=[("ps", pqk), "Ei"], w=["aT"])
                        pt = nextps()
                        P.op("pe", lambda e, pt=pt: e.transpose(PS[pt][:64, :64], W["PaT"][:], ident[:64, :64]), r=["PaT", "consts"], w=[("ps", pt)])
                        ev("act", W["Pa"][:], PS[pt][:64, :64], [("ps", pt)], ["Pa"])
                        P.op("dve", lambda e: e.tensor_tensor(out=W["Y"][:], in0=W["PaT"][:], in1=ident[:64, :64], op=ALU.add), r=["PaT", "consts"], w=["Y"])
                        cur, nxt = ("Pa", "PaT"), ("Pb", "PbT")
                        for lev in range(1, 6):
                            p1 = nextps()
                            P.op("pe", lambda e, p1=p1, cur=cur: e.matmul(PS[p1][:64, :64], W[cur[1]][:], W[cur[0]][:], start=True, stop=True), r=[cur[0], cur[1]], w=[("ps", p1)])
                            ev("act", W[nxt[0]][:], PS[p1][:64, :64], [("ps", p1)], [nxt[0]])
                            if lev < 5:
                                p2 = nextps()
                                P.op("pe", lambda e, p2=p2, cur=cur: e.matmul(PS[p2][:64, :64], W[cur[0]][:], W[cur[1]][:], start=True, stop=True), r=[cur[0], cur[1]], w=[("ps", p2)])
                                ev("dve", W[nxt[1]][:], PS[p2][:64, :64], [("ps", p2)], [nxt[1]])
                            p3 = nextps()
                            P.op("pe", lambda e, p3=p3, nxt=nxt: e.matmul(PS[p3][:64, :64], W[nxt[0]][:], W["Y"][:], start=True, stop=True), r=[nxt[0], "Y"], w=[("ps", p3)])
                            P.op("dve", lambda e, p3=p3: e.tensor_tensor(out=W["Y"][:], in0=W["Y"][:], in1=PS[p3][:64, :64], op=ALU.add), r=[("ps", p3), "Y"], w=["Y"])
                            cur, nxt = nxt, cur
                        pu, pw = nextps(), nextps()
                        P.op("pe", lambda e, pu=pu: e.matmul(PS[pu][:64, :128], W["Y"][:], W["vt"][:], start=True, stop=True), r=["Y", "vt"], w=[("ps", pu)])
                        P.op("pe", lambda e, pw=pw: e.matmul(PS[pw][:, :64], W["kbg"][:], W["Y"][:], start=True, stop=True), r=["Y", "kbg"], w=[("ps", pw)])
                        P.op("dve", lambda e, pu=pu, h=h, d=d: e.tensor_scalar(out=W["ub"][:], in0=PS[pu][:64, :128], scalar1=gtok[:, 16 + d * 8 + h:17 + d * 8 + h], scalar2=None, op0=ALU.mult),
                             r=[("ps", pu), "gtok"], w=["ub"])
                        ev("act", W["wT"][:], PS[pw][:, :64], [("ps", pw)], ["wT"])
                        p1, p2 = nextps(), nextps()
                        P.op("pe", lambda e, p1=p1, Sh=Sh: e.matmul(PS[p1][:64, :128], W["wT"][:], Sh, start=True, stop=True), r=["wT", Sk], w=[("ps", p1)])
                        P.op("pe", lambda e, p2=p2, Sh=Sh, qT=qT: e.matmul(PS[p2][:64, :128], qT, Sh, start=True, stop=True), r=[qkk, Sk], w=[("ps", p2)])
                        P.op("dve", lambda e, p1=p1, h=h: e.scalar_tensor_tensor(out=W["vn"][:], in0=PS[p1][:64, :128], scalar=negb[:, h:h + 1], in1=W["ub"][:], op0=ALU.mult, op1=ALU.add),
                             r=[("ps", p1), "negb", "ub"], w=["vn"])
                        P.op("act", lambda e, p2=p2, h=h: e.activation(out=W["o1"][:], in_=PS[p2][:64, :128], func=AF.Copy, scale=egc[:, h:h + 1]), r=[("ps", p2), "egc"], w=["o1"])
                        p3, p4 = nextps(), nextps()
                        P.op("pe", lambda e, p3=p3: e.matmul(PS[p3][:64, :128], W["aT"][:], W["vn"][:], start=True, stop=True), r=["aT", "vn"], w=[("ps", p3)])
                        P.op("pe", lambda e, p4=p4: e.matmul(PS[p4][:, :128], W["ktl"][:], W["vn"][:], start=True, stop=True), r=["ktl", "vn"], w=[("ps", p4)])
                        P.op("dve", lambda e, p3=p3, h=h, ob_=ob_: e.tensor_tensor(out=ob_[:, h * 128:(h + 1) * 128], in0=W["o1"][:], in1=PS[p3][:64, :128], op=ALU.add),
                             r=[("ps", p3), "o1"], w=[obk])
                        P.op("dve", lambda e, p4=p4, h=h, Sh=Sh: e.scalar_tensor_tensor(out=Sh, in0=Sh, scalar=egl[:, h:h + 1], in1=PS[p4][:, :128], op0=ALU.mult, op1=ALU.add),
                             r=[("ps", p4), "egl", Sk], w=[Sk])
                    P.dma("sp", "d_" + obk, oTok[d, c0:c0 + 64, :], ob_[:], r=[obk], w=[("oT", d, c0)])
    P.barrier()
    with ExitStack() as st_:
        wo = sb("o_w", [128, 8, 1024], BF16, st_)
        P.dma("pool", "d_ow", wo[:], gd_w_out[j].rearrange("(k p) n -> p k n", p=128), w=["ow"])
        of_ = sb("o_f", [128, 1024], F32, st_)
        obb = sb("o_b", [128, 1024], F32, st_)
        sqj = sb("o_sq", [128, 128], F32, st_)
        ssq = sb("o_ss", [128, 8], F32, st_)
        szt = sb("o_sz", [128, 8, 128], F32, st_)
        og = sb("o_g", [128, 8, 128], BF16, st_)
        xb = sb("o_x", [128, 8, 128], F32, st_)
        outt = list(x_tiles) + (list(c_tiles) if ctx_out else [])
        for (t0, n, mi) in outt:
            for c0 in range(t0, t0 + n, 128):
                P.dma("sp", "d_of", of_[:], oTok[0, c0:c0 + 128, :], w=["of"])
                P.dma("sp", "d_ob", obb[:], oTok[1, c0:c0 + 128, :], w=["obb"])
                P.dma("sp", "d_osz", szt[:], pj_v[:, 24:32, c0:c0 + 128], w=["szt"])
                P.dma("sp", "d_ox", xb[:], xs_v[:, :, c0:c0 + 128], r=xs_keys(c0, 128), w=["oxb"])
                P.op("dve", lambda e: e.tensor_tensor(out=of_[:], in0=of_[:], in1=obb[:], op=ALU.add), r=["of", "obb"], w=["of"])
                for h in range(8):
                    P.op("act", lambda e, h=h: e.activation(out=sqj[:], in_=of_[:, h * 128:(h + 1) * 128], func=AF.Square, accum_out=ssq[:, h:h + 1]), r=["of"], w=["sqj", ("ssq", h)])
                P.op("act", lambda e: e.activation(out=ssq[:], in_=ssq[:], func=AF.Sqrt, bias=epsc, scale=1.0 / 128.0), r=[("ssq", h) for h in range(8)] + ["consts"], w=[("ssq", h) for h in range(8)])
                P.op("dve", lambda e: e.reciprocal(out=ssq[:], in_=ssq[:]), r=[("ssq", h) for h in range(8)], w=[("ssq", h) for h in range(8)])
                for h in range(8):
                    P.op("dve", lambda e, h=h: e.tensor_scalar(out=of_[:, h * 128:(h + 1) * 128], in0=of_[:, h * 128:(h + 1) * 128], scalar1=ssq[:, h:h + 1], scalar2=None, op0=ALU.mult),
                         r=["of", ("ssq", h)], w=["of"])
                    pi = nextps()
                    P.op("pe", lambda e, h=h, pi=pi: e.transpose(PS[pi][:, :128], of_[:, h * 128:(h + 1) * 128], ident), r=["of", "consts"], w=[("ps", pi)])
                    P.op("dve", lambda e, h=h, pi=pi: e.scalar_tensor_tensor(out=og[:, h, :], in0=PS[pi][:, :128], scalar=cols[:, om + 120:om + 121], in1=szt[:, h, :], op0=ALU.mult, op1=ALU.mult),
                         r=[("ps", pi), "cols", "szt"], w=[("og", h)])
                for m in range(8):
                    pi = nextps()

                    def mm(e, pi=pi, m=m):
                        for k in range(8):
                            ins = e.matmul(PS[pi][:, :128], wo[:, k, m * 128:(m + 1) * 128], og[:, k, :], start=(k == 0), stop=(k == 7))
                        return ins
                    P.op("pe", mm, r=["ow"] + [("og", h) for h in range(8)], w=[("ps", pi)])
                    P.op("dve", lambda e, pi=pi, m=m, mi=mi: e.scalar_tensor_tensor(out=xb[:, m, :], in0=PS[pi][:, :128], scalar=mcol(2 * 8 + m, mi), in1=xb[:, m, :], op0=ALU.mult, op1=ALU.add),
                         r=[("ps", pi), "oxb", "modT"], w=["oxb"])
                P.dma("sp", "d_oxs", xs_v[:, :, c0:c0 + 128], xb[:], r=["oxb"], w=xs_keys(c0, 128))


def gmlp_layer(nc, cfg, P, sb, PS, nextps, l, j, xs_v, xs_keys, norm_mod, mcol, cols,
               gm_w_in, gm_w_out, gm_w_sT, gm_rows, x_tiles, c_tiles, ctx_out, consts):
    om = cfg.o_mix
    epsc = consts[:, 384:385]
    with ExitStack() as st_:
        wi = sb("m_wi", [128, 8, 4096], BF16, st_)
        wiv = gm_w_in[j].rearrange("(k p) n -> p k n", p=128)
        for q4 in range(4):
            P.dma("pool", "d_mwi%d" % q4, wi[:, :, q4 * 1024:(q4 + 1) * 1024], wiv[:, :, q4 * 1024:(q4 + 1) * 1024], w=[("wi", q4)])
        wik = [("wi", q4) for q4 in range(4)]
        wo = sb("m_wo", [128, 16, 1024], BF16, st_)
        P.dma("pool", "d_mwo", wo[:], gm_w_out[j].rearrange("(k p) n -> p k n", p=128), w=["wo"])
        ws = sb("m_ws", [128, 8, 128], BF16, st_)
        P.dma("pool", "d_mws", ws[:], gm_w_sT[j].rearrange("g j i -> j g i"), w=["ws"])
        rows = sb("m_rows", [128, 3 * GW + 1024], F32, st_)
        P.dma("sp", "d_mrows", rows[:], gm_rows[j, :, :], w=["rows"])
        big = sb("m_big", [128, 8, 128], F32, st_)
        sq = sb("m_sq", [128, 8, 128], F32, st_)
        stn = {"sq": sq, "rstd": sb("m_rstd", [128, 128], F32, st_), "tmp": sq}
        h16 = sb("m_h16", [128, 8, 128], BF16, st_)
        v = sb("m_v", [128, GW], F32, st_)
        vn = sb("m_vn", [128, GW], BF16, st_)
        u = sb("m_u", [128, 16, 128], F32, st_)
        gt = sb("m_gt", [128, 16, 128], BF16, st_)
        tt = [sb("m_tt%d" % i, [128, 128], F32, st_) for i in range(2)]
        st1 = sb("m_st", [128, 4], F32, st_)
        xb = sb("m_xb", [128, 8, 128], F32, st_)
        outt = list(x_tiles) + (list(c_tiles) if ctx_out else [])
        for (t0, n, mi) in outt:
            for c0 in range(t0, t0 + n, 128):
                P.dma("sp", "d_mgbig", big[:], xs_v[:, :, c0:c0 + 128], r=xs_keys(c0, 128), w=[("xt", c) for c in range(8)])
                P.dma("sp", "d_mgx", xb[:], xs_v[:, :, c0:c0 + 128], r=xs_keys(c0, 128), w=["mxb"])
                norm_mod(stn, big, 128, 0, mi, h16)
                hk = [("h16", c) for c in range(8)]
                for jc in range(16):
                    pi = nextps()

                    def mm(e, pi=pi, jc=jc):
                        for k in range(8):
                            ins = e.matmul(PS[pi][:, :128], wi[:, k, jc * 128:(jc + 1) * 128], h16[:, k, :], start=(k == 0), stop=(k == 7))
                        return ins
                    P.op("pe", mm, r=wik + hk, w=[("ps", pi)])
                    P.op("act", lambda e, pi=pi, jc=jc: e.activation(out=u[:, jc, :], in_=PS[pi][:, :128], func=AF.Gelu, bias=cols[:, om + jc:om + jc + 1], scale=1.0),
                         r=[("ps", pi), "cols"], w=[("u", jc)])
                for q4 in range(4):
                    pi = nextps()

                    def mm(e, pi=pi, q4=q4):
                        for k in range(8):
                            ins = e.matmul(PS[pi][:, :512], h16[:, k, :], wi[:, k, GW + q4 * 512:GW + (q4 + 1) * 512], start=(k == 0), stop=(k == 7))
                        return ins
                    P.op("pe", mm, r=wik + hk, w=[("ps", pi)])
                    P.op("dve", lambda e, pi=pi, q4=q4: e.tensor_tensor(out=v[:, q4 * 512:(q4 + 1) * 512], in0=PS[pi][:, :512], in1=rows[:, q4 * 512:(q4 + 1) * 512], op=ALU.add),
                         r=[("ps", pi), "rows"], w=["v"])
                P.op("act", lambda e: e.activation(out=v[:], in_=v[:], func=AF.Gelu), r=["v"], w=["v"])
                P.op("dve", lambda e: e.tensor_reduce(out=st1[:, 0:1], in_=v[:], axis=AX.X, op=ALU.add), r=["v"], w=["st1"])
                P.op("dve", lambda e: e.tensor_scalar(out=st1[:, 0:1], in0=st1[:, 0:1], scalar1=-1.0 / GW, scalar2=None, op0=ALU.mult), r=["st1"], w=["st1"])
                P.op("dve", lambda e: e.tensor_scalar(out=v[:], in0=v[:], scalar1=st1[:, 0:1], scalar2=None, op0=ALU.add), r=["v", "st1"], w=["v"])
                P.op("act", lambda e: e.activation(out=vn[:], in_=v[:], func=AF.Square, accum_out=st1[:, 1:2]), r=["v"], w=["vn", "st2"])
                P.op("act", lambda e: e.activation(out=st1[:, 1:2], in_=st1[:, 1:2], func=AF.Sqrt, bias=epsc, scale=1.0 / GW), r=["st2", "consts"], w=["st2"])
                P.op("dve", lambda e: e.reciprocal(out=st1[:, 1:2], in_=st1[:, 1:2]), r=["st2"], w=["st2"])
                P.op("dve", lambda e: e.scalar_tensor_tensor(out=v[:], in0=v[:], scalar=st1[:, 1:2], in1=rows[:, GW:2 * GW], op0=ALU.mult, op1=ALU.mult), r=["v", "st2", "rows"], w=["v"])
                P.op("dve", lambda e: e.tensor_tensor(out=vn[:], in0=v[:], in1=rows[:, 2 * GW:3 * GW], op=ALU.add), r=["v", "rows", "vn"], w=["vn"])
                for fc in range(16):
                    g = fc // 2
                    pi = nextps()
                    P.op("pe", lambda e, pi=pi, fc=fc, g=g: e.matmul(PS[pi][:, :128], vn[:, fc * 128:(fc + 1) * 128], ws[:, g, :], start=True, stop=True), r=["vn", "ws"], w=[("ps", pi)])
                    t_ = tt[fc % 2]
                    tk = "mtt%d" % (fc % 2)
                    P.op("dve", lambda e, pi=pi, g=g, t_=t_: e.tensor_tensor(out=t_[:], in0=PS[pi][:, :128], in1=rows[:, 3 * GW + g * 128:3 * GW + (g + 1) * 128], op=ALU.add),
                         r=[("ps", pi), "rows"], w=[tk])
                    P.op("pool", lambda e, fc=fc, t_=t_: e.tensor_tensor(out=gt[:, fc, :], in0=t_[:], in1=u[:, fc, :], op=ALU.mult), r=[tk, ("u", fc)], w=[("gt", fc)])
                for m in range(8):
                    pi = nextps()

                    def mm(e, pi=pi, m=m):
                        for k in range(16):
                            ins = e.matmul(PS[pi][:, :128], wo[:, k, m * 128:(m + 1) * 128], gt[:, k, :], start=(k == 0), stop=(k == 15))
                        return ins
                    P.op("pe", mm, r=["wo"] + [("gt", k) for k in range(16)], w=[("ps", pi)])
                    P.op("dve", lambda e, pi=pi, m=m, mi=mi: e.scalar_tensor_tensor(out=xb[:, m, :], in0=PS[pi][:, :128], scalar=mcol(2 * 8 + m, mi), in1=xb[:, m, :], op0=ALU.mult, op1=ALU.add),
                         r=[("ps", pi), "mxb", "modT"], w=["mxb"])
                P.dma("sp", "d_mgxs", xs_v[:, :, c0:c0 + 128], xb[:], r=["mxb"], w=xs_keys(c0, 128))


def _prep_inputs(cfg, inp, core, ncores):
    NB, T, TC, DEPTH, NE = cfg.NB, cfg.T, cfg.TC, cfg.DEPTH, cfg.NE
    f = np.float32
    b0 = core * NB
    x = inp["x"][b0:b0 + NB].reshape(NB * T, D)
    cx = inp["ctx"][b0:b0 + NB].reshape(NB * TC, D)
    cc = np.concatenate([inp["c"][b0:b0 + NB], inp["c_ctx"][None, :]], 0)
    cT = np.ascontiguousarray(cc.reshape(NB + 1, 8, 128).transpose(2, 1, 0).reshape(128, 8 * (NB + 1)))
    m = {"xT": np.ascontiguousarray(x.T), "cxT": np.ascontiguousarray(cx.T), "cT": cT}
    return m


def _colmat(v):
    v = np.asarray(v, np.float32).reshape(-1, 128)
    return v.T


def _shared_inputs(cfg, inp):
    NB, T, TC, DEPTH, NE = cfg.NB, cfg.T, cfg.TC, cfg.DEPTH, cfg.NE
    f = np.float32
    cols = np.zeros((DEPTH, 128, cfg.NCOL), f)
    for l in range(DEPTH):
        cols[l, :, cfg.o_adab:cfg.o_adab + 48] = _colmat(inp["ada_b"][l])
        cols[l, :, cfg.o_n1:cfg.o_n1 + 8] = _colmat(inp["norm1_g"][l])
        cols[l, :, cfg.o_n2:cfg.o_n2 + 8] = _colmat(inp["norm2_g"][l])
        cols[l, :, cfg.o_bgu:cfg.o_bgu + NE * 16] = _colmat(inp["moe_b_gu"][l])
    for l in range(DEPTH):
        if l % 2 == 0 and cfg.mixers[0]:
            jj = l // 2
            o = cfg.o_mix
            cw = np.asarray(inp["gdn_conv_w"][jj], f)
            for tp in range(5):
                cols[l, :, o + tp * 24:o + tp * 24 + 24] = _colmat(cw[tp])
            cols[l, :, o + 120] = np.asarray(inp["gdn_norm_g"][jj], f)
            cols[l, :16, o + 121] = np.asarray(inp["gdn_a_log"][jj], f).reshape(16)
            cols[l, :16, o + 122] = np.asarray(inp["gdn_dt_bias"][jj], f).reshape(16)
            cols[l, 16:32, o + 123] = 1.0
            cols[l, :16, o + 124] = 1.0
    for l in range(DEPTH):
        if l % 2 == 1 and cfg.mixers[1]:
            jj = l // 2
            cols[l, :, cfg.o_mix:cfg.o_mix + 16] = _colmat(np.asarray(inp["gmlp_b_in"][jj], f)[:GW])
    consts = np.zeros((128, 6 * 128), f)
    consts[:, 0:128] = np.eye(128, dtype=f)
    consts[:, 128:256] = 1.0 / 1024.0
    consts[:, 256:384] = 1.0
    consts[:, 384:512] = EPS
    jj_, ii_ = np.meshgrid(np.arange(64), np.arange(64), indexing="ij")
    consts[:64, 512:576] = (jj_ <= ii_)
    consts[:64, 576:640] = (jj_ < ii_)
    consts[:64, 640:704] = (jj_ >= ii_)
    consts[:64, 704:768] = (jj_ > ii_)
    sel = np.zeros((NE, NE * 128), f)
    for e in range(NE):
        sel[e, e * 128:(e + 1) * 128] = 1.0
    m = {
        "cols": cols, "consts": consts, "sel": sel,
        "ada_w": np.asarray(inp["ada_w"], f), "router_w": np.asarray(inp["router_w"], f),
        "router_bb": np.ascontiguousarray(np.broadcast_to(np.asarray(inp["router_b"], f)[:, None, :], (DEPTH, 128, NE))),
        "moe_w_gu": np.asarray(inp["moe_w_gu"], f), "moe_w_dn": np.asarray(inp["moe_w_dn"], f), "moe_b_dn": np.asarray(inp["moe_b_dn"], f),
        "final_g": np.ascontiguousarray(_colmat(inp["final_g"])),
    }
    if cfg.mixers[1] and DEPTH >= 2:
        nb_ = DEPTH // 2
        m["gm_w_in"] = np.asarray(inp["gmlp_w_in"], f)
        m["gm_w_out"] = np.asarray(inp["gmlp_w_out"], f)
        m["gm_w_sT"] = np.ascontiguousarray(np.asarray(inp["gmlp_w_s"], f).transpose(0, 1, 3, 2))
        rows = np.concatenate([np.asarray(inp["gmlp_b_in"], f)[:, GW:], np.asarray(inp["gmlp_ln_g"], f), np.asarray(inp["gmlp_ln_b"], f),
                               np.asarray(inp["gmlp_b_s"], f).reshape(nb_, 1024)], axis=1)
        m["gm_rows"] = np.ascontiguousarray(np.broadcast_to(rows[:, None, :], (nb_, 128, rows.shape[1])))
    if cfg.mixers[0]:
        m["gd_w_in"] = np.asarray(inp["gdn_w_in"], f)
        m["gd_w_out"] = np.asarray(inp["gdn_w_out"], f)
    return m


def run(cfg, inp, ncores, trace=False):
    nc = build(cfg)
    shared = _shared_inputs(cfg, inp)
    in_maps = []
    for c in range(ncores):
        m = dict(shared)
        m.update(_prep_inputs(cfg, inp, c, ncores))
        in_maps.append(m)
    res = run_bass_kernel_spmd(nc, in_maps, core_ids=list(range(ncores)), trace=trace)
    outs = []
    run.last = res
    for c in range(ncores):
        yT = res.results[c]["yT"]
        outs.append(np.ascontiguousarray(yT.T).reshape(cfg.NB, cfg.T, D))
    return np.concatenate(outs, 0), res


def kernel(**inputs):
    cfg = Cfg()
    out, _ = run(cfg, inputs, 8)
    return out.astype(np.float32)
```

```python
import numpy as np
from contextlib import ExitStack
import concourse.bass as bass
import concourse.mybir as mybir
from concourse.bass_utils import run_bass_kernel_spmd

F32 = mybir.dt.float32
BF16 = mybir.dt.bfloat16
AF = mybir.ActivationFunctionType
ALU = mybir.AluOpType
AX = mybir.AxisListType

D = 1024
EPS = 1e-6
H = 8
CONVK = 5
GW = 2048
LIMIT = 7.0
ALPHA = 1.702
TOPK = 4


class Cfg:
    def __init__(self, NB=2, T=4096, TC=256, DEPTH=4, NE=32, mixers=(True, True), moe=True):
        self.NB, self.T, self.TC, self.DEPTH, self.NE = NB, T, TC, DEPTH, NE
        self.mixers = mixers
        self.moe = moe
        self.NX = NB * T
        self.NTOK = NB * T + NB * TC
        self.last_ctx_reader = max(i for i in range(DEPTH) if i % 2 == 0)
        self.o_adab = 0
        self.o_n1 = 48
        self.o_n2 = 56
        self.o_bgu = 64
        self.o_mix = 64 + NE * 16
        self.NCOL = self.o_mix + 128


class Prog:
    ENG = ("pe", "act", "dve", "pool", "sp")

    def __init__(self, nc, es):
        self.nc, self.es = nc, es
        self.q = {e: [] for e in self.ENG}
        self.cnt, self.sems = {}, {}
        self.lastw, self.readers = {}, {}
        self.seen = {e: {} for e in self.ENG}
        for e in self.ENG:
            self._sem("E_" + e)

    def _sem(self, name):
        if name not in self.sems:
            self.sems[name] = self.es.enter_context(self.nc.semaphore(name))
            self.cnt[name] = 0
        return name

    def _deps(self, eng, r, w):
        waits = {}

        def add(s, v):
            if eng == "pe" and s == "E_pe":
                return
            if waits.get(s, 0) < v:
                waits[s] = v

        for k in r:
            t = self.lastw.get(k)
            if t:
                add(*t)
        for k in w:
            t = self.lastw.get(k)
            if t:
                add(*t)
            for s, v in self.readers.get(k, {}).items():
                add(s, v)
        out = []
        for s, v in waits.items():
            if self.seen[eng].get(s, 0) < v:
                self.seen[eng][s] = v
                out.append((s, v))
        return out

    def _commit(self, tok, r, w):
        for k in r:
            d = self.readers.setdefault(k, {})
            if d.get(tok[0], 0) < tok[1]:
                d[tok[0]] = tok[1]
        for k in w:
            self.lastw[k] = tok
            self.readers[k] = {}

    def op(self, eng, fn, r=(), w=()):
        waits = self._deps(eng, r, w)
        s = "E_" + eng
        self.cnt[s] += 1
        self.q[eng].append((fn, waits, s, 1))
        self._commit((s, self.cnt[s]), r, w)

    def dma(self, eng, sem, out, in_, r=(), w=()):
        self._sem(sem)
        waits = self._deps(eng, r, w)
        self.cnt[sem] += 16
        self.q[eng].append((lambda e, o=out, i=in_: e.dma_start(out=o, in_=i), waits, sem, 16))
        self._commit((sem, self.cnt[sem]), r, w)

    nobar = ()

    def barrier(self):
        for e in self.ENG:
            waits = []
            for s, v in self.cnt.items():
                if s in self.nobar:
                    continue
                if v > 0 and self.seen[e].get(s, 0) < v and not (s == "E_" + e):
                    self.seen[e][s] = v
                    waits.append((s, v))
            if waits:
                self.q[e].append((None, waits, None, 0))
        self.lastw = {k: t for k, t in self.lastw.items() if t[0] in self.nobar}
        self.readers = {}

    def emit(self):
        nc = self.nc
        names = {"pe": "tensor", "act": "scalar", "dve": "vector", "pool": "gpsimd", "sp": "sync"}
        self.barrier()
        with nc.Block() as block:
            for e in self.ENG:
                def body(eng, e=e):
                    for fn, waits, s, inc in self.q[e]:
                        for ws, wv in waits:
                            eng.wait_ge(self.sems[ws], wv)
                        if fn is not None:
                            ins = fn(eng)
                            ins.then_inc(self.sems[s], inc)
                getattr(block, names[e])(body)


def build(cfg):
    nc = bass.Bass("TRN2", target_bir_lowering=False)
    NB, T, TC, DEPTH, NE = cfg.NB, cfg.T, cfg.TC, cfg.DEPTH, cfg.NE
    NX, NTOK = cfg.NX, cfg.NTOK
    na = (DEPTH + 1) // 2
    nb_ = DEPTH // 2

    def din(name, shape, dt=F32):
        return nc.dram_tensor(name, list(shape), dt, kind="ExternalInput").ap()

    xT_in = din("xT", [D, NX])
    cxT_in = din("cxT", [D, NB * TC])
    cT_in = din("cT", [128, 8 * (NB + 1)])
    cols_in = din("cols", [DEPTH, 128, cfg.NCOL])
    consts_in = din("consts", [128, 6 * 128])
    ada_w = din("ada_w", [DEPTH, D, 6 * D])
    router_w = din("router_w", [DEPTH, D, NE])
    router_bb = din("router_bb", [DEPTH, 128, NE])
    sel_in = din("sel", [NE, NE * 128])
    moe_w_gu = din("moe_w_gu", [DEPTH, NE, D, 2 * D])
    moe_w_dn = din("moe_w_dn", [DEPTH, NE, D, D])
    moe_b_dn = din("moe_b_dn", [DEPTH, NE, D])
    final_g = din("final_g", [128, 8])
    gm_w_in = gm_w_out = gm_w_sT = gm_rows = gd_w_in = gd_w_out = None
    if nb_ > 0 and cfg.mixers[1]:
        gm_w_in = din("gm_w_in", [nb_, D, 2 * GW])
        gm_w_out = din("gm_w_out", [nb_, GW, D])
        gm_w_sT = din("gm_w_sT", [nb_, 8, 128, 128])
        gm_rows = din("gm_rows", [nb_, 128, 3 * GW + 8 * 128])
    if na > 0 and cfg.mixers[0]:
        gd_w_in = din("gd_w_in", [na, D, 4128])
        gd_w_out = din("gd_w_out", [na, D, D])
    y_out = nc.dram_tensor("yT", [D, NX], F32, kind="ExternalOutput").ap()
    cfg.dbg = None
    if getattr(cfg, "debug", False):
        cfg.dbg = (nc.dram_tensor("dbg_acc", [D, 512], F32, kind="ExternalOutput").ap(),
                   nc.dram_tensor("dbg_comb", [NE, 512], F32, kind="ExternalOutput").ap(),
                   nc.dram_tensor("dbg_h", [D, 512], BF16, kind="ExternalOutput").ap())
    xs = nc.dram_tensor("xs", [D, NTOK], F32).ap()
    gscr = None
    if na > 0 and cfg.mixers[0]:
        gscr = (nc.dram_tensor("g_pj", [4096, NTOK], F32).ap(), nc.dram_tensor("g_gb", [32, NTOK], F32).ap(),
                nc.dram_tensor("g_qk", [3072, NTOK], F32).ap(), nc.dram_tensor("g_ot", [2, NTOK, 1024], F32).ap())

    es = ExitStack()
    P = Prog(nc, es)

    uid = [0]

    def sb(name, shape, dt=F32, st=es):
        uid[0] += 1
        return st.enter_context(nc.sbuf_tensor("s%d_%s" % (uid[0], name), list(shape), dt))

    PS = [es.enter_context(nc.psum_tensor("ps%d" % i, [128, 512], F32)) for i in range(8)]
    psn = [0]

    def nextps():
        i = psn[0] % 8
        psn[0] += 1
        return i

    consts = sb("consts", [128, 6 * 128])
    ident = consts[:, 0:128]
    onesm = consts[:, 128:256]
    ones1 = consts[:, 256:384]
    epsc = consts[:, 384:385]
    P.dma("sp", "d_const", consts[:], consts_in[:, :], w=["consts"])
    cT = sb("cT", [128, 8 * (NB + 1)])
    sT = sb("sT", [128, 8 * (NB + 1)], BF16)
    P.dma("sp", "d_const", cT[:], cT_in[:, :], w=["cT"])
    P.op("act", lambda e: e.activation(out=sT[:], in_=cT[:], func=AF.Silu), r=["cT"], w=["sT"])
    cols = sb("cols", [128, cfg.NCOL])
    modT = sb("modT", [128, 48 * (NB + 1)])
    modA = sb("modA", [128, 2 * 8 * (NB + 1)])
    fing = sb("fing", [128, 8])
    P.dma("sp", "d_const", fing[:], final_g[:, :], w=["fing"])
    selt = None

    def mcol(j, b):
        return modT[:, j * (NB + 1) + b: j * (NB + 1) + b + 1]

    def acol(which, c, b):
        i = (which * 8 + c) * (NB + 1) + b
        return modA[:, i:i + 1]

    def tiles(seq_len, base, nseq, tile, midx_fn):
        out = []
        for s in range(nseq):
            for t0 in range(0, seq_len, tile):
                n = min(tile, seq_len - t0)
                out.append((base + s * seq_len + t0, n, midx_fn(s)))
        return out

    x_tiles = tiles(T, 0, NB, 512, lambda s: s)
    c_tiles = tiles(TC, NX, NB, 512, lambda s: NB)

    for (src, base, n) in ((xT_in, 0, NX), (cxT_in, NX, NB * TC)):
        for t0 in range(0, n, 2048):
            nn = min(2048, n - t0)
            P.dma("sp", "d_cp", xs[:, base + t0: base + t0 + nn], src[:, t0:t0 + nn], w=[("xs", base + t0 + i) for i in range(0, nn, 128)])

    def xs_keys(c0, n):
        return [("xs", c0 + i) for i in range(0, n, 128)]

    xs_v = xs.rearrange("(c p) t -> p c t", p=128)

    def norm_mod(st, xt, n, which, mi, h16, h32=None, tag="nm"):
        sq = st["sq"]
        xk = [("xt", c) for c in range(8)]
        P.op("act", lambda e: e.activation(out=sq[:, :, :n], in_=xt[:, :, :n], func=AF.Square), r=xk, w=["sq"] + [("tmp", c) for c in range(8)])
        pi = nextps()
        ps = PS[pi]

        def mm(e):
            for c in range(8):
                ins = e.matmul(ps[:, :n], onesm, sq[:, c, :n], start=(c == 0), stop=(c == 7))
            return ins
        P.op("pe", mm, r=["sq", "consts"], w=[("ps", pi)])
        rstd = st["rstd"]
        P.op("act", lambda e: e.activation(out=rstd[:, :n], in_=ps[:, :n], func=AF.Sqrt, bias=epsc, scale=1.0), r=[("ps", pi), "consts"], w=["rstd"])
        P.op("dve", lambda e: e.reciprocal(out=rstd[:, :n], in_=rstd[:, :n]), r=["rstd"], w=["rstd"])
        tmp = st["tmp"]
        for c in range(8):
            P.op("dve", lambda e, c=c: e.tensor_tensor(out=tmp[:, c, :n], in0=xt[:, c, :n], in1=rstd[:, :n], op=ALU.mult),
                 r=[("xt", c), "rstd", "sq"], w=[("tmp", c)])
            dst = h32 if h32 is not None else h16
            P.op("act", lambda e, c=c, dst=dst: e.activation(out=dst[:, c, :n], in_=tmp[:, c, :n], func=AF.Identity,
                                                              scale=acol(which, c, mi), bias=mcol((0 if which == 0 else 3) * 8 + c, mi)),
                 r=[("tmp", c), "modA", "modT"], w=[("xt", c)] if h32 is not None else [("h16", c)])
            if h32 is not None:
                P.op("pool", lambda e, c=c: e.tensor_copy(out=h16[:, c, :n], in_=h32[:, c, :n]), r=[("xt", c)], w=[("h16", c)])

    for l in range(DEPTH):
        j = l // 2
        ctx_in = l <= cfg.last_ctx_reader
        ctx_out = l < cfg.last_ctx_reader
        P.barrier()
        P.dma("sp", "d_cols", cols[:], cols_in[l, :, :], w=["cols"])
        with ExitStack() as st_:
            aw = [sb("aw%d" % i, [128, 8, 1024], BF16, st_) for i in range(2)]
            awv = ada_w[l].rearrange("(k p) n -> p k n", p=128)
            for g6 in range(6):
                a = aw[g6 % 2]
                ak = "aw%d" % (g6 % 2)
                P.dma("pool", "d_" + ak, a[:], awv[:, :, g6 * 1024:(g6 + 1) * 1024], w=[ak])
                for jj in range(8):
                    jg = g6 * 8 + jj
                    pi = nextps()
                    ps = PS[pi]

                    def mm(e, a=a, jj=jj, ps=ps):
                        for k in range(8):
                            ins = e.matmul(ps[:, :NB + 1], a[:, k, jj * 128:(jj + 1) * 128], sT[:, k * (NB + 1):(k + 1) * (NB + 1)],
                                           start=(k == 0), stop=(k == 7))
                        return ins
                    P.op("pe", mm, r=[ak, "sT"], w=[("ps", pi)])
                    P.op("dve", lambda e, jg=jg, ps=ps: e.tensor_scalar(out=modT[:, jg * (NB + 1):(jg + 1) * (NB + 1)], in0=ps[:, :NB + 1],
                                                                         scalar1=cols[:, cfg.o_adab + jg: cfg.o_adab + jg + 1], scalar2=None, op0=ALU.add),
                         r=[("ps", pi), "cols"], w=["modT"])
            for which, grp, og in ((0, 1, cfg.o_n1), (1, 4, cfg.o_n2)):
                for c in range(8):
                    jg = grp * 8 + c
                    i0 = (which * 8 + c) * (NB + 1)
                    P.op("dve", lambda e, jg=jg, i0=i0, og=og, c=c: e.tensor_scalar(
                        out=modA[:, i0:i0 + NB + 1], in0=modT[:, jg * (NB + 1):(jg + 1) * (NB + 1)],
                        scalar1=1.0, scalar2=cols[:, og + c: og + c + 1], op0=ALU.add, op1=ALU.mult),
                        r=["modT", "cols"], w=["modA"])
        P.barrier()

        if l % 2 == 1 and cfg.mixers[1]:
            gmlp_layer(nc, cfg, P, sb, PS, nextps, l, j, xs_v, xs_keys, norm_mod, mcol, cols,
                       gm_w_in, gm_w_out, gm_w_sT, gm_rows, x_tiles, c_tiles, ctx_out, consts)
            P.barrier()
        if l % 2 == 0 and cfg.mixers[0]:
            gdn_layer(nc, cfg, P, sb, PS, nextps, l, j, xs_v, xs_keys, norm_mod, mcol, cols, consts,
                      gd_w_in, gd_w_out, None, x_tiles, c_tiles, ctx_out, gscr)
            P.barrier()

        if cfg.moe:
            toks = list(x_tiles) + (list(c_tiles) if ctx_out else [])
            moe_layer(nc, cfg, P, sb, PS, nextps, l, xs_v, xs_keys, norm_mod, mcol, cols, consts, selt,
                      router_w, router_bb, moe_w_gu, moe_w_dn, moe_b_dn, toks)
            P.barrier()

    with ExitStack() as st_:
        xt2 = [sb("fx%d" % i, [128, 8, 512], F32, st_) for i in range(2)]
        sq = sb("fsq", [128, 8, 512], F32, st_)
        rstd = sb("frstd", [128, 512], F32, st_)
        yo = [sb("fy%d" % i, [128, 8, 512], F32, st_) for i in range(2)]
        yv = y_out.rearrange("(c p) t -> p c t", p=128)
        for ti, (c0, n, mi) in enumerate(x_tiles):
            xt = xt2[ti % 2]
            xk = "fx%d" % (ti % 2)
            yk = "fy%d" % (ti % 2)
            yt = yo[ti % 2]
            P.dma("sp", "d_" + xk, xt[:, :, :n], xs_v[:, :, c0:c0 + n], r=xs_keys(c0, n), w=[xk])
            P.op("act", lambda e, xt=xt, n=n: e.activation(out=sq[:, :, :n], in_=xt[:, :, :n], func=AF.Square), r=[xk], w=["fsq"])
            pi = nextps()
            ps = PS[pi]

            def mm(e, ps=ps, n=n):
                for c in range(8):
                    ins = e.matmul(ps[:, :n], onesm, sq[:, c, :n], start=(c == 0), stop=(c == 7))
                return ins
            P.op("pe", mm, r=["fsq", "consts"], w=[("ps", pi)])
            P.op("act", lambda e, ps=ps, n=n: e.activation(out=rstd[:, :n], in_=ps[:, :n], func=AF.Sqrt, bias=epsc, scale=1.0), r=[("ps", pi), "consts"], w=["frstd"])
            P.op("dve", lambda e, n=n: e.reciprocal(out=rstd[:, :n], in_=rstd[:, :n]), r=["frstd"], w=["frstd"])
            for c in range(8):
                P.op("dve", lambda e, c=c, xt=xt, yt=yt, n=n: e.scalar_tensor_tensor(
                    out=yt[:, c, :n], in0=xt[:, c, :n], scalar=fing[:, c:c + 1], in1=rstd[:, :n], op0=ALU.mult, op1=ALU.mult),
                    r=[xk, "frstd", "fing"], w=[yk])
            P.dma("sp", "d_" + yk, yv[:, :, c0:c0 + n], yt[:, :, :n], r=[yk], w=[("y", c0)])
    P.emit()
    es.close()
    return nc


def moe_layer(nc, cfg, P, sb, PS, nextps, l, xs_v, xs_keys, norm_mod, mcol, cols, consts, selt,
              router_w, router_bb, moe_w_gu, moe_w_dn, moe_b_dn, toks):
    NE, NB = cfg.NE, cfg.NB
    ident = consts[:, 0:128]
    GT = 1024
    groups, cur, curn = [], [], 0
    for t in toks:
        if curn + t[1] > GT:
            groups.append(cur)
            cur, curn = [], 0
        cur.append(t)
        curn += t[1]
    if cur:
        groups.append(cur)
    with ExitStack() as st_:
        h16 = sb("m_h16", [128, 8, GT], BF16, st_)
        acc = sb("m_acc", [128, 8, GT], F32, st_)
        combT = sb("m_combT", [NE, GT], F32, st_)
        wgu = [sb("m_wgu%d" % i, [128, 8, 2048], BF16, st_) for i in range(2)]
        wdn = [sb("m_wdn%d" % i, [128, 8, 1024], BF16, st_) for i in range(2)]
        NWDN = 2
        cm2 = [sb("m_cm%d" % i, [NE, 512], F32, st_) for i in range(2)]
        P.nobar = ()
        with ExitStack() as tmp_:
            big = sb("m_big", [128, 8, 512], F32, tmp_)
            sq = sb("m_sq", [128, 8, 512], F32, tmp_)
        hmid = [sb("m_hmid%d" % i, [128, 8, 512], BF16, st_) for i in range(2)]
        gg = [sb("m_g%d" % i, [128, 512], F32, st_) for i in range(2)]
        sg = [sb("m_s%d" % i, [128, 512], F32, st_) for i in range(2)]
        uu = [sb("m_u%d" % i, [128, 512], F32, st_) for i in range(2)]
        cbc = sb("m_cbc", [128, 512], F32, st_)
        stn = {"sq": sq, "rstd": sb("m_rstd", [128, 512], F32, st_), "tmp": sq}
        h32 = big
        rw = sb("m_rw", [128, 8, NE], F32, st_)
        rbb = sb("m_rbb", [128, NE], F32, st_)
        bdn = sb("m_bdn", [NE, 1024], F32, st_)
        lg = sb("m_lg", [128, 4, NE], F32, st_)
        top8 = sb("m_top8", [128, 4, 8], F32, st_)
        ex = sb("m_ex", [128, 4, NE], F32, st_)
        ssum = sb("m_ssum", [128, 4], F32, st_)
        bu1 = sb("m_bu1", [128, NE * 8], F32, st_)
        P.dma("sp", "d_mrw", rw[:], router_w[l].rearrange("(k p) n -> p k n", p=128), w=["rw"])
        P.dma("sp", "d_mrbb", rbb[:], router_bb[l, :, :], w=["rbb"])
        P.dma("sp", "d_mbdn", bdn[:], moe_b_dn[l, :, :], w=["bdn"])
        for e_ in range(NE):
            P.op("dve", lambda e, e_=e_: e.tensor_scalar(out=bu1[:, e_ * 8:(e_ + 1) * 8],
                                                         in0=cols[:, cfg.o_bgu + e_ * 16 + 8: cfg.o_bgu + e_ * 16 + 16],
                                                         scalar1=1.0, scalar2=None, op0=ALU.add), r=["cols"], w=["bu1"])
        wload = [0]

        def load_gu(e_):
            s = wload[0] % 2
            wload[0] += 1
            for half in range(2):
                P.dma("pool", "d_wgu%d" % s, wgu[s][:, half * 4:(half + 1) * 4, :],
                      moe_w_gu[l, e_].rearrange("(k p) n -> p k n", p=128)[:, half * 4:(half + 1) * 4, :], w=[("wgu", s, half)])
            return s

        dnload = [0]

        def load_dn(e_):
            sd = dnload[0] % 2
            dnload[0] += 1
            P.dma("pool", "d_wdn%d" % sd, wdn[sd][:], moe_w_dn[l, e_].rearrange("(k p) n -> p k n", p=128), w=[("wdn", sd)])
            return sd

        ones1 = consts[:, 256:384]
        pending = [None]
        for gi, grp in enumerate(groups):
            P.barrier()
            off = 0
            offs = []
            for (c0, n, mi) in grp:
                offs.append(off)
                nblk = n // 128
                P.dma("sp", "d_mbig", big[:, :, :n], xs_v[:, :, c0:c0 + n], r=xs_keys(c0, n), w=[("xt", c) for c in range(8)])
                norm_mod(stn, big, n, 1, mi, h16[:, :, off:off + n], h32=h32)
                pi = nextps()
                ps = PS[pi]

                def mm(e, ps=ps, nblk=nblk):
                    for b in range(nblk):
                        for k in range(8):
                            ins = e.matmul(ps[:, b * NE:(b + 1) * NE], h32[:, k, b * 128:(b + 1) * 128], rw[:, k, :], start=(k == 0), stop=(k == 7))
                    return ins
                P.op("pe", mm, r=[("xt", c) for c in range(8)] + ["rw"], w=[("ps", pi)])
                for b in range(nblk):
                    P.op("dve", lambda e, b=b, ps=ps: e.tensor_tensor(out=lg[:, b, :], in0=ps[:, b * NE:(b + 1) * NE], in1=rbb[:], op=ALU.add),
                         r=[("ps", pi), "rbb"], w=[("lg", b)])
                    P.op("dve", lambda e, b=b: e.max(out=top8[:, b, :], in_=lg[:, b, :]), r=[("lg", b)], w=[("top8", b)])
                    P.op("dve", lambda e, b=b: e.tensor_scalar(out=ex[:, b, :], in0=lg[:, b, :], scalar1=top8[:, b, 0:1], scalar2=None, op0=ALU.subtract),
                         r=[("lg", b), ("top8", b)], w=[("ex", b)])
                    P.op("act", lambda e, b=b: e.activation(out=ex[:, b, :], in_=ex[:, b, :], func=AF.Exp), r=[("ex", b)], w=[("ex", b)])
                    P.op("dve", lambda e, b=b: e.scalar_tensor_tensor(out=ex[:, b, :], in0=lg[:, b, :], scalar=top8[:, b, TOPK - 1:TOPK], in1=ex[:, b, :],
                                                                      op0=ALU.is_ge, op1=ALU.mult), r=[("lg", b), ("top8", b), ("ex", b)], w=[("ex", b)])
                    P.op("dve", lambda e, b=b: e.tensor_reduce(out=ssum[:, b:b + 1], in_=ex[:, b, :], axis=AX.X, op=ALU.add), r=[("ex", b)], w=[("ssum", b)])
                    P.op("dve", lambda e, b=b: e.reciprocal(out=ssum[:, b:b + 1], in_=ssum[:, b:b + 1]), r=[("ssum", b)], w=[("ssum", b)])
                    P.op("dve", lambda e, b=b: e.tensor_scalar(out=ex[:, b, :], in0=ex[:, b, :], scalar1=ssum[:, b:b + 1], scalar2=None, op0=ALU.mult),
                         r=[("ex", b), ("ssum", b)], w=[("ex", b)])
                pi2 = nextps()
                ps2 = PS[pi2]

                def tr(e, ps2=ps2, nblk=nblk):
                    for b in range(nblk):
                        ins = e.transpose(ps2[:NE, b * 128:(b + 1) * 128], ex[:, b, :], ident)
                    return ins
                P.op("pe", tr, r=[("ex", b) for b in range(nblk)] + ["consts"], w=[("ps", pi2)])
                P.op("act", lambda e, ps2=ps2, off=off, n=n: e.copy(out=combT[:, off:off + n], in_=ps2[:NE, :n]), r=[("ps", pi2)], w=[("combT", off)])
                for m in range(8):
                    pi3 = nextps()
                    ps3 = PS[pi3]
                    P.op("pe", lambda e, ps3=ps3, m=m, off=off, n=n: e.matmul(ps3[:, :n], bdn[:, m * 128:(m + 1) * 128], combT[:, off:off + n], start=True, stop=True),
                         r=["bdn", ("combT", off)], w=[("ps", pi3)])
                    P.op("act", lambda e, ps3=ps3, m=m, off=off, n=n: e.copy(out=acc[:, m, off:off + n], in_=ps3[:, :n]), r=[("ps", pi3)], w=[("acc", m, off)])
                off += n
            P.barrier()
            if pending[0] is None:
                pending[0] = (load_gu(0), load_dn(0))
            slots = {0: pending[0]}
            pending[0] = None
            units = [(e_, ti) for e_ in range(NE) for ti in range(len(grp))]

            def emit_gu(ui):
                e_, ti = units[ui]
                (c0, n, mi) = grp[ti]
                off = offs[ti]
                s = slots[e_][0]
                hm = hmid[ui % 2]
                hk = "hmid%d" % (ui % 2)
                def prep_cm(uj):
                    ee, tj = units[uj]
                    nj, oj = grp[tj][1], offs[tj]
                    cmj = cm2[uj % 2]
                    P.op("dve", lambda e: e.tensor_scalar(out=cmj[:, :nj], in0=combT[:, oj:oj + nj], scalar1=ident[:NE, ee:ee + 1], scalar2=None, op0=ALU.mult),
                         r=[("combT", oj), "consts"], w=["cm%d" % (uj % 2)])
                if ui == 0:
                    prep_cm(0)
                cm = cm2[ui % 2]
                pi = nextps()
                ps = PS[pi]
                P.op("pe", lambda e, ps=ps, n=n, cm=cm: e.matmul(ps[:, :n], ones1[:NE, :], cm[:, :n], start=True, stop=True), r=["cm%d" % (ui % 2), "consts"], w=[("ps", pi)])
                if ui + 1 < len(units):
                    prep_cm(ui + 1)
                P.op("act", lambda e, ps=ps, n=n: e.copy(out=cbc[:, :n], in_=ps[:, :n]), r=[("ps", pi)], w=["cbc"])
                for jp in range(8):
                    pg, pu = nextps(), nextps()
                    psg, psu = PS[pg], PS[pu]

                    def mmgu(e, psg=psg, psu=psu, jp=jp, s=s, off=off, n=n):
                        for k in range(8):
                            e.matmul(psg[:, :n], wgu[s][:, k, jp * 128:(jp + 1) * 128], h16[:, k, off:off + n], start=(k == 0), stop=(k == 7))
                        for k in range(8):
                            ins = e.matmul(psu[:, :n], wgu[s][:, k, 1024 + jp * 128:1024 + (jp + 1) * 128], h16[:, k, off:off + n], start=(k == 0), stop=(k == 7))
                        return ins
                    P.op("pe", mmgu, r=[("wgu", s, 0), ("wgu", s, 1)] + [("h16", c) for c in range(8)], w=[("ps", pg), ("ps", pu)])
                    g_, s_, u_ = gg[jp % 2], sg[jp % 2], uu[jp % 2]
                    gk, sk, uk = "gg%d" % (jp % 2), "sg%d" % (jp % 2), "uu%d" % (jp % 2)
                    bcol = cols[:, cfg.o_bgu + e_ * 16 + jp: cfg.o_bgu + e_ * 16 + jp + 1]
                    P.op("dve", lambda e, g_=g_, psg=psg, bcol=bcol, n=n: e.tensor_scalar(out=g_[:, :n], in0=psg[:, :n], scalar1=bcol, scalar2=LIMIT, op0=ALU.add, op1=ALU.min),
                         r=[("ps", pg), "cols"], w=[gk])
                    P.op("act", lambda e, g_=g_, s_=s_, n=n: e.activation(out=s_[:, :n], in_=g_[:, :n], func=AF.Sigmoid, scale=ALPHA), r=[gk], w=[sk])
                    P.op("dve", lambda e, u_=u_, psu=psu, e_=e_, jp=jp, n=n: e.tensor_scalar(out=u_[:, :n], in0=psu[:, :n], scalar1=bu1[:, e_ * 8 + jp: e_ * 8 + jp + 1],
                                                                                     scalar2=LIMIT + 1.0, op0=ALU.add, op1=ALU.min), r=[("ps", pu), "bu1"], w=[uk])
                    P.op("pool", lambda e, g_=g_, s_=s_, n=n: e.tensor_tensor(out=s_[:, :n], in0=g_[:, :n], in1=s_[:, :n], op=ALU.mult), r=[gk, sk], w=[sk])
                    P.op("pool", lambda e, s_=s_, n=n: e.tensor_tensor(out=s_[:, :n], in0=s_[:, :n], in1=cbc[:, :n], op=ALU.mult), r=[sk, "cbc"], w=[sk])
                    P.op("dve", lambda e, u_=u_, s_=s_, hm=hm, jp=jp, n=n: e.scalar_tensor_tensor(out=hm[:, jp, :n], in0=u_[:, :n], scalar=1.0 - LIMIT, in1=s_[:, :n],
                                                                                           op0=ALU.max, op1=ALU.mult), r=[uk, sk], w=[(hk, jp)])

            def emit_dn(ui):
                e_, ti = units[ui]
                (c0, n, mi) = grp[ti]
                off = offs[ti]
                sd = slots[e_][1]
                hm = hmid[ui % 2]
                hk = "hmid%d" % (ui % 2)
                for m in range(8):
                    pi = nextps()
                    ps = PS[pi]

                    def mmdn(e, ps=ps, m=m, sd=sd, hm=hm, n=n):
                        for k in range(8):
                            ins = e.matmul(ps[:, :n], wdn[sd][:, k, m * 128:(m + 1) * 128], hm[:, k, :n], start=(k == 0), stop=(k == 7))
                        return ins
                    P.op("pe", mmdn, r=[("wdn", sd)] + [(hk, k) for k in range(8)], w=[("ps", pi)])
                    P.op("dve", lambda e, ps=ps, m=m, off=off, n=n: e.tensor_tensor(out=acc[:, m, off:off + n], in0=acc[:, m, off:off + n], in1=ps[:, :n], op=ALU.add),
                         r=[("ps", pi), ("acc", m, off)], w=[("acc", m, off)])

            for ui in range(len(units)):
                e_, ti = units[ui]
                emit_gu(ui)
                if ui > 0:
                    emit_dn(ui - 1)
                if ti == 0:
                    if e_ + 1 < NE:
                        slots[e_ + 1] = (load_gu(e_ + 1), load_dn(e_ + 1))
            emit_dn(len(units) - 1)
            P.barrier()
            if cfg.dbg is not None and grp is groups[0]:
                P.dma("sp", "d_dbg", cfg.dbg[0].rearrange("(c p) t -> p c t", p=128), acc[:, :, 0:512], w=["dbg0"])
                P.dma("sp", "d_dbg", cfg.dbg[1][:, :], combT[:, 0:512], w=["dbg1"])
                P.dma("sp", "d_dbg", cfg.dbg[2].rearrange("(c p) t -> p c t", p=128), h16[:, :, 0:512], w=["dbg2"])
                P.barrier()
            for ti, (c0, n, mi) in enumerate(grp):
                off = offs[ti]
                P.dma("sp", "d_mbig", big[:, :, :n], xs_v[:, :, c0:c0 + n], r=xs_keys(c0, n), w=[("xt", c) for c in range(8)])
                for m in range(8):
                    P.op("dve", lambda e, m=m, off=off, n=n, mi=mi: e.scalar_tensor_tensor(out=big[:, m, :n], in0=acc[:, m, off:off + n], scalar=mcol(5 * 8 + m, mi),
                                                                                         in1=big[:, m, :n], op0=ALU.mult, op1=ALU.add),
                         r=[("acc", m, off), ("xt", m), "modT"], w=[("xt", m)])
                P.dma("sp", "d_mst", xs_v[:, :, c0:c0 + n], big[:, :, :n], r=[("xt", c) for c in range(8)], w=xs_keys(c0, n))


def gdn_layer(nc, cfg, P, sb, PS, nextps, l, j, xs_v, xs_keys, norm_mod, mcol, cols, consts,
              gd_w_in, gd_w_out, gd_rows, x_tiles, c_tiles, ctx_out, scr):
    NB, T, TC = cfg.NB, cfg.T, cfg.TC
    NX, NTOK = cfg.NX, cfg.NTOK
    ident = consts[:, 0:128]
    ones1 = consts[:, 256:384]
    epsc = consts[:, 384:385]
    om = cfg.o_mix
    pj, gb, qkvn, oTok = scr
    pj_v = pj.rearrange("(c p) t -> p c t", p=128)
    qk_v = qkvn.rearrange("(c p) t -> p c t", p=128)
    toks = list(x_tiles) + list(c_tiles)
    with ExitStack() as st_:
        w16 = sb("g_w", [128, 8, 4128], BF16, st_)
        wv = gd_w_in[j].rearrange("(k p) n -> p k n", p=128)
        for q4 in range(4):
            P.dma("pool", "d_gw%d" % q4, w16[:, :, q4 * 1032:(q4 + 1) * 1032], wv[:, :, q4 * 1032:(q4 + 1) * 1032], w=[("gw", q4)])
        gwk = [("gw", q4) for q4 in range(4)]
        big = sb("g_big", [128, 8, 512], F32, st_)
        sq = sb("g_sq", [128, 8, 512], F32, st_)
        stn = {"sq": sq, "rstd": sb("g_rstd", [128, 512], F32, st_), "tmp": sq}
        h16 = sb("g_h16", [128, 8, 512], BF16, st_)
        ob = [sb("g_ob%d" % i, [128, 512], F32, st_) for i in range(4)]
        gt = sb("g_gt", [32, 512], F32, st_)
        gt2 = sb("g_gt2", [32, 512], F32, st_)
        nega = sb("g_nega", [32, 1], F32, st_)
        P.op("act", lambda e: e.activation(out=nega[:], in_=cols[:32, om + 121:om + 122], func=AF.Exp), r=["cols"], w=["nega"])
        P.op("dve", lambda e: e.tensor_scalar(out=nega[:], in0=nega[:], scalar1=cols[:32, om + 124:om + 125], scalar2=-1.0, op0=ALU.mult, op1=ALU.mult), r=["nega", "cols"], w=["nega"])
        for (c0, n, mi) in toks:
            P.dma("sp", "d_gbig", big[:, :, :n], xs_v[:, :, c0:c0 + n], r=xs_keys(c0, n), w=[("xt", c) for c in range(8)])
            norm_mod(stn, big, n, 0, mi, h16)
            for jc in range(32):
                pi = nextps()
                ps = PS[pi]

                def mm(e, ps=ps, jc=jc, n=n):
                    for k in range(8):
                        ins = e.matmul(ps[:, :n], w16[:, k, jc * 128:(jc + 1) * 128], h16[:, k, :n], start=(k == 0), stop=(k == 7))
                    return ins
                P.op("pe", mm, r=gwk + [("h16", c) for c in range(8)], w=[("ps", pi)])
                o_ = ob[jc % 4]
                okk = "gob%d" % (jc % 4)
                if jc < 24:
                    P.op("act", lambda e, o_=o_, ps=ps, n=n: e.copy(out=o_[:, :n], in_=ps[:, :n]), r=[("ps", pi)], w=[okk])
                else:
                    P.op("act", lambda e, o_=o_, ps=ps, n=n: e.activation(out=o_[:, :n], in_=ps[:, :n], func=AF.Silu), r=[("ps", pi)], w=[okk])
                P.dma("sp", "d_" + okk, pj[jc * 128:(jc + 1) * 128, c0:c0 + n], o_[:, :n], r=[okk], w=[("pj", jc, c0)])
            pi = nextps()
            ps = PS[pi]

            def mm2(e, ps=ps, n=n):
                for k in range(8):
                    ins = e.matmul(ps[:32, :n], w16[:, k, 4096:4128], h16[:, k, :n], start=(k == 0), stop=(k == 7))
                return ins
            P.op("pe", mm2, r=gwk + [("h16", c) for c in range(8)], w=[("ps", pi)])
            P.op("act", lambda e, ps=ps, n=n: e.activation(out=gt[:, :n], in_=ps[:32, :n], func=AF.Exp, bias=cols[:32, om + 122:om + 123], scale=1.0), r=[("ps", pi), "cols"], w=["gt"])
            P.op("act", lambda e, n=n: e.activation(out=gt[:, :n], in_=gt[:, :n], func=AF.Ln, bias=ones1[:32, 0:1], scale=1.0), r=["gt", "consts"], w=["gt"])
            P.op("act", lambda e, ps=ps, n=n: e.activation(out=gt2[:, :n], in_=ps[:32, :n], func=AF.Sigmoid), r=[("ps", pi)], w=["gt2"])
            P.op("dve", lambda e, n=n: e.tensor_scalar(out=gt[:, :n], in0=gt[:, :n], scalar1=nega[:, 0:1], scalar2=None, op0=ALU.mult), r=["gt", "nega"], w=["gt"])
            P.op("dve", lambda e, n=n: e.scalar_tensor_tensor(out=gt2[:, :n], in0=gt2[:, :n], scalar=cols[:32, om + 123:om + 124], in1=gt[:, :n], op0=ALU.mult, op1=ALU.add),
                 r=["gt", "gt2", "cols"], w=["gt2"])
            P.dma("sp", "d_ggb", gb[:, c0:c0 + n], gt2[:, :n], r=["gt2"], w=[("gb", c0)])
    P.barrier()
    with ExitStack() as st_:
        buf = [sb("c_buf%d" % i, [128, 516], F32, st_) for i in range(2)]
        ac = [sb("c_ac%d" % i, [128, 512], F32, st_) for i in range(2)]
        sqq = sb("c_sq", [128, 512], F32, st_)
        rr = sb("c_rr", [128, 512], F32, st_)
        it = 0
        for (c0, n, mi) in toks:
            seq0 = (c0 // T) * T if c0 < NX else NX + ((c0 - NX) // TC) * TC
            seqn = T if c0 < NX else TC
            first = (c0 == seq0)
            last = (c0 + n == seq0 + seqn)
            for jc in range(24):
                b_ = buf[it % 2]
                bk = "cbuf%d" % (it % 2)
                a_ = ac[it % 2]
                ak = "cac%d" % (it % 2)
                it += 1
                lo = 0 if first else 2
                hi = 0 if last else 2
                if first:
                    P.op("pool", lambda e, b_=b_: e.memset(b_[:, 0:2], 0.0), w=[bk])
                if last:
                    P.op("pool", lambda e, b_=b_, n=n: e.memset(b_[:, n + 2:n + 4], 0.0), w=[bk])
                P.dma("sp", "d_" + bk, b_[:, 2 - lo:n + 2 + hi], pj[jc * 128:(jc + 1) * 128, c0 - lo:c0 + n + hi], r=[("pj", jc, c0)], w=[bk])
                for tp in range(5):
                    wc = cols[:, om + tp * 24 + jc: om + tp * 24 + jc + 1]
                    if tp == 0:
                        P.op("dve", lambda e, a_=a_, b_=b_, wc=wc, n=n: e.tensor_scalar(out=a_[:, :n], in0=b_[:, 0:n], scalar1=wc, scalar2=None, op0=ALU.mult), r=[bk, "cols"], w=[ak])
                    else:
                        P.op("dve", lambda e, a_=a_, b_=b_, wc=wc, n=n, tp=tp: e.scalar_tensor_tensor(out=a_[:, :n], in0=b_[:, tp:tp + n], scalar=wc, in1=a_[:, :n], op0=ALU.mult, op1=ALU.add),
                             r=[bk, ak, "cols"], w=[ak])
                P.op("act", lambda e, a_=a_, n=n: e.activation(out=a_[:, :n], in_=a_[:, :n], func=AF.Silu), r=[ak], w=[ak])
                if jc < 16:
                    P.op("act", lambda e, a_=a_, n=n: e.activation(out=sqq[:, :n], in_=a_[:, :n], func=AF.Square), r=[ak], w=["csq"])
                    pi = nextps()
                    ps = PS[pi]
                    P.op("pe", lambda e, ps=ps, n=n: e.matmul(ps[:, :n], ones1, sqq[:, :n], start=True, stop=True), r=["csq", "consts"], w=[("ps", pi)])
                    P.op("act", lambda e, ps=ps, n=n: e.activation(out=rr[:, :n], in_=ps[:, :n], func=AF.Sqrt, bias=epsc, scale=1.0), r=[("ps", pi), "consts"], w=["crr"])
                    P.op("dve", lambda e, n=n: e.reciprocal(out=rr[:, :n], in_=rr[:, :n]), r=["crr"], w=["crr"])
                    sc = (128.0 ** -0.5) if jc < 8 else 1.0
                    P.op("dve", lambda e, a_=a_, n=n, sc=sc: e.scalar_tensor_tensor(out=a_[:, :n], in0=a_[:, :n], scalar=sc, in1=rr[:, :n], op0=ALU.mult, op1=ALU.mult), r=[ak, "crr"], w=[ak])
                P.dma("sp", "d_" + ak, qkvn[jc * 128:(jc + 1) * 128, c0:c0 + n], a_[:, :n], r=[ak], w=[("qk", jc, c0)])
    P.barrier()
    with ExitStack() as st_:
        S = sb("s_S", [128, NB * 8 * 128], F32, st_)
        qkv = [sb("s_qkv%d" % i, [128, 24, 64], F32, st_) for i in range(2)]
        gbr = sb("s_gbr", [32, 64], F32, st_)
        gtok = sb("s_gtok", [64, 32], F32, st_)
        gc = sb("s_gc", [64, 8], F32, st_)
        egc = sb("s_egc", [64, 8], F32, st_)
        ekt = sb("s_ekt", [64, 8], F32, st_)
        egl = sb("s_egl", [128, 8], F32, st_)
        glb = sb("s_glb", [128, 8], F32, st_)
        negb = sb("s_negb", [64, 8], F32, st_)
        obuf = [sb("s_obuf%d" % i, [64, 1024], F32, st_) for i in range(2)]
        NWS = 4
        WW = [{} for _ in range(NWS)]
        for nm, shp in (("kbg", [64, 128]), ("ktl", [64, 128]), ("vt", [64, 128]), ("dg", [64, 64]), ("E", [64, 64]), ("Ei", [64, 64]), ("Es", [64, 64]),
                        ("aT", [64, 64]), ("Pa", [64, 64]), ("PaT", [64, 64]), ("Pb", [64, 64]), ("PbT", [64, 64]), ("Y", [64, 64]),
                        ("ub", [64, 128]), ("wT", [128, 64]), ("vn", [64, 128]), ("o1", [64, 128])):
            for wi_ in range(NWS):
                WW[wi_][nm] = sb("s_" + nm + str(wi_), shp, F32, st_)
        def maskI(d):
            return consts[:64, 512 + d * 128: 512 + d * 128 + 64]

        def maskS(d):
            return consts[:64, 512 + d * 128 + 64: 512 + d * 128 + 128]

        def chunk_list(s, d):
            cx = [(NX + s * TC + i * 64) for i in range(TC // 64)]
            xx = [(s * T + i * 64) for i in range(T // 64)]
            return (cx + xx) if d == 0 else (cx[::-1] + xx[::-1])

        def ev(eng, out, in_, r, w):
            if eng == "act":
                P.op("act", lambda e: e.copy(out=out, in_=in_), r=r, w=w)
            else:
                P.op("dve", lambda e: e.tensor_copy(out=out, in_=in_), r=r, w=w)

        ci = 0
        for d in range(2):
            P.op("pool", lambda e: e.memset(S[:], 0.0), w=[("S", s, h) for s in range(NB) for h in range(8)])
            for s in range(NB):
                for c0 in chunk_list(s, d):
                    qb = qkv[ci % 2]
                    qkk = "qkv%d" % (ci % 2)
                    ob_ = obuf[ci % 2]
                    obk = "obuf%d" % (ci % 2)
                    ci += 1
                    P.dma("sp", "d_" + qkk, qb[:], qk_v[:, :, c0:c0 + 64], r=[("qk", jc, (c0 // 512) * 512 if False else None) for jc in range(0)], w=[qkk])
                    P.dma("sp", "d_gbr", gbr[:], gb[:, c0:c0 + 64], w=["gbr"])
                    pi = nextps()
                    ps = PS[pi]
                    P.op("pe", lambda e, ps=ps: e.transpose(ps[:64, :32], gbr[:, :], ident[:32, :32]), r=["gbr", "consts"], w=[("ps", pi)])
                    ev("act", gtok[:], ps[:64, :32], [("ps", pi)], ["gtok"])
                    P.op("dve", lambda e, d=d: e.tensor_scalar(out=negb[:], in0=gtok[:, 16 + d * 8:24 + d * 8], scalar1=-1.0, scalar2=None, op0=ALU.mult), r=["gtok"], w=["negb"])
                    pi = nextps()
                    ps = PS[pi]
                    P.op("pe", lambda e, ps=ps, d=d: e.matmul(ps[:64, :8], maskI(d), gtok[:, d * 8:d * 8 + 8], start=True, stop=True), r=["gtok", "consts"], w=[("ps", pi)])
                    ev("dve", gc[:], ps[:64, :8], [("ps", pi)], ["gc"])
                    pi = nextps()
                    ps = PS[pi]
                    P.op("pe", lambda e, ps=ps, d=d: e.matmul(ps[:, :8], ones1[:64, :], gtok[:, d * 8:d * 8 + 8], start=True, stop=True), r=["gtok", "consts"], w=[("ps", pi)])
                    ev("dve", glb[:], ps[:, :8], [("ps", pi)], ["glb"])
                    P.op("act", lambda e: e.activation(out=egl[:], in_=glb[:], func=AF.Exp), r=["glb"], w=["egl"])
                    P.op("act", lambda e: e.activation(out=egc[:], in_=gc[:], func=AF.Exp), r=["gc"], w=["egc"])
                    P.op("dve", lambda e: e.tensor_tensor(out=ekt[:], in0=glb[:64, :], in1=gc[:], op=ALU.subtract), r=["glb", "gc"], w=["ekt"])
                    P.op("act", lambda e: e.activation(out=ekt[:], in_=ekt[:], func=AF.Exp), r=["ekt"], w=["ekt"])
                    def head_gen(h, Wp, par):
                        K = lambda nm: nm + str(par)
                        qT, kT, vT = qb[:, h, :], qb[:, 8 + h, :], qb[:, 16 + h, :]
                        Sh = S[:, (s * 8 + h) * 128:(s * 8 + h + 1) * 128]
                        Sk = ("S", s, h)
                        pk, pv = nextps(), nextps()
                        P.op("pe", lambda e, kT=kT, pk=pk: e.transpose(PS[pk][:64, :128], kT, ident), r=[qkk, "consts"], w=[("ps", pk)])
                        yield
                        P.op("pe", lambda e, vT=vT, pv=pv: e.transpose(PS[pv][:64, :128], vT, ident), r=[qkk, "consts"], w=[("ps", pv)])
                        yield
                        P.op("dve", lambda e, pk=pk, h=h: e.tensor_scalar(out=Wp["kbg"][:], in0=PS[pk][:64, :128], scalar1=egc[:, h:h + 1], scalar2=None, op0=ALU.mult), r=[("ps", pk), "egc"], w=[K("kbg")])
                        yield
                        P.op("dve", lambda e, pk=pk, h=h: e.tensor_scalar(out=Wp["ktl"][:], in0=PS[pk][:64, :128], scalar1=ekt[:, h:h + 1], scalar2=None, op0=ALU.mult), r=[("ps", pk), "ekt"], w=[K("ktl")])
                        yield
                        ev("act", Wp["vt"][:], PS[pv][:64, :128], [("ps", pv)], [K("vt")])
                        yield
                        P.op("dve", lambda e, h=h: e.tensor_scalar(out=Wp["dg"][:], in0=ident[:64, :64], scalar1=gc[:, h:h + 1], scalar2=None, op0=ALU.mult), r=["gc", "consts"], w=[K("dg")])
                        yield
                        pr = nextps()
                        P.op("pe", lambda e, pr=pr: e.matmul(PS[pr][:64, :64], ones1[:64, :64], Wp["dg"][:], start=True, stop=True), r=[K("dg"), "consts"], w=[("ps", pr)])
                        yield
                        P.op("dve", lambda e, pr=pr, h=h: e.tensor_scalar(out=Wp["E"][:], in0=PS[pr][:64, :64], scalar1=gc[:, h:h + 1], scalar2=0.0, op0=ALU.subtract, op1=ALU.min), r=[("ps", pr), "gc"], w=[K("E")])
                        yield
                        P.op("act", lambda e: e.activation(out=Wp["E"][:], in_=Wp["E"][:], func=AF.Exp), r=[K("E")], w=[K("E")])
                        yield
                        P.op("pool", lambda e, d=d: e.tensor_tensor(out=Wp["Ei"][:], in0=Wp["E"][:], in1=maskI(d), op=ALU.mult), r=[K("E"), "consts"], w=[K("Ei")])
                        yield
                        P.op("pool", lambda e, d=d: e.tensor_tensor(out=Wp["Es"][:], in0=Wp["E"][:], in1=maskS(d), op=ALU.mult), r=[K("E"), "consts"], w=[K("Es")])
                        yield
                        pkk, pqk = nextps(), nextps()
                        P.op("pe", lambda e, kT=kT, pkk=pkk: e.matmul(PS[pkk][:64, :64], kT, kT, start=True, stop=True), r=[qkk], w=[("ps", pkk)])
                        yield
                        P.op("pe", lambda e, kT=kT, qT=qT, pqk=pqk: e.matmul(PS[pqk][:64, :64], kT, qT, start=True, stop=True), r=[qkk], w=[("ps", pqk)])
                        yield
                        P.op("dve", lambda e, pkk=pkk, h=h: e.scalar_tensor_tensor(out=Wp["PaT"][:], in0=PS[pkk][:64, :64], scalar=negb[:, h:h + 1], in1=Wp["Es"][:], op0=ALU.mult, op1=ALU.mult),
                             r=[("ps", pkk), "negb", K("Es")], w=[K("PaT")])
                        yield
                        P.op("dve", lambda e, pqk=pqk: e.tensor_tensor(out=Wp["aT"][:], in0=PS[pqk][:64, :64], in1=Wp["Ei"][:], op=ALU.mult), r=[("ps", pqk), K("Ei")], w=[K("aT")])
                        yield
                        pt = nextps()
                        P.op("pe", lambda e, pt=pt: e.transpose(PS[pt][:64, :64], Wp["PaT"][:], ident[:64, :64]), r=[K("PaT"), "consts"], w=[("ps", pt)])
                        yield
                        ev("act", Wp["Pa"][:], PS[pt][:64, :64], [("ps", pt)], [K("Pa")])
                        yield
                        P.op("dve", lambda e: e.tensor_tensor(out=Wp["Y"][:], in0=Wp["PaT"][:], in1=ident[:64, :64], op=ALU.add), r=[K("PaT"), "consts"], w=[K("Y")])
                        yield
                        cur, nxt = ("Pa", "PaT"), ("Pb", "PbT")
                        for lev in range(1, 6):
                            p1 = nextps()
                            P.op("pe", lambda e, p1=p1, cur=cur: e.matmul(PS[p1][:64, :64], Wp[cur[1]][:], Wp[cur[0]][:], start=True, stop=True), r=[K(cur[0]), K(cur[1])], w=[("ps", p1)])
                            yield
                            ev("act", Wp[nxt[0]][:], PS[p1][:64, :64], [("ps", p1)], [K(nxt[0])])
                            yield
                            if lev < 5:
                                p2 = nextps()
                                P.op("pe", lambda e, p2=p2, cur=cur: e.matmul(PS[p2][:64, :64], Wp[cur[0]][:], Wp[cur[1]][:], start=True, stop=True), r=[K(cur[0]), K(cur[1])], w=[("ps", p2)])
                                yield
                                ev("dve", Wp[nxt[1]][:], PS[p2][:64, :64], [("ps", p2)], [K(nxt[1])])
                                yield
                            p3 = nextps()
                            P.op("pe", lambda e, p3=p3, nxt=nxt: e.matmul(PS[p3][:64, :64], Wp[nxt[0]][:], Wp["Y"][:], start=True, stop=True), r=[K(nxt[0]), K("Y")], w=[("ps", p3)])
                            yield
                            P.op("dve", lambda e, p3=p3: e.tensor_tensor(out=Wp["Y"][:], in0=Wp["Y"][:], in1=PS[p3][:64, :64], op=ALU.add), r=[("ps", p3), K("Y")], w=[K("Y")])
                            yield
                            cur, nxt = nxt, cur
                        pu, pw = nextps(), nextps()
                        P.op("pe", lambda e, pu=pu: e.matmul(PS[pu][:64, :128], Wp["Y"][:], Wp["vt"][:], start=True, stop=True), r=[K("Y"), K("vt")], w=[("ps", pu)])
                        yield
                        P.op("pe", lambda e, pw=pw: e.matmul(PS[pw][:, :64], Wp["kbg"][:], Wp["Y"][:], start=True, stop=True), r=[K("Y"), K("kbg")], w=[("ps", pw)])
                        yield
                        P.op("dve", lambda e, pu=pu, h=h, d=d: e.tensor_scalar(out=Wp["ub"][:], in0=PS[pu][:64, :128], scalar1=gtok[:, 16 + d * 8 + h:17 + d * 8 + h], scalar2=None, op0=ALU.mult),
                             r=[("ps", pu), "gtok"], w=[K("ub")])
                        yield
                        ev("act", Wp["wT"][:], PS[pw][:, :64], [("ps", pw)], [K("wT")])
                        yield
                        p1, p2 = nextps(), nextps()
                        P.op("pe", lambda e, p1=p1, Sh=Sh: e.matmul(PS[p1][:64, :128], Wp["wT"][:], Sh, start=True, stop=True), r=[K("wT"), Sk], w=[("ps", p1)])
                        yield
                        P.op("pe", lambda e, p2=p2, Sh=Sh, qT=qT: e.matmul(PS[p2][:64, :128], qT, Sh, start=True, stop=True), r=[qkk, Sk], w=[("ps", p2)])
                        yield
                        P.op("dve", lambda e, p1=p1, h=h: e.scalar_tensor_tensor(out=Wp["vn"][:], in0=PS[p1][:64, :128], scalar=negb[:, h:h + 1], in1=Wp["ub"][:], op0=ALU.mult, op1=ALU.add),
                             r=[("ps", p1), "negb", K("ub")], w=[K("vn")])
                        yield
                        P.op("act", lambda e, p2=p2, h=h: e.activation(out=Wp["o1"][:], in_=PS[p2][:64, :128], func=AF.Copy, scale=egc[:, h:h + 1]), r=[("ps", p2), "egc"], w=[K("o1")])
                        yield
                        p3, p4 = nextps(), nextps()
                        P.op("pe", lambda e, p3=p3: e.matmul(PS[p3][:64, :128], Wp["aT"][:], Wp["vn"][:], start=True, stop=True), r=[K("aT"), K("vn")], w=[("ps", p3)])
                        yield
                        P.op("pe", lambda e, p4=p4: e.matmul(PS[p4][:, :128], Wp["ktl"][:], Wp["vn"][:], start=True, stop=True), r=[K("ktl"), K("vn")], w=[("ps", p4)])
                        yield
                        P.op("dve", lambda e, p3=p3, h=h, ob_=ob_: e.tensor_tensor(out=ob_[:, h * 128:(h + 1) * 128], in0=Wp["o1"][:], in1=PS[p3][:64, :128], op=ALU.add),
                             r=[("ps", p3), K("o1")], w=[obk])
                        yield
                        P.op("dve", lambda e, p4=p4, h=h, Sh=Sh: e.scalar_tensor_tensor(out=Sh, in0=Sh, scalar=egl[:, h:h + 1], in1=PS[p4][:, :128], op0=ALU.mult, op1=ALU.add),
                             r=[("ps", p4), "egl", Sk], w=[Sk])
                        yield
                    for hp in range(8 // NWS):
                        alive = [head_gen(NWS * hp + w_, WW[w_], w_) for w_ in range(NWS)]
                        while alive:
                            for g_ in list(alive):
                                try:
                                    next(g_)
                                except StopIteration:
                                    alive.remove(g_)
                    P.dma("sp", "d_" + obk, oTok[d, c0:c0 + 64, :], ob_[:], r=[obk], w=[("oT", d, c0)])
    P.barrier()
    with ExitStack() as st_:
        wo = sb("o_w", [128, 8, 1024], BF16, st_)
        P.dma("pool", "d_ow", wo[:], gd_w_out[j].rearrange("(k p) n -> p k n", p=128), w=["ow"])
        of_ = sb("o_f", [128, 1024], F32, st_)
        obb = sb("o_b", [128, 1024], F32, st_)
        sqj = sb("o_sq", [128, 128], F32, st_)
        ssq = sb("o_ss", [128, 8], F32, st_)
        szt = sb("o_sz", [128, 8, 128], F32, st_)
        og = sb("o_g", [128, 8, 128], BF16, st_)
        xb = sb("o_x", [128, 8, 128], F32, st_)
        outt = list(x_tiles) + (list(c_tiles) if ctx_out else [])
        for (t0, n, mi) in outt:
            for c0 in range(t0, t0 + n, 128):
                P.dma("sp", "d_of", of_[:], oTok[0, c0:c0 + 128, :], w=["of"])
                P.dma("sp", "d_ob", obb[:], oTok[1, c0:c0 + 128, :], w=["obb"])
                P.dma("sp", "d_osz", szt[:], pj_v[:, 24:32, c0:c0 + 128], w=["szt"])
                P.dma("sp", "d_ox", xb[:], xs_v[:, :, c0:c0 + 128], r=xs_keys(c0, 128), w=["oxb"])
                P.op("dve", lambda e: e.tensor_tensor(out=of_[:], in0=of_[:], in1=obb[:], op=ALU.add), r=["of", "obb"], w=["of"])
                for h in range(8):
                    P.op("act", lambda e, h=h: e.activation(out=sqj[:], in_=of_[:, h * 128:(h + 1) * 128], func=AF.Square, accum_out=ssq[:, h:h + 1]), r=["of"], w=["sqj", ("ssq", h)])
                P.op("act", lambda e: e.activation(out=ssq[:], in_=ssq[:], func=AF.Sqrt, bias=epsc, scale=1.0 / 128.0), r=[("ssq", h) for h in range(8)] + ["consts"], w=[("ssq", h) for h in range(8)])
                P.op("dve", lambda e: e.reciprocal(out=ssq[:], in_=ssq[:]), r=[("ssq", h) for h in range(8)], w=[("ssq", h) for h in range(8)])
                for h in range(8):
                    P.op("dve", lambda e, h=h: e.tensor_scalar(out=of_[:, h * 128:(h + 1) * 128], in0=of_[:, h * 128:(h + 1) * 128], scalar1=ssq[:, h:h + 1], scalar2=None, op0=ALU.mult),
                         r=["of", ("ssq", h)], w=["of"])
                    pi = nextps()
                    P.op("pe", lambda e, h=h, pi=pi: e.transpose(PS[pi][:, :128], of_[:, h * 128:(h + 1) * 128], ident), r=["of", "consts"], w=[("ps", pi)])
                    P.op("dve", lambda e, h=h, pi=pi: e.scalar_tensor_tensor(out=og[:, h, :], in0=PS[pi][:, :128], scalar=cols[:, om + 120:om + 121], in1=szt[:, h, :], op0=ALU.mult, op1=ALU.mult),
                         r=[("ps", pi), "cols", "szt"], w=[("og", h)])
                for m in range(8):
                    pi = nextps()

                    def mm(e, pi=pi, m=m):
                        for k in range(8):
                            ins = e.matmul(PS[pi][:, :128], wo[:, k, m * 128:(m + 1) * 128], og[:, k, :], start=(k == 0), stop=(k == 7))
                        return ins
                    P.op("pe", mm, r=["ow"] + [("og", h) for h in range(8)], w=[("ps", pi)])
                    P.op("dve", lambda e, pi=pi, m=m, mi=mi: e.scalar_tensor_tensor(out=xb[:, m, :], in0=PS[pi][:, :128], scalar=mcol(2 * 8 + m, mi), in1=xb[:, m, :], op0=ALU.mult, op1=ALU.add),
                         r=[("ps", pi), "oxb", "modT"], w=["oxb"])
                P.dma("sp", "d_oxs", xs_v[:, :, c0:c0 + 128], xb[:], r=["oxb"], w=xs_keys(c0, 128))


def gmlp_layer(nc, cfg, P, sb, PS, nextps, l, j, xs_v, xs_keys, norm_mod, mcol, cols,
               gm_w_in, gm_w_out, gm_w_sT, gm_rows, x_tiles, c_tiles, ctx_out, consts):
    om = cfg.o_mix
    epsc = consts[:, 384:385]
    with ExitStack() as st_:
        wi = sb("m_wi", [128, 8, 4096], BF16, st_)
        wiv = gm_w_in[j].rearrange("(k p) n -> p k n", p=128)
        for q4 in range(4):
            P.dma("pool", "d_mwi%d" % q4, wi[:, :, q4 * 1024:(q4 + 1) * 1024], wiv[:, :, q4 * 1024:(q4 + 1) * 1024], w=[("wi", q4)])
        wik = [("wi", q4) for q4 in range(4)]
        wo = sb("m_wo", [128, 16, 1024], BF16, st_)
        P.dma("pool", "d_mwo", wo[:], gm_w_out[j].rearrange("(k p) n -> p k n", p=128), w=["wo"])
        ws = sb("m_ws", [128, 8, 128], BF16, st_)
        P.dma("pool", "d_mws", ws[:], gm_w_sT[j].rearrange("g j i -> j g i"), w=["ws"])
        rows = sb("m_rows", [128, 3 * GW + 1024], F32, st_)
        P.dma("sp", "d_mrows", rows[:], gm_rows[j, :, :], w=["rows"])
        big = sb("m_big", [128, 8, 128], F32, st_)
        sq = sb("m_sq", [128, 8, 128], F32, st_)
        stn = {"sq": sq, "rstd": sb("m_rstd", [128, 128], F32, st_), "tmp": sq}
        h16 = sb("m_h16", [128, 8, 128], BF16, st_)
        v = sb("m_v", [128, GW], F32, st_)
        vn = sb("m_vn", [128, GW], BF16, st_)
        u = sb("m_u", [128, 16, 128], F32, st_)
        gt = sb("m_gt", [128, 16, 128], BF16, st_)
        tt = [sb("m_tt%d" % i, [128, 128], F32, st_) for i in range(2)]
        st1 = sb("m_st", [128, 4], F32, st_)
        xb = sb("m_xb", [128, 8, 128], F32, st_)
        outt = list(x_tiles) + (list(c_tiles) if ctx_out else [])
        for (t0, n, mi) in outt:
            for c0 in range(t0, t0 + n, 128):
                P.dma("sp", "d_mgbig", big[:], xs_v[:, :, c0:c0 + 128], r=xs_keys(c0, 128), w=[("xt", c) for c in range(8)])
                P.dma("sp", "d_mgx", xb[:], xs_v[:, :, c0:c0 + 128], r=xs_keys(c0, 128), w=["mxb"])
                norm_mod(stn, big, 128, 0, mi, h16)
                hk = [("h16", c) for c in range(8)]
                for jc in range(16):
                    pi = nextps()

                    def mm(e, pi=pi, jc=jc):
                        for k in range(8):
                            ins = e.matmul(PS[pi][:, :128], wi[:, k, jc * 128:(jc + 1) * 128], h16[:, k, :], start=(k == 0), stop=(k == 7))
                        return ins
                    P.op("pe", mm, r=wik + hk, w=[("ps", pi)])
                    P.op("act", lambda e, pi=pi, jc=jc: e.activation(out=u[:, jc, :], in_=PS[pi][:, :128], func=AF.Gelu, bias=cols[:, om + jc:om + jc + 1], scale=1.0),
                         r=[("ps", pi), "cols"], w=[("u", jc)])
                for q4 in range(4):
                    pi = nextps()

                    def mm(e, pi=pi, q4=q4):
                        for k in range(8):
                            ins = e.matmul(PS[pi][:, :512], h16[:, k, :], wi[:, k, GW + q4 * 512:GW + (q4 + 1) * 512], start=(k == 0), stop=(k == 7))
                        return ins
                    P.op("pe", mm, r=wik + hk, w=[("ps", pi)])
                    P.op("dve", lambda e, pi=pi, q4=q4: e.tensor_tensor(out=v[:, q4 * 512:(q4 + 1) * 512], in0=PS[pi][:, :512], in1=rows[:, q4 * 512:(q4 + 1) * 512], op=ALU.add),
                         r=[("ps", pi), "rows"], w=["v"])
                P.op("act", lambda e: e.activation(out=v[:], in_=v[:], func=AF.Gelu), r=["v"], w=["v"])
                P.op("dve", lambda e: e.tensor_reduce(out=st1[:, 0:1], in_=v[:], axis=AX.X, op=ALU.add), r=["v"], w=["st1"])
                P.op("dve", lambda e: e.tensor_scalar(out=st1[:, 0:1], in0=st1[:, 0:1], scalar1=-1.0 / GW, scalar2=None, op0=ALU.mult), r=["st1"], w=["st1"])
                P.op("dve", lambda e: e.tensor_scalar(out=v[:], in0=v[:], scalar1=st1[:, 0:1], scalar2=None, op0=ALU.add), r=["v", "st1"], w=["v"])
                P.op("act", lambda e: e.activation(out=vn[:], in_=v[:], func=AF.Square, accum_out=st1[:, 1:2]), r=["v"], w=["vn", "st2"])
                P.op("act", lambda e: e.activation(out=st1[:, 1:2], in_=st1[:, 1:2], func=AF.Sqrt, bias=epsc, scale=1.0 / GW), r=["st2", "consts"], w=["st2"])
                P.op("dve", lambda e: e.reciprocal(out=st1[:, 1:2], in_=st1[:, 1:2]), r=["st2"], w=["st2"])
                P.op("dve", lambda e: e.scalar_tensor_tensor(out=v[:], in0=v[:], scalar=st1[:, 1:2], in1=rows[:, GW:2 * GW], op0=ALU.mult, op1=ALU.mult), r=["v", "st2", "rows"], w=["v"])
                P.op("dve", lambda e: e.tensor_tensor(out=vn[:], in0=v[:], in1=rows[:, 2 * GW:3 * GW], op=ALU.add), r=["v", "rows", "vn"], w=["vn"])
                for fc in range(16):
                    g = fc // 2
                    pi = nextps()
                    P.op("pe", lambda e, pi=pi, fc=fc, g=g: e.matmul(PS[pi][:, :128], vn[:, fc * 128:(fc + 1) * 128], ws[:, g, :], start=True, stop=True), r=["vn", "ws"], w=[("ps", pi)])
                    t_ = tt[fc % 2]
                    tk = "mtt%d" % (fc % 2)
                    P.op("dve", lambda e, pi=pi, g=g, t_=t_: e.tensor_tensor(out=t_[:], in0=PS[pi][:, :128], in1=rows[:, 3 * GW + g * 128:3 * GW + (g + 1) * 128], op=ALU.add),
                         r=[("ps", pi), "rows"], w=[tk])
                    P.op("pool", lambda e, fc=fc, t_=t_: e.tensor_tensor(out=gt[:, fc, :], in0=t_[:], in1=u[:, fc, :], op=ALU.mult), r=[tk, ("u", fc)], w=[("gt", fc)])
                for m in range(8):
                    pi = nextps()

                    def mm(e, pi=pi, m=m):
                        for k in range(16):
                            ins = e.matmul(PS[pi][:, :128], wo[:, k, m * 128:(m + 1) * 128], gt[:, k, :], start=(k == 0), stop=(k == 15))
                        return ins
                    P.op("pe", mm, r=["wo"] + [("gt", k) for k in range(16)], w=[("ps", pi)])
                    P.op("dve", lambda e, pi=pi, m=m, mi=mi: e.scalar_tensor_tensor(out=xb[:, m, :], in0=PS[pi][:, :128], scalar=mcol(2 * 8 + m, mi), in1=xb[:, m, :], op0=ALU.mult, op1=ALU.add),
                         r=[("ps", pi), "mxb", "modT"], w=["mxb"])
                P.dma("sp", "d_mgxs", xs_v[:, :, c0:c0 + 128], xb[:], r=["mxb"], w=xs_keys(c0, 128))


def _prep_inputs(cfg, inp, core, ncores):
    NB, T, TC, DEPTH, NE = cfg.NB, cfg.T, cfg.TC, cfg.DEPTH, cfg.NE
    f = np.float32
    b0 = core * NB
    x = inp["x"][b0:b0 + NB].reshape(NB * T, D)
    cx = inp["ctx"][b0:b0 + NB].reshape(NB * TC, D)
    cc = np.concatenate([inp["c"][b0:b0 + NB], inp["c_ctx"][None, :]], 0)
    cT = np.ascontiguousarray(cc.reshape(NB + 1, 8, 128).transpose(2, 1, 0).reshape(128, 8 * (NB + 1)))
    m = {"xT": np.ascontiguousarray(x.T), "cxT": np.ascontiguousarray(cx.T), "cT": cT}
    return m


def _colmat(v):
    v = np.asarray(v, np.float32).reshape(-1, 128)
    return v.T


def _shared_inputs(cfg, inp):
    NB, T, TC, DEPTH, NE = cfg.NB, cfg.T, cfg.TC, cfg.DEPTH, cfg.NE
    f = np.float32
    cols = np.zeros((DEPTH, 128, cfg.NCOL), f)
    for l in range(DEPTH):
        cols[l, :, cfg.o_adab:cfg.o_adab + 48] = _colmat(inp["ada_b"][l])
        cols[l, :, cfg.o_n1:cfg.o_n1 + 8] = _colmat(inp["norm1_g"][l])
        cols[l, :, cfg.o_n2:cfg.o_n2 + 8] = _colmat(inp["norm2_g"][l])
        cols[l, :, cfg.o_bgu:cfg.o_bgu + NE * 16] = _colmat(inp["moe_b_gu"][l])
    for l in range(DEPTH):
        if l % 2 == 0 and cfg.mixers[0]:
            jj = l // 2
            o = cfg.o_mix
            cw = np.asarray(inp["gdn_conv_w"][jj], f)
            for tp in range(5):
                cols[l, :, o + tp * 24:o + tp * 24 + 24] = _colmat(cw[tp])
            cols[l, :, o + 120] = np.asarray(inp["gdn_norm_g"][jj], f)
            cols[l, :16, o + 121] = np.asarray(inp["gdn_a_log"][jj], f).reshape(16)
            cols[l, :16, o + 122] = np.asarray(inp["gdn_dt_bias"][jj], f).reshape(16)
            cols[l, 16:32, o + 123] = 1.0
            cols[l, :16, o + 124] = 1.0
    for l in range(DEPTH):
        if l % 2 == 1 and cfg.mixers[1]:
            jj = l // 2
            cols[l, :, cfg.o_mix:cfg.o_mix + 16] = _colmat(np.asarray(inp["gmlp_b_in"][jj], f)[:GW])
    consts = np.zeros((128, 6 * 128), f)
    consts[:, 0:128] = np.eye(128, dtype=f)
    consts[:, 128:256] = 1.0 / 1024.0
    consts[:, 256:384] = 1.0
    consts[:, 384:512] = EPS
    jj_, ii_ = np.meshgrid(np.arange(64), np.arange(64), indexing="ij")
    consts[:64, 512:576] = (jj_ <= ii_)
    consts[:64, 576:640] = (jj_ < ii_)
    consts[:64, 640:704] = (jj_ >= ii_)
    consts[:64, 704:768] = (jj_ > ii_)
    sel = np.zeros((NE, NE * 128), f)
    for e in range(NE):
        sel[e, e * 128:(e + 1) * 128] = 1.0
    m = {
        "cols": cols, "consts": consts, "sel": sel,
        "ada_w": np.asarray(inp["ada_w"], f), "router_w": np.asarray(inp["router_w"], f),
        "router_bb": np.ascontiguousarray(np.broadcast_to(np.asarray(inp["router_b"], f)[:, None, :], (DEPTH, 128, NE))),
        "moe_w_gu": np.asarray(inp["moe_w_gu"], f), "moe_w_dn": np.asarray(inp["moe_w_dn"], f), "moe_b_dn": np.asarray(inp["moe_b_dn"], f),
        "final_g": np.ascontiguousarray(_colmat(inp["final_g"])),
    }
    if cfg.mixers[1] and DEPTH >= 2:
        nb_ = DEPTH // 2
        m["gm_w_in"] = np.asarray(inp["gmlp_w_in"], f)
        m["gm_w_out"] = np.asarray(inp["gmlp_w_out"], f)
        m["gm_w_sT"] = np.ascontiguousarray(np.asarray(inp["gmlp_w_s"], f).transpose(0, 1, 3, 2))
        rows = np.concatenate([np.asarray(inp["gmlp_b_in"], f)[:, GW:], np.asarray(inp["gmlp_ln_g"], f), np.asarray(inp["gmlp_ln_b"], f),
                               np.asarray(inp["gmlp_b_s"], f).reshape(nb_, 1024)], axis=1)
        m["gm_rows"] = np.ascontiguousarray(np.broadcast_to(rows[:, None, :], (nb_, 128, rows.shape[1])))
    if cfg.mixers[0]:
        m["gd_w_in"] = np.asarray(inp["gdn_w_in"], f)
        m["gd_w_out"] = np.asarray(inp["gdn_w_out"], f)
    return m


def run(cfg, inp, ncores, trace=False):
    nc = build(cfg)
    shared = _shared_inputs(cfg, inp)
    in_maps = []
    for c in range(ncores):
        m = dict(shared)
        m.update(_prep_inputs(cfg, inp, c, ncores))
        in_maps.append(m)
    res = run_bass_kernel_spmd(nc, in_maps, core_ids=list(range(ncores)), trace=trace)
    outs = []
    run.last = res
    for c in range(ncores):
        yT = res.results[c]["yT"]
        outs.append(np.ascontiguousarray(yT.T).reshape(cfg.NB, cfg.T, D))
    return np.concatenate(outs, 0), res


def kernel(**inputs):
    cfg = Cfg()
    out, _ = run(cfg, inputs, 8)
    return out.astype(np.float32)
```

```python
import numpy as np
from contextlib import ExitStack
import concourse.bass as bass
import concourse.mybir as mybir
from concourse.bass_utils import run_bass_kernel_spmd

F32 = mybir.dt.float32
BF16 = mybir.dt.bfloat16
AF = mybir.ActivationFunctionType
ALU = mybir.AluOpType
AX = mybir.AxisListType

D = 1024
EPS = 1e-6
H = 8
CONVK = 5
GW = 2048
LIMIT = 7.0
ALPHA = 1.702
TOPK = 4


class Cfg:
    def __init__(self, NB=2, T=4096, TC=256, DEPTH=4, NE=32, mixers=(True, True), moe=True):
        self.NB, self.T, self.TC, self.DEPTH, self.NE = NB, T, TC, DEPTH, NE
        self.mixers = mixers
        self.moe = moe
        self.NX = NB * T
        self.NTOK = NB * T + NB * TC
        self.last_ctx_reader = max(i for i in range(DEPTH) if i % 2 == 0)
        self.o_adab = 0
        self.o_n1 = 48
        self.o_n2 = 56
        self.o_bgu = 64
        self.o_mix = 64 + NE * 16
        self.NCOL = self.o_mix + 128


class Prog:
    ENG = ("pe", "act", "dve", "pool", "sp")

    def __init__(self, nc, es):
        self.nc, self.es = nc, es
        self.q = {e: [] for e in self.ENG}
        self.cnt, self.sems = {}, {}
        self.lastw, self.readers = {}, {}
        self.seen = {e: {} for e in self.ENG}
        for e in self.ENG:
            self._sem("E_" + e)

    def _sem(self, name):
        if name not in self.sems:
            self.sems[name] = self.es.enter_context(self.nc.semaphore(name))
            self.cnt[name] = 0
        return name

    def _deps(self, eng, r, w):
        waits = {}

        def add(s, v):
            if eng == "pe" and s == "E_pe":
                return
            if waits.get(s, 0) < v:
                waits[s] = v

        for k in r:
            t = self.lastw.get(k)
            if t:
                add(*t)
        for k in w:
            t = self.lastw.get(k)
            if t:
                add(*t)
            for s, v in self.readers.get(k, {}).items():
                add(s, v)
        out = []
        for s, v in waits.items():
            if self.seen[eng].get(s, 0) < v:
                self.seen[eng][s] = v
                out.append((s, v))
        return out

    def _commit(self, tok, r, w):
        for k in r:
            d = self.readers.setdefault(k, {})
            if d.get(tok[0], 0) < tok[1]:
                d[tok[0]] = tok[1]
        for k in w:
            self.lastw[k] = tok
            self.readers[k] = {}

    def op(self, eng, fn, r=(), w=()):
        waits = self._deps(eng, r, w)
        s = "E_" + eng
        self.cnt[s] += 1
        self.q[eng].append((fn, waits, s, 1))
        self._commit((s, self.cnt[s]), r, w)

    def dma(self, eng, sem, out, in_, r=(), w=()):
        self._sem(sem)
        waits = self._deps(eng, r, w)
        self.cnt[sem] += 16
        self.q[eng].append((lambda e, o=out, i=in_: e.dma_start(out=o, in_=i), waits, sem, 16))
        self._commit((sem, self.cnt[sem]), r, w)

    nobar = ()

    def barrier(self):
        for e in self.ENG:
            waits = []
            for s, v in self.cnt.items():
                if s in self.nobar:
                    continue
                if v > 0 and self.seen[e].get(s, 0) < v and not (s == "E_" + e):
                    self.seen[e][s] = v
                    waits.append((s, v))
            if waits:
                self.q[e].append((None, waits, None, 0))
        self.lastw = {k: t for k, t in self.lastw.items() if t[0] in self.nobar}
        self.readers = {}

    def emit(self):
        nc = self.nc
        names = {"pe": "tensor", "act": "scalar", "dve": "vector", "pool": "gpsimd", "sp": "sync"}
        self.barrier()
        with nc.Block() as block:
            for e in self.ENG:
                def body(eng, e=e):
                    for fn, waits, s, inc in self.q[e]:
                        for ws, wv in waits:
                            eng.wait_ge(self.sems[ws], wv)
                        if fn is not None:
                            ins = fn(eng)
                            ins.then_inc(self.sems[s], inc)
                getattr(block, names[e])(body)


def build(cfg):
    nc = bass.Bass("TRN2", target_bir_lowering=False)
    NB, T, TC, DEPTH, NE = cfg.NB, cfg.T, cfg.TC, cfg.DEPTH, cfg.NE
    NX, NTOK = cfg.NX, cfg.NTOK
    na = (DEPTH + 1) // 2
    nb_ = DEPTH // 2

    def din(name, shape, dt=F32):
        return nc.dram_tensor(name, list(shape), dt, kind="ExternalInput").ap()

    xT_in = din("xT", [D, NX])
    cxT_in = din("cxT", [D, NB * TC])
    cT_in = din("cT", [128, 8 * (NB + 1)])
    cols_in = din("cols", [DEPTH, 128, cfg.NCOL])
    consts_in = din("consts", [128, 6 * 128])
    ada_w = din("ada_w", [DEPTH, D, 6 * D])
    router_w = din("router_w", [DEPTH, D, NE])
    router_bb = din("router_bb", [DEPTH, 128, NE])
    sel_in = din("sel", [NE, NE * 128])
    moe_w_gu = din("moe_w_gu", [DEPTH, NE, D, 2 * D])
    moe_w_dn = din("moe_w_dn", [DEPTH, NE, D, D])
    moe_b_dn = din("moe_b_dn", [DEPTH, NE, D])
    final_g = din("final_g", [128, 8])
    gm_w_in = gm_w_out = gm_w_sT = gm_rows = gd_w_in = gd_w_out = None
    if nb_ > 0 and cfg.mixers[1]:
        gm_w_in = din("gm_w_in", [nb_, D, 2 * GW])
        gm_w_out = din("gm_w_out", [nb_, GW, D])
        gm_w_sT = din("gm_w_sT", [nb_, 8, 128, 128])
        gm_rows = din("gm_rows", [nb_, 128, 3 * GW + 8 * 128])
    if na > 0 and cfg.mixers[0]:
        gd_w_in = din("gd_w_in", [na, D, 4128])
        gd_w_out = din("gd_w_out", [na, D, D])
    y_out = nc.dram_tensor("yT", [D, NX], F32, kind="ExternalOutput").ap()
    cfg.dbg = None
    if getattr(cfg, "debug", False):
        cfg.dbg = (nc.dram_tensor("dbg_acc", [D, 512], F32, kind="ExternalOutput").ap(),
                   nc.dram_tensor("dbg_comb", [NE, 512], F32, kind="ExternalOutput").ap(),
                   nc.dram_tensor("dbg_h", [D, 512], BF16, kind="ExternalOutput").ap())
    xs = nc.dram_tensor("xs", [D, NTOK], F32).ap()
    gscr = None
    if na > 0 and cfg.mixers[0]:
        gscr = (nc.dram_tensor("g_pj", [4096, NTOK], F32).ap(), nc.dram_tensor("g_gb", [32, NTOK], F32).ap(),
                nc.dram_tensor("g_qk", [3072, NTOK], F32).ap(), nc.dram_tensor("g_ot", [2, NTOK, 1024], F32).ap())

    es = ExitStack()
    P = Prog(nc, es)

    uid = [0]

    def sb(name, shape, dt=F32, st=es):
        uid[0] += 1
        return st.enter_context(nc.sbuf_tensor("s%d_%s" % (uid[0], name), list(shape), dt))

    PS = [es.enter_context(nc.psum_tensor("ps%d" % i, [128, 512], F32)) for i in range(8)]
    psn = [0]

    def nextps():
        i = psn[0] % 8
        psn[0] += 1
        return i

    consts = sb("consts", [128, 6 * 128])
    ident = consts[:, 0:128]
    onesm = consts[:, 128:256]
    ones1 = consts[:, 256:384]
    epsc = consts[:, 384:385]
    P.dma("sp", "d_const", consts[:], consts_in[:, :], w=["consts"])
    cT = sb("cT", [128, 8 * (NB + 1)])
    sT = sb("sT", [128, 8 * (NB + 1)], BF16)
    P.dma("sp", "d_const", cT[:], cT_in[:, :], w=["cT"])
    P.op("act", lambda e: e.activation(out=sT[:], in_=cT[:], func=AF.Silu), r=["cT"], w=["sT"])
    cols = sb("cols", [128, cfg.NCOL])
    modT = sb("modT", [128, 48 * (NB + 1)])
    modA = sb("modA", [128, 2 * 8 * (NB + 1)])
    fing = sb("fing", [128, 8])
    P.dma("sp", "d_const", fing[:], final_g[:, :], w=["fing"])
    selt = None

    def mcol(j, b):
        return modT[:, j * (NB + 1) + b: j * (NB + 1) + b + 1]

    def acol(which, c, b):
        i = (which * 8 + c) * (NB + 1) + b
        return modA[:, i:i + 1]

    def tiles(seq_len, base, nseq, tile, midx_fn):
        out = []
        for s in range(nseq):
            for t0 in range(0, seq_len, tile):
                n = min(tile, seq_len - t0)
                out.append((base + s * seq_len + t0, n, midx_fn(s)))
        return out

    x_tiles = tiles(T, 0, NB, 512, lambda s: s)
    c_tiles = tiles(TC, NX, NB, 512, lambda s: NB)

    for (src, base, n) in ((xT_in, 0, NX), (cxT_in, NX, NB * TC)):
        for t0 in range(0, n, 2048):
            nn = min(2048, n - t0)
            P.dma("sp", "d_cp", xs[:, base + t0: base + t0 + nn], src[:, t0:t0 + nn], w=[("xs", base + t0 + i) for i in range(0, nn, 128)])

    def xs_keys(c0, n):
        return [("xs", c0 + i) for i in range(0, n, 128)]

    xs_v = xs.rearrange("(c p) t -> p c t", p=128)

    def norm_mod(st, xt, n, which, mi, h16, h32=None, tag="nm"):
        sq = st["sq"]
        xk = [("xt", c) for c in range(8)]
        P.op("act", lambda e: e.activation(out=sq[:, :, :n], in_=xt[:, :, :n], func=AF.Square), r=xk, w=["sq"] + [("tmp", c) for c in range(8)])
        pi = nextps()
        ps = PS[pi]

        def mm(e):
            for c in range(8):
                ins = e.matmul(ps[:, :n], onesm, sq[:, c, :n], start=(c == 0), stop=(c == 7))
            return ins
        P.op("pe", mm, r=["sq", "consts"], w=[("ps", pi)])
        rstd = st["rstd"]
        P.op("act", lambda e: e.activation(out=rstd[:, :n], in_=ps[:, :n], func=AF.Sqrt, bias=epsc, scale=1.0), r=[("ps", pi), "consts"], w=["rstd"])
        P.op("dve", lambda e: e.reciprocal(out=rstd[:, :n], in_=rstd[:, :n]), r=["rstd"], w=["rstd"])
        tmp = st["tmp"]
        for c in range(8):
            P.op("dve", lambda e, c=c: e.tensor_tensor(out=tmp[:, c, :n], in0=xt[:, c, :n], in1=rstd[:, :n], op=ALU.mult),
                 r=[("xt", c), "rstd", "sq"], w=[("tmp", c)])
            dst = h32 if h32 is not None else h16
            P.op("act", lambda e, c=c, dst=dst: e.activation(out=dst[:, c, :n], in_=tmp[:, c, :n], func=AF.Identity,
                                                              scale=acol(which, c, mi), bias=mcol((0 if which == 0 else 3) * 8 + c, mi)),
                 r=[("tmp", c), "modA", "modT"], w=[("xt", c)] if h32 is not None else [("h16", c)])
            if h32 is not None:
                P.op("pool", lambda e, c=c: e.tensor_copy(out=h16[:, c, :n], in_=h32[:, c, :n]), r=[("xt", c)], w=[("h16", c)])

    for l in range(DEPTH):
        j = l // 2
        ctx_in = l <= cfg.last_ctx_reader
        ctx_out = l < cfg.last_ctx_reader
        P.barrier()
        P.dma("sp", "d_cols", cols[:], cols_in[l, :, :], w=["cols"])
        with ExitStack() as st_:
            aw = [sb("aw%d" % i, [128, 8, 1024], BF16, st_) for i in range(2)]
            awv = ada_w[l].rearrange("(k p) n -> p k n", p=128)
            for g6 in range(6):
                a = aw[g6 % 2]
                ak = "aw%d" % (g6 % 2)
                P.dma("pool", "d_" + ak, a[:], awv[:, :, g6 * 1024:(g6 + 1) * 1024], w=[ak])
                for jj in range(8):
                    jg = g6 * 8 + jj
                    pi = nextps()
                    ps = PS[pi]

                    def mm(e, a=a, jj=jj, ps=ps):
                        for k in range(8):
                            ins = e.matmul(ps[:, :NB + 1], a[:, k, jj * 128:(jj + 1) * 128], sT[:, k * (NB + 1):(k + 1) * (NB + 1)],
                                           start=(k == 0), stop=(k == 7))
                        return ins
                    P.op("pe", mm, r=[ak, "sT"], w=[("ps", pi)])
                    P.op("dve", lambda e, jg=jg, ps=ps: e.tensor_scalar(out=modT[:, jg * (NB + 1):(jg + 1) * (NB + 1)], in0=ps[:, :NB + 1],
                                                                         scalar1=cols[:, cfg.o_adab + jg: cfg.o_adab + jg + 1], scalar2=None, op0=ALU.add),
                         r=[("ps", pi), "cols"], w=["modT"])
            for which, grp, og in ((0, 1, cfg.o_n1), (1, 4, cfg.o_n2)):
                for c in range(8):
                    jg = grp * 8 + c
                    i0 = (which * 8 + c) * (NB + 1)
                    P.op("dve", lambda e, jg=jg, i0=i0, og=og, c=c: e.tensor_scalar(
                        out=modA[:, i0:i0 + NB + 1], in0=modT[:, jg * (NB + 1):(jg + 1) * (NB + 1)],
                        scalar1=1.0, scalar2=cols[:, og + c: og + c + 1], op0=ALU.add, op1=ALU.mult),
                        r=["modT", "cols"], w=["modA"])
        P.barrier()

        if l % 2 == 1 and cfg.mixers[1]:
            gmlp_layer(nc, cfg, P, sb, PS, nextps, l, j, xs_v, xs_keys, norm_mod, mcol, cols,
                       gm_w_in, gm_w_out, gm_w_sT, gm_rows, x_tiles, c_tiles, ctx_out, consts)
            P.barrier()
        if l % 2 == 0 and cfg.mixers[0]:
            gdn_layer(nc, cfg, P, sb, PS, nextps, l, j, xs_v, xs_keys, norm_mod, mcol, cols, consts,
                      gd_w_in, gd_w_out, None, x_tiles, c_tiles, ctx_out, gscr)
            P.barrier()

        if cfg.moe:
            toks = list(x_tiles) + (list(c_tiles) if ctx_out else [])
            moe_layer(nc, cfg, P, sb, PS, nextps, l, xs_v, xs_keys, norm_mod, mcol, cols, consts, selt,
                      router_w, router_bb, moe_w_gu, moe_w_dn, moe_b_dn, toks)
            P.barrier()

    with ExitStack() as st_:
        xt2 = [sb("fx%d" % i, [128, 8, 512], F32, st_) for i in range(2)]
        sq = sb("fsq", [128, 8, 512], F32, st_)
        rstd = sb("frstd", [128, 512], F32, st_)
        yo = [sb("fy%d" % i, [128, 8, 512], F32, st_) for i in range(2)]
        yv = y_out.rearrange("(c p) t -> p c t", p=128)
        for ti, (c0, n, mi) in enumerate(x_tiles):
            xt = xt2[ti % 2]
            xk = "fx%d" % (ti % 2)
            yk = "fy%d" % (ti % 2)
            yt = yo[ti % 2]
            P.dma("sp", "d_" + xk, xt[:, :, :n], xs_v[:, :, c0:c0 + n], r=xs_keys(c0, n), w=[xk])
            P.op("act", lambda e, xt=xt, n=n: e.activation(out=sq[:, :, :n], in_=xt[:, :, :n], func=AF.Square), r=[xk], w=["fsq"])
            pi = nextps()
            ps = PS[pi]

            def mm(e, ps=ps, n=n):
                for c in range(8):
                    ins = e.matmul(ps[:, :n], onesm, sq[:, c, :n], start=(c == 0), stop=(c == 7))
                return ins
            P.op("pe", mm, r=["fsq", "consts"], w=[("ps", pi)])
            P.op("act", lambda e, ps=ps, n=n: e.activation(out=rstd[:, :n], in_=ps[:, :n], func=AF.Sqrt, bias=epsc, scale=1.0), r=[("ps", pi), "consts"], w=["frstd"])
            P.op("dve", lambda e, n=n: e.reciprocal(out=rstd[:, :n], in_=rstd[:, :n]), r=["frstd"], w=["frstd"])
            for c in range(8):
                P.op("dve", lambda e, c=c, xt=xt, yt=yt, n=n: e.scalar_tensor_tensor(
                    out=yt[:, c, :n], in0=xt[:, c, :n], scalar=fing[:, c:c + 1], in1=rstd[:, :n], op0=ALU.mult, op1=ALU.mult),
                    r=[xk, "frstd", "fing"], w=[yk])
            P.dma("sp", "d_" + yk, yv[:, :, c0:c0 + n], yt[:, :, :n], r=[yk], w=[("y", c0)])
    P.emit()
    es.close()
    return nc


def moe_layer(nc, cfg, P, sb, PS, nextps, l, xs_v, xs_keys, norm_mod, mcol, cols, consts, selt,
              router_w, router_bb, moe_w_gu, moe_w_dn, moe_b_dn, toks):
    NE, NB = cfg.NE, cfg.NB
    ident = consts[:, 0:128]
    GT = 1024
    groups, cur, curn = [], [], 0
    for t in toks:
        if curn + t[1] > GT:
            groups.append(cur)
            cur, curn = [], 0
        cur.append(t)
        curn += t[1]
    if cur:
        groups.append(cur)
    with ExitStack() as st_:
        h16 = sb("m_h16", [128, 8, GT], BF16, st_)
        acc = sb("m_acc", [128, 8, GT], F32, st_)
        combT = sb("m_combT", [NE, GT], F32, st_)
        wgu = [sb("m_wgu%d" % i, [128, 8, 2048], BF16, st_) for i in range(2)]
        wdn = [sb("m_wdn%d" % i, [128, 8, 1024], BF16, st_) for i in range(2)]
        NWDN = 2
        cm2 = [sb("m_cm%d" % i, [NE, 512], F32, st_) for i in range(2)]
        P.nobar = ()
        with ExitStack() as tmp_:
            big = sb("m_big", [128, 8, 512], F32, tmp_)
            sq = sb("m_sq", [128, 8, 512], F32, tmp_)
        hmid = [sb("m_hmid%d" % i, [128, 8, 512], BF16, st_) for i in range(2)]
        gg = [sb("m_g%d" % i, [128, 512], F32, st_) for i in range(2)]
        sg = [sb("m_s%d" % i, [128, 512], F32, st_) for i in range(2)]
        uu = [sb("m_u%d" % i, [128, 512], F32, st_) for i in range(2)]
        cbc = sb("m_cbc", [128, 512], F32, st_)
        stn = {"sq": sq, "rstd": sb("m_rstd", [128, 512], F32, st_), "tmp": sq}
        h32 = big
        rw = sb("m_rw", [128, 8, NE], F32, st_)
        rbb = sb("m_rbb", [128, NE], F32, st_)
        bdn = sb("m_bdn", [NE, 1024], F32, st_)
        lg = sb("m_lg", [128, 4, NE], F32, st_)
        top8 = sb("m_top8", [128, 4, 8], F32, st_)
        ex = sb("m_ex", [128, 4, NE], F32, st_)
        ssum = sb("m_ssum", [128, 4], F32, st_)
        bu1 = sb("m_bu1", [128, NE * 8], F32, st_)
        P.dma("sp", "d_mrw", rw[:], router_w[l].rearrange("(k p) n -> p k n", p=128), w=["rw"])
        P.dma("sp", "d_mrbb", rbb[:], router_bb[l, :, :], w=["rbb"])
        P.dma("sp", "d_mbdn", bdn[:], moe_b_dn[l, :, :], w=["bdn"])
        for e_ in range(NE):
            P.op("dve", lambda e, e_=e_: e.tensor_scalar(out=bu1[:, e_ * 8:(e_ + 1) * 8],
                                                         in0=cols[:, cfg.o_bgu + e_ * 16 + 8: cfg.o_bgu + e_ * 16 + 16],
                                                         scalar1=1.0, scalar2=None, op0=ALU.add), r=["cols"], w=["bu1"])
        wload = [0]

        def load_gu(e_):
            s = wload[0] % 2
            wload[0] += 1
            for half in range(2):
                P.dma("pool", "d_wgu%d" % s, wgu[s][:, half * 4:(half + 1) * 4, :],
                      moe_w_gu[l, e_].rearrange("(k p) n -> p k n", p=128)[:, half * 4:(half + 1) * 4, :], w=[("wgu", s, half)])
            return s

        dnload = [0]

        def load_dn(e_):
            sd = dnload[0] % 2
            dnload[0] += 1
            P.dma("pool", "d_wdn%d" % sd, wdn[sd][:], moe_w_dn[l, e_].rearrange("(k p) n -> p k n", p=128), w=[("wdn", sd)])
            return sd

        ones1 = consts[:, 256:384]
        pending = [None]
        for gi, grp in enumerate(groups):
            P.barrier()
            pending[0] = (load_gu(0), load_dn(0))
            off = 0
            offs = []
            for (c0, n, mi) in grp:
                offs.append(off)
                nblk = n // 128
                P.dma("sp", "d_mbig", big[:, :, :n], xs_v[:, :, c0:c0 + n], r=xs_keys(c0, n), w=[("xt", c) for c in range(8)])
                norm_mod(stn, big, n, 1, mi, h16[:, :, off:off + n], h32=h32)
                pi = nextps()
                ps = PS[pi]

                def mm(e, ps=ps, nblk=nblk):
                    for b in range(nblk):
                        for k in range(8):
                            ins = e.matmul(ps[:, b * NE:(b + 1) * NE], h32[:, k, b * 128:(b + 1) * 128], rw[:, k, :], start=(k == 0), stop=(k == 7))
                    return ins
                P.op("pe", mm, r=[("xt", c) for c in range(8)] + ["rw"], w=[("ps", pi)])
                for b in range(nblk):
                    P.op("dve", lambda e, b=b, ps=ps: e.tensor_tensor(out=lg[:, b, :], in0=ps[:, b * NE:(b + 1) * NE], in1=rbb[:], op=ALU.add),
                         r=[("ps", pi), "rbb"], w=[("lg", b)])
                    P.op("dve", lambda e, b=b: e.max(out=top8[:, b, :], in_=lg[:, b, :]), r=[("lg", b)], w=[("top8", b)])
                    P.op("dve", lambda e, b=b: e.tensor_scalar(out=ex[:, b, :], in0=lg[:, b, :], scalar1=top8[:, b, 0:1], scalar2=None, op0=ALU.subtract),
                         r=[("lg", b), ("top8", b)], w=[("ex", b)])
                    P.op("act", lambda e, b=b: e.activation(out=ex[:, b, :], in_=ex[:, b, :], func=AF.Exp), r=[("ex", b)], w=[("ex", b)])
                    P.op("dve", lambda e, b=b: e.scalar_tensor_tensor(out=ex[:, b, :], in0=lg[:, b, :], scalar=top8[:, b, TOPK - 1:TOPK], in1=ex[:, b, :],
                                                                      op0=ALU.is_ge, op1=ALU.mult), r=[("lg", b), ("top8", b), ("ex", b)], w=[("ex", b)])
                    P.op("dve", lambda e, b=b: e.tensor_reduce(out=ssum[:, b:b + 1], in_=ex[:, b, :], axis=AX.X, op=ALU.add), r=[("ex", b)], w=[("ssum", b)])
                    P.op("dve", lambda e, b=b: e.reciprocal(out=ssum[:, b:b + 1], in_=ssum[:, b:b + 1]), r=[("ssum", b)], w=[("ssum", b)])
                    P.op("dve", lambda e, b=b: e.tensor_scalar(out=ex[:, b, :], in0=ex[:, b, :], scalar1=ssum[:, b:b + 1], scalar2=None, op0=ALU.mult),
                         r=[("ex", b), ("ssum", b)], w=[("ex", b)])
                pi2 = nextps()
                ps2 = PS[pi2]

                def tr(e, ps2=ps2, nblk=nblk):
                    for b in range(nblk):
                        ins = e.transpose(ps2[:NE, b * 128:(b + 1) * 128], ex[:, b, :], ident)
                    return ins
                P.op("pe", tr, r=[("ex", b) for b in range(nblk)] + ["consts"], w=[("ps", pi2)])
                P.op("act", lambda e, ps2=ps2, off=off, n=n: e.copy(out=combT[:, off:off + n], in_=ps2[:NE, :n]), r=[("ps", pi2)], w=[("combT", off)])
                for m in range(8):
                    pi3 = nextps()
                    ps3 = PS[pi3]
                    P.op("pe", lambda e, ps3=ps3, m=m, off=off, n=n: e.matmul(ps3[:, :n], bdn[:, m * 128:(m + 1) * 128], combT[:, off:off + n], start=True, stop=True),
                         r=["bdn", ("combT", off)], w=[("ps", pi3)])
                    P.op("act", lambda e, ps3=ps3, m=m, off=off, n=n: e.copy(out=acc[:, m, off:off + n], in_=ps3[:, :n]), r=[("ps", pi3)], w=[("acc", m, off)])
                off += n
            P.barrier()
            if pending[0] is None:
                pending[0] = (load_gu(0), load_dn(0))
            slots = {0: pending[0]}
            pending[0] = None
            units = [(e_, ti) for e_ in range(NE) for ti in range(len(grp))]

            def emit_gu(ui):
                e_, ti = units[ui]
                (c0, n, mi) = grp[ti]
                off = offs[ti]
                s = slots[e_][0]
                hm = hmid[ui % 2]
                hk = "hmid%d" % (ui % 2)
                def prep_cm(uj):
                    ee, tj = units[uj]
                    nj, oj = grp[tj][1], offs[tj]
                    cmj = cm2[uj % 2]
                    P.op("dve", lambda e: e.tensor_scalar(out=cmj[:, :nj], in0=combT[:, oj:oj + nj], scalar1=ident[:NE, ee:ee + 1], scalar2=None, op0=ALU.mult),
                         r=[("combT", oj), "consts"], w=["cm%d" % (uj % 2)])
                if ui == 0:
                    prep_cm(0)
                cm = cm2[ui % 2]
                pi = nextps()
                ps = PS[pi]
                P.op("pe", lambda e, ps=ps, n=n, cm=cm: e.matmul(ps[:, :n], ones1[:NE, :], cm[:, :n], start=True, stop=True), r=["cm%d" % (ui % 2), "consts"], w=[("ps", pi)])
                if ui + 1 < len(units):
                    prep_cm(ui + 1)
                P.op("act", lambda e, ps=ps, n=n: e.copy(out=cbc[:, :n], in_=ps[:, :n]), r=[("ps", pi)], w=["cbc"])
                for jp in range(8):
                    pg, pu = nextps(), nextps()
                    psg, psu = PS[pg], PS[pu]

                    def mmgu(e, psg=psg, psu=psu, jp=jp, s=s, off=off, n=n):
                        for k in range(8):
                            e.matmul(psg[:, :n], wgu[s][:, k, jp * 128:(jp + 1) * 128], h16[:, k, off:off + n], start=(k == 0), stop=(k == 7))
                        for k in range(8):
                            ins = e.matmul(psu[:, :n], wgu[s][:, k, 1024 + jp * 128:1024 + (jp + 1) * 128], h16[:, k, off:off + n], start=(k == 0), stop=(k == 7))
                        return ins
                    P.op("pe", mmgu, r=[("wgu", s, 0), ("wgu", s, 1)] + [("h16", c) for c in range(8)], w=[("ps", pg), ("ps", pu)])
                    g_, s_, u_ = gg[jp % 2], sg[jp % 2], uu[jp % 2]
                    gk, sk, uk = "gg%d" % (jp % 2), "sg%d" % (jp % 2), "uu%d" % (jp % 2)
                    bcol = cols[:, cfg.o_bgu + e_ * 16 + jp: cfg.o_bgu + e_ * 16 + jp + 1]
                    P.op("dve", lambda e, g_=g_, psg=psg, bcol=bcol, n=n: e.tensor_scalar(out=g_[:, :n], in0=psg[:, :n], scalar1=bcol, scalar2=LIMIT, op0=ALU.add, op1=ALU.min),
                         r=[("ps", pg), "cols"], w=[gk])
                    P.op("act", lambda e, g_=g_, s_=s_, n=n: e.activation(out=s_[:, :n], in_=g_[:, :n], func=AF.Sigmoid, scale=ALPHA), r=[gk], w=[sk])
                    P.op("dve", lambda e, u_=u_, psu=psu, e_=e_, jp=jp, n=n: e.tensor_scalar(out=u_[:, :n], in0=psu[:, :n], scalar1=bu1[:, e_ * 8 + jp: e_ * 8 + jp + 1],
                                                                                     scalar2=LIMIT + 1.0, op0=ALU.add, op1=ALU.min), r=[("ps", pu), "bu1"], w=[uk])
                    P.op("pool", lambda e, g_=g_, s_=s_, n=n: e.tensor_tensor(out=s_[:, :n], in0=g_[:, :n], in1=s_[:, :n], op=ALU.mult), r=[gk, sk], w=[sk])
                    P.op("pool", lambda e, s_=s_, n=n: e.tensor_tensor(out=s_[:, :n], in0=s_[:, :n], in1=cbc[:, :n], op=ALU.mult), r=[sk, "cbc"], w=[sk])
                    P.op("dve", lambda e, u_=u_, s_=s_, hm=hm, jp=jp, n=n: e.scalar_tensor_tensor(out=hm[:, jp, :n], in0=u_[:, :n], scalar=1.0 - LIMIT, in1=s_[:, :n],
                                                                                           op0=ALU.max, op1=ALU.mult), r=[uk, sk], w=[(hk, jp)])

            def emit_dn(ui):
                e_, ti = units[ui]
                (c0, n, mi) = grp[ti]
                off = offs[ti]
                sd = slots[e_][1]
                hm = hmid[ui % 2]
                hk = "hmid%d" % (ui % 2)
                for m in range(8):
                    pi = nextps()
                    ps = PS[pi]

                    def mmdn(e, ps=ps, m=m, sd=sd, hm=hm, n=n):
                        for k in range(8):
                            ins = e.matmul(ps[:, :n], wdn[sd][:, k, m * 128:(m + 1) * 128], hm[:, k, :n], start=(k == 0), stop=(k == 7))
                        return ins
                    P.op("pe", mmdn, r=[("wdn", sd)] + [(hk, k) for k in range(8)], w=[("ps", pi)])
                    P.op("dve", lambda e, ps=ps, m=m, off=off, n=n: e.tensor_tensor(out=acc[:, m, off:off + n], in0=acc[:, m, off:off + n], in1=ps[:, :n], op=ALU.add),
                         r=[("ps", pi), ("acc", m, off)], w=[("acc", m, off)])

            for ui in range(len(units)):
                e_, ti = units[ui]
                emit_gu(ui)
                if ui > 0:
                    emit_dn(ui - 1)
                if ti == 0:
                    if e_ + 1 < NE:
                        slots[e_ + 1] = (load_gu(e_ + 1), load_dn(e_ + 1))
            emit_dn(len(units) - 1)
            P.barrier()
            if cfg.dbg is not None and grp is groups[0]:
                P.dma("sp", "d_dbg", cfg.dbg[0].rearrange("(c p) t -> p c t", p=128), acc[:, :, 0:512], w=["dbg0"])
                P.dma("sp", "d_dbg", cfg.dbg[1][:, :], combT[:, 0:512], w=["dbg1"])
                P.dma("sp", "d_dbg", cfg.dbg[2].rearrange("(c p) t -> p c t", p=128), h16[:, :, 0:512], w=["dbg2"])
                P.barrier()
            for ti, (c0, n, mi) in enumerate(grp):
                off = offs[ti]
                P.dma("sp", "d_mbig", big[:, :, :n], xs_v[:, :, c0:c0 + n], r=xs_keys(c0, n), w=[("xt", c) for c in range(8)])
                for m in range(8):
                    P.op("dve", lambda e, m=m, off=off, n=n, mi=mi: e.scalar_tensor_tensor(out=big[:, m, :n], in0=acc[:, m, off:off + n], scalar=mcol(5 * 8 + m, mi),
                                                                                         in1=big[:, m, :n], op0=ALU.mult, op1=ALU.add),
                         r=[("acc", m, off), ("xt", m), "modT"], w=[("xt", m)])
                P.dma("sp", "d_mst", xs_v[:, :, c0:c0 + n], big[:, :, :n], r=[("xt", c) for c in range(8)], w=xs_keys(c0, n))


def gdn_layer(nc, cfg, P, sb, PS, nextps, l, j, xs_v, xs_keys, norm_mod, mcol, cols, consts,
              gd_w_in, gd_w_out, gd_rows, x_tiles, c_tiles, ctx_out, scr):
    NB, T, TC = cfg.NB, cfg.T, cfg.TC
    NX, NTOK = cfg.NX, cfg.NTOK
    ident = consts[:, 0:128]
    ones1 = consts[:, 256:384]
    epsc = consts[:, 384:385]
    om = cfg.o_mix
    pj, gb, qkvn, oTok = scr
    pj_v = pj.rearrange("(c p) t -> p c t", p=128)
    qk_v = qkvn.rearrange("(c p) t -> p c t", p=128)
    toks = list(x_tiles) + list(c_tiles)
    with ExitStack() as st_:
        w16 = sb("g_w", [128, 8, 4128], BF16, st_)
        wv = gd_w_in[j].rearrange("(k p) n -> p k n", p=128)
        for q4 in range(4):
            P.dma("pool", "d_gw%d" % q4, w16[:, :, q4 * 1032:(q4 + 1) * 1032], wv[:, :, q4 * 1032:(q4 + 1) * 1032], w=[("gw", q4)])
        gwk = [("gw", q4) for q4 in range(4)]
        big = sb("g_big", [128, 8, 512], F32, st_)
        sq = sb("g_sq", [128, 8, 512], F32, st_)
        stn = {"sq": sq, "rstd": sb("g_rstd", [128, 512], F32, st_), "tmp": sq}
        h16 = sb("g_h16", [128, 8, 512], BF16, st_)
        ob = [sb("g_ob%d" % i, [128, 512], F32, st_) for i in range(4)]
        gt = sb("g_gt", [32, 512], F32, st_)
        gt2 = sb("g_gt2", [32, 512], F32, st_)
        nega = sb("g_nega", [32, 1], F32, st_)
        P.op("act", lambda e: e.activation(out=nega[:], in_=cols[:32, om + 121:om + 122], func=AF.Exp), r=["cols"], w=["nega"])
        P.op("dve", lambda e: e.tensor_scalar(out=nega[:], in0=nega[:], scalar1=cols[:32, om + 124:om + 125], scalar2=-1.0, op0=ALU.mult, op1=ALU.mult), r=["nega", "cols"], w=["nega"])
        for (c0, n, mi) in toks:
            P.dma("sp", "d_gbig", big[:, :, :n], xs_v[:, :, c0:c0 + n], r=xs_keys(c0, n), w=[("xt", c) for c in range(8)])
            norm_mod(stn, big, n, 0, mi, h16)
            for jc in range(32):
                pi = nextps()
                ps = PS[pi]

                def mm(e, ps=ps, jc=jc, n=n):
                    for k in range(8):
                        ins = e.matmul(ps[:, :n], w16[:, k, jc * 128:(jc + 1) * 128], h16[:, k, :n], start=(k == 0), stop=(k == 7))
                    return ins
                P.op("pe", mm, r=gwk + [("h16", c) for c in range(8)], w=[("ps", pi)])
                o_ = ob[jc % 4]
                okk = "gob%d" % (jc % 4)
                if jc < 24:
                    P.op("act", lambda e, o_=o_, ps=ps, n=n: e.copy(out=o_[:, :n], in_=ps[:, :n]), r=[("ps", pi)], w=[okk])
                else:
                    P.op("act", lambda e, o_=o_, ps=ps, n=n: e.activation(out=o_[:, :n], in_=ps[:, :n], func=AF.Silu), r=[("ps", pi)], w=[okk])
                P.dma("sp", "d_" + okk, pj[jc * 128:(jc + 1) * 128, c0:c0 + n], o_[:, :n], r=[okk], w=[("pj", jc, c0)])
            pi = nextps()
            ps = PS[pi]

            def mm2(e, ps=ps, n=n):
                for k in range(8):
                    ins = e.matmul(ps[:32, :n], w16[:, k, 4096:4128], h16[:, k, :n], start=(k == 0), stop=(k == 7))
                return ins
            P.op("pe", mm2, r=gwk + [("h16", c) for c in range(8)], w=[("ps", pi)])
            P.op("act", lambda e, ps=ps, n=n: e.activation(out=gt[:, :n], in_=ps[:32, :n], func=AF.Exp, bias=cols[:32, om + 122:om + 123], scale=1.0), r=[("ps", pi), "cols"], w=["gt"])
            P.op("act", lambda e, n=n: e.activation(out=gt[:, :n], in_=gt[:, :n], func=AF.Ln, bias=ones1[:32, 0:1], scale=1.0), r=["gt", "consts"], w=["gt"])
            P.op("act", lambda e, ps=ps, n=n: e.activation(out=gt2[:, :n], in_=ps[:32, :n], func=AF.Sigmoid), r=[("ps", pi)], w=["gt2"])
            P.op("dve", lambda e, n=n: e.tensor_scalar(out=gt[:, :n], in0=gt[:, :n], scalar1=nega[:, 0:1], scalar2=None, op0=ALU.mult), r=["gt", "nega"], w=["gt"])
            P.op("dve", lambda e, n=n: e.scalar_tensor_tensor(out=gt2[:, :n], in0=gt2[:, :n], scalar=cols[:32, om + 123:om + 124], in1=gt[:, :n], op0=ALU.mult, op1=ALU.add),
                 r=["gt", "gt2", "cols"], w=["gt2"])
            P.dma("sp", "d_ggb", gb[:, c0:c0 + n], gt2[:, :n], r=["gt2"], w=[("gb", c0)])
    P.barrier()
    with ExitStack() as st_:
        buf = [sb("c_buf%d" % i, [128, 516], F32, st_) for i in range(2)]
        ac = [sb("c_ac%d" % i, [128, 512], F32, st_) for i in range(2)]
        sqq = sb("c_sq", [128, 512], F32, st_)
        rr = sb("c_rr", [128, 512], F32, st_)
        it = 0
        for (c0, n, mi) in toks:
            seq0 = (c0 // T) * T if c0 < NX else NX + ((c0 - NX) // TC) * TC
            seqn = T if c0 < NX else TC
            first = (c0 == seq0)
            last = (c0 + n == seq0 + seqn)
            for jc in range(24):
                b_ = buf[it % 2]
                bk = "cbuf%d" % (it % 2)
                a_ = ac[it % 2]
                ak = "cac%d" % (it % 2)
                it += 1
                lo = 0 if first else 2
                hi = 0 if last else 2
                if first:
                    P.op("pool", lambda e, b_=b_: e.memset(b_[:, 0:2], 0.0), w=[bk])
                if last:
                    P.op("pool", lambda e, b_=b_, n=n: e.memset(b_[:, n + 2:n + 4], 0.0), w=[bk])
                P.dma("sp", "d_" + bk, b_[:, 2 - lo:n + 2 + hi], pj[jc * 128:(jc + 1) * 128, c0 - lo:c0 + n + hi], r=[("pj", jc, c0)], w=[bk])
                for tp in range(5):
                    wc = cols[:, om + tp * 24 + jc: om + tp * 24 + jc + 1]
                    if tp == 0:
                        P.op("dve", lambda e, a_=a_, b_=b_, wc=wc, n=n: e.tensor_scalar(out=a_[:, :n], in0=b_[:, 0:n], scalar1=wc, scalar2=None, op0=ALU.mult), r=[bk, "cols"], w=[ak])
                    else:
                        P.op("dve", lambda e, a_=a_, b_=b_, wc=wc, n=n, tp=tp: e.scalar_tensor_tensor(out=a_[:, :n], in0=b_[:, tp:tp + n], scalar=wc, in1=a_[:, :n], op0=ALU.mult, op1=ALU.add),
                             r=[bk, ak, "cols"], w=[ak])
                P.op("act", lambda e, a_=a_, n=n: e.activation(out=a_[:, :n], in_=a_[:, :n], func=AF.Silu), r=[ak], w=[ak])
                if jc < 16:
                    P.op("act", lambda e, a_=a_, n=n: e.activation(out=sqq[:, :n], in_=a_[:, :n], func=AF.Square), r=[ak], w=["csq"])
                    pi = nextps()
                    ps = PS[pi]
                    P.op("pe", lambda e, ps=ps, n=n: e.matmul(ps[:, :n], ones1, sqq[:, :n], start=True, stop=True), r=["csq", "consts"], w=[("ps", pi)])
                    P.op("act", lambda e, ps=ps, n=n: e.activation(out=rr[:, :n], in_=ps[:, :n], func=AF.Sqrt, bias=epsc, scale=1.0), r=[("ps", pi), "consts"], w=["crr"])
                    P.op("dve", lambda e, n=n: e.reciprocal(out=rr[:, :n], in_=rr[:, :n]), r=["crr"], w=["crr"])
                    sc = (128.0 ** -0.5) if jc < 8 else 1.0
                    P.op("dve", lambda e, a_=a_, n=n, sc=sc: e.scalar_tensor_tensor(out=a_[:, :n], in0=a_[:, :n], scalar=sc, in1=rr[:, :n], op0=ALU.mult, op1=ALU.mult), r=[ak, "crr"], w=[ak])
                P.dma("sp", "d_" + ak, qkvn[jc * 128:(jc + 1) * 128, c0:c0 + n], a_[:, :n], r=[ak], w=[("qk", jc, c0)])
    P.barrier()
    with ExitStack() as st_:
        S = sb("s_S", [128, NB * 8 * 128], F32, st_)
        qkv = [sb("s_qkv%d" % i, [128, 24, 64], F32, st_) for i in range(2)]
        gbr = sb("s_gbr", [32, 64], F32, st_)
        gtok = sb("s_gtok", [64, 32], F32, st_)
        gc = sb("s_gc", [64, 8], F32, st_)
        egc = sb("s_egc", [64, 8], F32, st_)
        ekt = sb("s_ekt", [64, 8], F32, st_)
        egl = sb("s_egl", [128, 8], F32, st_)
        glb = sb("s_glb", [128, 8], F32, st_)
        negb = sb("s_negb", [64, 8], F32, st_)
        obuf = [sb("s_obuf%d" % i, [64, 1024], F32, st_) for i in range(2)]
        NWS = 4
        WW = [{} for _ in range(NWS)]
        for nm, shp in (("kbg", [64, 128]), ("ktl", [64, 128]), ("vt", [64, 128]), ("dg", [64, 64]), ("E", [64, 64]), ("Ei", [64, 64]), ("Es", [64, 64]),
                        ("aT", [64, 64]), ("Pa", [64, 64]), ("PaT", [64, 64]), ("Pb", [64, 64]), ("PbT", [64, 64]), ("Y", [64, 64]),
                        ("ub", [64, 128]), ("wT", [128, 64]), ("vn", [64, 128]), ("o1", [64, 128])):
            for wi_ in range(NWS):
                WW[wi_][nm] = sb("s_" + nm + str(wi_), shp, F32, st_)
        def maskI(d):
            return consts[:64, 512 + d * 128: 512 + d * 128 + 64]

        def maskS(d):
            return consts[:64, 512 + d * 128 + 64: 512 + d * 128 + 128]

        def chunk_list(s, d):
            cx = [(NX + s * TC + i * 64) for i in range(TC // 64)]
            xx = [(s * T + i * 64) for i in range(T // 64)]
            return (cx + xx) if d == 0 else (cx[::-1] + xx[::-1])

        def ev(eng, out, in_, r, w):
            if eng == "act":
                P.op("act", lambda e: e.copy(out=out, in_=in_), r=r, w=w)
            else:
                P.op("dve", lambda e: e.tensor_copy(out=out, in_=in_), r=r, w=w)

        ci = 0
        for d in range(2):
            P.op("pool", lambda e: e.memset(S[:], 0.0), w=[("S", s, h) for s in range(NB) for h in range(8)])
            for s in range(NB):
                for c0 in chunk_list(s, d):
                    qb = qkv[ci % 2]
                    qkk = "qkv%d" % (ci % 2)
                    ob_ = obuf[ci % 2]
                    obk = "obuf%d" % (ci % 2)
                    ci += 1
                    P.dma("sp", "d_" + qkk, qb[:], qk_v[:, :, c0:c0 + 64], r=[("qk", jc, (c0 // 512) * 512 if False else None) for jc in range(0)], w=[qkk])
                    P.dma("sp", "d_gbr", gbr[:], gb[:, c0:c0 + 64], w=["gbr"])
                    pi = nextps()
                    ps = PS[pi]
                    P.op("pe", lambda e, ps=ps: e.transpose(ps[:64, :32], gbr[:, :], ident[:32, :32]), r=["gbr", "consts"], w=[("ps", pi)])
                    ev("act", gtok[:], ps[:64, :32], [("ps", pi)], ["gtok"])
                    P.op("dve", lambda e, d=d: e.tensor_scalar(out=negb[:], in0=gtok[:, 16 + d * 8:24 + d * 8], scalar1=-1.0, scalar2=None, op0=ALU.mult), r=["gtok"], w=["negb"])
                    pi = nextps()
                    ps = PS[pi]
                    P.op("pe", lambda e, ps=ps, d=d: e.matmul(ps[:64, :8], maskI(d), gtok[:, d * 8:d * 8 + 8], start=True, stop=True), r=["gtok", "consts"], w=[("ps", pi)])
                    ev("dve", gc[:], ps[:64, :8], [("ps", pi)], ["gc"])
                    pi = nextps()
                    ps = PS[pi]
                    P.op("pe", lambda e, ps=ps, d=d: e.matmul(ps[:, :8], ones1[:64, :], gtok[:, d * 8:d * 8 + 8], start=True, stop=True), r=["gtok", "consts"], w=[("ps", pi)])
                    ev("dve", glb[:], ps[:, :8], [("ps", pi)], ["glb"])
                    P.op("act", lambda e: e.activation(out=egl[:], in_=glb[:], func=AF.Exp), r=["glb"], w=["egl"])
                    P.op("act", lambda e: e.activation(out=egc[:], in_=gc[:], func=AF.Exp), r=["gc"], w=["egc"])
                    P.op("dve", lambda e: e.tensor_tensor(out=ekt[:], in0=glb[:64, :], in1=gc[:], op=ALU.subtract), r=["glb", "gc"], w=["ekt"])
                    P.op("act", lambda e: e.activation(out=ekt[:], in_=ekt[:], func=AF.Exp), r=["ekt"], w=["ekt"])
                    def head_gen(h, Wp, par):
                        K = lambda nm: nm + str(par)
                        qT, kT, vT = qb[:, h, :], qb[:, 8 + h, :], qb[:, 16 + h, :]
                        Sh = S[:, (s * 8 + h) * 128:(s * 8 + h + 1) * 128]
                        Sk = ("S", s, h)
                        pk, pv = nextps(), nextps()
                        P.op("pe", lambda e, kT=kT, pk=pk: e.transpose(PS[pk][:64, :128], kT, ident), r=[qkk, "consts"], w=[("ps", pk)])
                        yield
                        P.op("pe", lambda e, vT=vT, pv=pv: e.transpose(PS[pv][:64, :128], vT, ident), r=[qkk, "consts"], w=[("ps", pv)])
                        yield
                        P.op("dve", lambda e, pk=pk, h=h: e.tensor_scalar(out=Wp["kbg"][:], in0=PS[pk][:64, :128], scalar1=egc[:, h:h + 1], scalar2=None, op0=ALU.mult), r=[("ps", pk), "egc"], w=[K("kbg")])
                        yield
                        P.op("dve", lambda e, pk=pk, h=h: e.tensor_scalar(out=Wp["ktl"][:], in0=PS[pk][:64, :128], scalar1=ekt[:, h:h + 1], scalar2=None, op0=ALU.mult), r=[("ps", pk), "ekt"], w=[K("ktl")])
                        yield
                        ev("act", Wp["vt"][:], PS[pv][:64, :128], [("ps", pv)], [K("vt")])
                        yield
                        P.op("dve", lambda e, h=h: e.tensor_scalar(out=Wp["dg"][:], in0=ident[:64, :64], scalar1=gc[:, h:h + 1], scalar2=None, op0=ALU.mult), r=["gc", "consts"], w=[K("dg")])
                        yield
                        pr = nextps()
                        P.op("pe", lambda e, pr=pr: e.matmul(PS[pr][:64, :64], ones1[:64, :64], Wp["dg"][:], start=True, stop=True), r=[K("dg"), "consts"], w=[("ps", pr)])
                        yield
                        P.op("dve", lambda e, pr=pr, h=h: e.tensor_scalar(out=Wp["E"][:], in0=PS[pr][:64, :64], scalar1=gc[:, h:h + 1], scalar2=0.0, op0=ALU.subtract, op1=ALU.min), r=[("ps", pr), "gc"], w=[K("E")])
                        yield
                        P.op("act", lambda e: e.activation(out=Wp["E"][:], in_=Wp["E"][:], func=AF.Exp), r=[K("E")], w=[K("E")])
                        yield
                        P.op("pool", lambda e, d=d: e.tensor_tensor(out=Wp["Ei"][:], in0=Wp["E"][:], in1=maskI(d), op=ALU.mult), r=[K("E"), "consts"], w=[K("Ei")])
                        yield
                        P.op("pool", lambda e, d=d: e.tensor_tensor(out=Wp["Es"][:], in0=Wp["E"][:], in1=maskS(d), op=ALU.mult), r=[K("E"), "consts"], w=[K("Es")])
                        yield
                        pkk, pqk = nextps(), nextps()
                        P.op("pe", lambda e, kT=kT, pkk=pkk: e.matmul(PS[pkk][:64, :64], kT, kT, start=True, stop=True), r=[qkk], w=[("ps", pkk)])
                        yield
                        P.op("pe", lambda e, kT=kT, qT=qT, pqk=pqk: e.matmul(PS[pqk][:64, :64], kT, qT, start=True, stop=True), r=[qkk], w=[("ps", pqk)])
                        yield
                        P.op("dve", lambda e, pkk=pkk, h=h: e.scalar_tensor_tensor(out=Wp["PaT"][:], in0=PS[pkk][:64, :64], scalar=negb[:, h:h + 1], in1=Wp["Es"][:], op0=ALU.mult, op1=ALU.mult),
                             r=[("ps", pkk), "negb", K("Es")], w=[K("PaT")])
                        yield
                        P.op("dve", lambda e, pqk=pqk: e.tensor_tensor(out=Wp["aT"][:], in0=PS[pqk][:64, :64], in1=Wp["Ei"][:], op=ALU.mult), r=[("ps", pqk), K("Ei")], w=[K("aT")])
                        yield
                        pt = nextps()
                        P.op("pe", lambda e, pt=pt: e.transpose(PS[pt][:64, :64], Wp["PaT"][:], ident[:64, :64]), r=[K("PaT"), "consts"], w=[("ps", pt)])
                        yield
                        ev("act", Wp["Pa"][:], PS[pt][:64, :64], [("ps", pt)], [K("Pa")])
                        yield
                        P.op("dve", lambda e: e.tensor_tensor(out=Wp["Y"][:], in0=Wp["PaT"][:], in1=ident[:64, :64], op=ALU.add), r=[K("PaT"), "consts"], w=[K("Y")])
                        yield
                        cur, nxt = ("Pa", "PaT"), ("Pb", "PbT")
                        for lev in range(1, 6):
                            p1 = nextps()
                            P.op("pe", lambda e, p1=p1, cur=cur: e.matmul(PS[p1][:64, :64], Wp[cur[1]][:], Wp[cur[0]][:], start=True, stop=True), r=[K(cur[0]), K(cur[1])], w=[("ps", p1)])
                            yield
                            ev("act", Wp[nxt[0]][:], PS[p1][:64, :64], [("ps", p1)], [K(nxt[0])])
                            yield
                            if lev < 5:
                                p2 = nextps()
                                P.op("pe", lambda e, p2=p2, cur=cur: e.matmul(PS[p2][:64, :64], Wp[cur[0]][:], Wp[cur[1]][:], start=True, stop=True), r=[K(cur[0]), K(cur[1])], w=[("ps", p2)])
                                yield
                                ev("dve", Wp[nxt[1]][:], PS[p2][:64, :64], [("ps", p2)], [K(nxt[1])])
                                yield
                            p3 = nextps()
                            P.op("pe", lambda e, p3=p3, nxt=nxt: e.matmul(PS[p3][:64, :64], Wp[nxt[0]][:], Wp["Y"][:], start=True, stop=True), r=[K(nxt[0]), K("Y")], w=[("ps", p3)])
                            yield
                            P.op("dve", lambda e, p3=p3: e.tensor_tensor(out=Wp["Y"][:], in0=Wp["Y"][:], in1=PS[p3][:64, :64], op=ALU.add), r=[("ps", p3), K("Y")], w=[K("Y")])
                            yield
                            cur, nxt = nxt, cur
                        pu, pw = nextps(), nextps()
                        P.op("pe", lambda e, pu=pu: e.matmul(PS[pu][:64, :128], Wp["Y"][:], Wp["vt"][:], start=True, stop=True), r=[K("Y"), K("vt")], w=[("ps", pu)])
                        yield
                        P.op("pe", lambda e, pw=pw: e.matmul(PS[pw][:, :64], Wp["kbg"][:], Wp["Y"][:], start=True, stop=True), r=[K("Y"), K("kbg")], w=[("ps", pw)])
                        yield
                        P.op("dve", lambda e, pu=pu, h=h, d=d: e.tensor_scalar(out=Wp["ub"][:], in0=PS[pu][:64, :128], scalar1=gtok[:, 16 + d * 8 + h:17 + d * 8 + h], scalar2=None, op0=ALU.mult),
                             r=[("ps", pu), "gtok"], w=[K("ub")])
                        yield
                        ev("act", Wp["wT"][:], PS[pw][:, :64], [("ps", pw)], [K("wT")])
                        yield
                        p1, p2 = nextps(), nextps()
                        P.op("pe", lambda e, p1=p1, Sh=Sh: e.matmul(PS[p1][:64, :128], Wp["wT"][:], Sh, start=True, stop=True), r=[K("wT"), Sk], w=[("ps", p1)])
                        yield
                        P.op("pe", lambda e, p2=p2, Sh=Sh, qT=qT: e.matmul(PS[p2][:64, :128], qT, Sh, start=True, stop=True), r=[qkk, Sk], w=[("ps", p2)])
                        yield
                        P.op("dve", lambda e, p1=p1, h=h: e.scalar_tensor_tensor(out=Wp["vn"][:], in0=PS[p1][:64, :128], scalar=negb[:, h:h + 1], in1=Wp["ub"][:], op0=ALU.mult, op1=ALU.add),
                             r=[("ps", p1), "negb", K("ub")], w=[K("vn")])
                        yield
                        P.op("act", lambda e, p2=p2, h=h: e.activation(out=Wp["o1"][:], in_=PS[p2][:64, :128], func=AF.Copy, scale=egc[:, h:h + 1]), r=[("ps", p2), "egc"], w=[K("o1")])
                        yield
                        p3, p4 = nextps(), nextps()
                        P.op("pe", lambda e, p3=p3: e.matmul(PS[p3][:64, :128], Wp["aT"][:], Wp["vn"][:], start=True, stop=True), r=[K("aT"), K("vn")], w=[("ps", p3)])
                        yield
                        P.op("pe", lambda e, p4=p4: e.matmul(PS[p4][:, :128], Wp["ktl"][:], Wp["vn"][:], start=True, stop=True), r=[K("ktl"), K("vn")], w=[("ps", p4)])
                        yield
                        P.op("dve", lambda e, p3=p3, h=h, ob_=ob_: e.tensor_tensor(out=ob_[:, h * 128:(h + 1) * 128], in0=Wp["o1"][:], in1=PS[p3][:64, :128], op=ALU.add),
                             r=[("ps", p3), K("o1")], w=[obk])
                        yield
                        P.op("dve", lambda e, p4=p4, h=h, Sh=Sh: e.scalar_tensor_tensor(out=Sh, in0=Sh, scalar=egl[:, h:h + 1], in1=PS[p4][:, :128], op0=ALU.mult, op1=ALU.add),
                             r=[("ps", p4), "egl", Sk], w=[Sk])
                        yield
                    for hp in range(8 // NWS):
                        alive = [head_gen(NWS * hp + w_, WW[w_], w_) for w_ in range(NWS)]
                        while alive:
                            for g_ in list(alive):
                                try:
                                    next(g_)
                                except StopIteration:
                                    alive.remove(g_)
                    P.dma("sp", "d_" + obk, oTok[d, c0:c0 + 64, :], ob_[:], r=[obk], w=[("oT", d, c0)])
    P.barrier()
    with ExitStack() as st_:
        wo = sb("o_w", [128, 8, 1024], BF16, st_)
        P.dma("pool", "d_ow", wo[:], gd_w_out[j].rearrange("(k p) n -> p k n", p=128), w=["ow"])
        of_ = sb("o_f", [128, 1024], F32, st_)
        obb = sb("o_b", [128, 1024], F32, st_)
        sqj = sb("o_sq", [128, 128], F32, st_)
        ssq = sb("o_ss", [128, 8], F32, st_)
        szt = sb("o_sz", [128, 8, 128], F32, st_)
        og = sb("o_g", [128, 8, 128], BF16, st_)
        xb = sb("o_x", [128, 8, 128], F32, st_)
        outt = list(x_tiles) + (list(c_tiles) if ctx_out else [])
        for (t0, n, mi) in outt:
            for c0 in range(t0, t0 + n, 128):
                P.dma("sp", "d_of", of_[:], oTok[0, c0:c0 + 128, :], w=["of"])
                P.dma("sp", "d_ob", obb[:], oTok[1, c0:c0 + 128, :], w=["obb"])
                P.dma("sp", "d_osz", szt[:], pj_v[:, 24:32, c0:c0 + 128], w=["szt"])
                P.dma("sp", "d_ox", xb[:], xs_v[:, :, c0:c0 + 128], r=xs_keys(c0, 128), w=["oxb"])
                P.op("dve", lambda e: e.tensor_tensor(out=of_[:], in0=of_[:], in1=obb[:], op=ALU.add), r=["of", "obb"], w=["of"])
                for h in range(8):
                    P.op("act", lambda e, h=h: e.activation(out=sqj[:], in_=of_[:, h * 128:(h + 1) * 128], func=AF.Square, accum_out=ssq[:, h:h + 1]), r=["of"], w=["sqj", ("ssq", h)])
                P.op("act", lambda e: e.activation(out=ssq[:], in_=ssq[:], func=AF.Sqrt, bias=epsc, scale=1.0 / 128.0), r=[("ssq", h) for h in range(8)] + ["consts"], w=[("ssq", h) for h in range(8)])
                P.op("dve", lambda e: e.reciprocal(out=ssq[:], in_=ssq[:]), r=[("ssq", h) for h in range(8)], w=[("ssq", h) for h in range(8)])
                for h in range(8):
                    P.op("dve", lambda e, h=h: e.tensor_scalar(out=of_[:, h * 128:(h + 1) * 128], in0=of_[:, h * 128:(h + 1) * 128], scalar1=ssq[:, h:h + 1], scalar2=None, op0=ALU.mult),
                         r=["of", ("ssq", h)], w=["of"])
                    pi = nextps()
                    P.op("pe", lambda e, h=h, pi=pi: e.transpose(PS[pi][:, :128], of_[:, h * 128:(h + 1) * 128], ident), r=["of", "consts"], w=[("ps", pi)])
                    P.op("dve", lambda e, h=h, pi=pi: e.scalar_tensor_tensor(out=og[:, h, :], in0=PS[pi][:, :128], scalar=cols[:, om + 120:om + 121], in1=szt[:, h, :], op0=ALU.mult, op1=ALU.mult),
                         r=[("ps", pi), "cols", "szt"], w=[("og", h)])
                for m in range(8):
                    pi = nextps()

                    def mm(e, pi=pi, m=m):
                        for k in range(8):
                            ins = e.matmul(PS[pi][:, :128], wo[:, k, m * 128:(m + 1) * 128], og[:, k, :], start=(k == 0), stop=(k == 7))
                        return ins
                    P.op("pe", mm, r=["ow"] + [("og", h) for h in range(8)], w=[("ps", pi)])
                    P.op("dve", lambda e, pi=pi, m=m, mi=mi: e.scalar_tensor_tensor(out=xb[:, m, :], in0=PS[pi][:, :128], scalar=mcol(2 * 8 + m, mi), in1=xb[:, m, :], op0=ALU.mult, op1=ALU.add),
                         r=[("ps", pi), "oxb", "modT"], w=["oxb"])
                P.dma("sp", "d_oxs", xs_v[:, :, c0:c0 + 128], xb[:], r=["oxb"], w=xs_keys(c0, 128))


def gmlp_layer(nc, cfg, P, sb, PS, nextps, l, j, xs_v, xs_keys, norm_mod, mcol, cols,
               gm_w_in, gm_w_out, gm_w_sT, gm_rows, x_tiles, c_tiles, ctx_out, consts):
    om = cfg.o_mix
    epsc = consts[:, 384:385]
    with ExitStack() as st_:
        wi = sb("m_wi", [128, 8, 4096], BF16, st_)
        wiv = gm_w_in[j].rearrange("(k p) n -> p k n", p=128)
        for q4 in range(4):
            P.dma("pool", "d_mwi%d" % q4, wi[:, :, q4 * 1024:(q4 + 1) * 1024], wiv[:, :, q4 * 1024:(q4 + 1) * 1024], w=[("wi", q4)])
        wik = [("wi", q4) for q4 in range(4)]
        wo = sb("m_wo", [128, 16, 1024], BF16, st_)
        P.dma("pool", "d_mwo", wo[:], gm_w_out[j].rearrange("(k p) n -> p k n", p=128), w=["wo"])
        ws = sb("m_ws", [128, 8, 128], BF16, st_)
        P.dma("pool", "d_mws", ws[:], gm_w_sT[j].rearrange("g j i -> j g i"), w=["ws"])
        rows = sb("m_rows", [128, 3 * GW + 1024], F32, st_)
        P.dma("sp", "d_mrows", rows[:], gm_rows[j, :, :], w=["rows"])
        big = sb("m_big", [128, 8, 128], F32, st_)
        sq = sb("m_sq", [128, 8, 128], F32, st_)
        stn = {"sq": sq, "rstd": sb("m_rstd", [128, 128], F32, st_), "tmp": sq}
        h16 = sb("m_h16", [128, 8, 128], BF16, st_)
        v = sb("m_v", [128, GW], F32, st_)
        vn = sb("m_vn", [128, GW], BF16, st_)
        u = sb("m_u", [128, 16, 128], F32, st_)
        gt = sb("m_gt", [128, 16, 128], BF16, st_)
        tt = [sb("m_tt%d" % i, [128, 128], F32, st_) for i in range(2)]
        st1 = sb("m_st", [128, 4], F32, st_)
        xb = sb("m_xb", [128, 8, 128], F32, st_)
        outt = list(x_tiles) + (list(c_tiles) if ctx_out else [])
        for (t0, n, mi) in outt:
            for c0 in range(t0, t0 + n, 128):
                P.dma("sp", "d_mgbig", big[:], xs_v[:, :, c0:c0 + 128], r=xs_keys(c0, 128), w=[("xt", c) for c in range(8)])
                P.dma("sp", "d_mgx", xb[:], xs_v[:, :, c0:c0 + 128], r=xs_keys(c0, 128), w=["mxb"])
                norm_mod(stn, big, 128, 0, mi, h16)
                hk = [("h16", c) for c in range(8)]
                for jc in range(16):
                    pi = nextps()

                    def mm(e, pi=pi, jc=jc):
                        for k in range(8):
                            ins = e.matmul(PS[pi][:, :128], wi[:, k, jc * 128:(jc + 1) * 128], h16[:, k, :], start=(k == 0), stop=(k == 7))
                        return ins
                    P.op("pe", mm, r=wik + hk, w=[("ps", pi)])
                    P.op("act", lambda e, pi=pi, jc=jc: e.activation(out=u[:, jc, :], in_=PS[pi][:, :128], func=AF.Gelu, bias=cols[:, om + jc:om + jc + 1], scale=1.0),
                         r=[("ps", pi), "cols"], w=[("u", jc)])
                for q4 in range(4):
                    pi = nextps()

                    def mm(e, pi=pi, q4=q4):
                        for k in range(8):
                            ins = e.matmul(PS[pi][:, :512], h16[:, k, :], wi[:, k, GW + q4 * 512:GW + (q4 + 1) * 512], start=(k == 0), stop=(k == 7))
                        return ins
                    P.op("pe", mm, r=wik + hk, w=[("ps", pi)])
                    P.op("dve", lambda e, pi=pi, q4=q4: e.tensor_tensor(out=v[:, q4 * 512:(q4 + 1) * 512], in0=PS[pi][:, :512], in1=rows[:, q4 * 512:(q4 + 1) * 512], op=ALU.add),
                         r=[("ps", pi), "rows"], w=["v"])
                P.op("act", lambda e: e.activation(out=v[:], in_=v[:], func=AF.Gelu), r=["v"], w=["v"])
                P.op("dve", lambda e: e.tensor_reduce(out=st1[:, 0:1], in_=v[:], axis=AX.X, op=ALU.add), r=["v"], w=["st1"])
                P.op("dve", lambda e: e.tensor_scalar(out=st1[:, 0:1], in0=st1[:, 0:1], scalar1=-1.0 / GW, scalar2=None, op0=ALU.mult), r=["st1"], w=["st1"])
                P.op("dve", lambda e: e.tensor_scalar(out=v[:], in0=v[:], scalar1=st1[:, 0:1], scalar2=None, op0=ALU.add), r=["v", "st1"], w=["v"])
                P.op("act", lambda e: e.activation(out=vn[:], in_=v[:], func=AF.Square, accum_out=st1[:, 1:2]), r=["v"], w=["vn", "st2"])
                P.op("act", lambda e: e.activation(out=st1[:, 1:2], in_=st1[:, 1:2], func=AF.Sqrt, bias=epsc, scale=1.0 / GW), r=["st2", "consts"], w=["st2"])
                P.op("dve", lambda e: e.reciprocal(out=st1[:, 1:2], in_=st1[:, 1:2]), r=["st2"], w=["st2"])
                P.op("dve", lambda e: e.scalar_tensor_tensor(out=v[:], in0=v[:], scalar=st1[:, 1:2], in1=rows[:, GW:2 * GW], op0=ALU.mult, op1=ALU.mult), r=["v", "st2", "rows"], w=["v"])
                P.op("dve", lambda e: e.tensor_tensor(out=vn[:], in0=v[:], in1=rows[:, 2 * GW:3 * GW], op=ALU.add), r=["v", "rows", "vn"], w=["vn"])
                for fc in range(16):
                    g = fc // 2
                    pi = nextps()
                    P.op("pe", lambda e, pi=pi, fc=fc, g=g: e.matmul(PS[pi][:, :128], vn[:, fc * 128:(fc + 1) * 128], ws[:, g, :], start=True, stop=True), r=["vn", "ws"], w=[("ps", pi)])
                    t_ = tt[fc % 2]
                    tk = "mtt%d" % (fc % 2)
                    P.op("dve", lambda e, pi=pi, g=g, t_=t_: e.tensor_tensor(out=t_[:], in0=PS[pi][:, :128], in1=rows[:, 3 * GW + g * 128:3 * GW + (g + 1) * 128], op=ALU.add),
                         r=[("ps", pi), "rows"], w=[tk])
                    P.op("pool", lambda e, fc=fc, t_=t_: e.tensor_tensor(out=gt[:, fc, :], in0=t_[:], in1=u[:, fc, :], op=ALU.mult), r=[tk, ("u", fc)], w=[("gt", fc)])
                for m in range(8):
                    pi = nextps()

                    def mm(e, pi=pi, m=m):
                        for k in range(16):
                            ins = e.matmul(PS[pi][:, :128], wo[:, k, m * 128:(m + 1) * 128], gt[:, k, :], start=(k == 0), stop=(k == 15))
                        return ins
                    P.op("pe", mm, r=["wo"] + [("gt", k) for k in range(16)], w=[("ps", pi)])
                    P.op("dve", lambda e, pi=pi, m=m, mi=mi: e.scalar_tensor_tensor(out=xb[:, m, :], in0=PS[pi][:, :128], scalar=mcol(2 * 8 + m, mi), in1=xb[:, m, :], op0=ALU.mult, op1=ALU.add),
                         r=[("ps", pi), "mxb", "modT"], w=["mxb"])
                P.dma("sp", "d_mgxs", xs_v[:, :, c0:c0 + 128], xb[:], r=["mxb"], w=xs_keys(c0, 128))


def _prep_inputs(cfg, inp, core, ncores):
    NB, T, TC, DEPTH, NE = cfg.NB, cfg.T, cfg.TC, cfg.DEPTH, cfg.NE
    f = np.float32
    b0 = core * NB
    x = inp["x"][b0:b0 + NB].reshape(NB * T, D)
    cx = inp["ctx"][b0:b0 + NB].reshape(NB * TC, D)
    cc = np.concatenate([inp["c"][b0:b0 + NB], inp["c_ctx"][None, :]], 0)
    cT = np.ascontiguousarray(cc.reshape(NB + 1, 8, 128).transpose(2, 1, 0).reshape(128, 8 * (NB + 1)))
    m = {"xT": np.ascontiguousarray(x.T), "cxT": np.ascontiguousarray(cx.T), "cT": cT}
    return m


def _colmat(v):
    v = np.asarray(v, np.float32).reshape(-1, 128)
    return v.T


def _shared_inputs(cfg, inp):
    NB, T, TC, DEPTH, NE = cfg.NB, cfg.T, cfg.TC, cfg.DEPTH, cfg.NE
    f = np.float32
    cols = np.zeros((DEPTH, 128, cfg.NCOL), f)
    for l in range(DEPTH):
        cols[l, :, cfg.o_adab:cfg.o_adab + 48] = _colmat(inp["ada_b"][l])
        cols[l, :, cfg.o_n1:cfg.o_n1 + 8] = _colmat(inp["norm1_g"][l])
        cols[l, :, cfg.o_n2:cfg.o_n2 + 8] = _colmat(inp["norm2_g"][l])
        cols[l, :, cfg.o_bgu:cfg.o_bgu + NE * 16] = _colmat(inp["moe_b_gu"][l])
    for l in range(DEPTH):
        if l % 2 == 0 and cfg.mixers[0]:
            jj = l // 2
            o = cfg.o_mix
            cw = np.asarray(inp["gdn_conv_w"][jj], f)
            for tp in range(5):
                cols[l, :, o + tp * 24:o + tp * 24 + 24] = _colmat(cw[tp])
            cols[l, :, o + 120] = np.asarray(inp["gdn_norm_g"][jj], f)
            cols[l, :16, o + 121] = np.asarray(inp["gdn_a_log"][jj], f).reshape(16)
            cols[l, :16, o + 122] = np.asarray(inp["gdn_dt_bias"][jj], f).reshape(16)
            cols[l, 16:32, o + 123] = 1.0
            cols[l, :16, o + 124] = 1.0
    for l in range(DEPTH):
        if l % 2 == 1 and cfg.mixers[1]:
            jj = l // 2
            cols[l, :, cfg.o_mix:cfg.o_mix + 16] = _colmat(np.asarray(inp["gmlp_b_in"][jj], f)[:GW])
    consts = np.zeros((128, 6 * 128), f)
    consts[:, 0:128] = np.eye(128, dtype=f)
    consts[:, 128:256] = 1.0 / 1024.0
    consts[:, 256:384] = 1.0
    consts[:, 384:512] = EPS
    jj_, ii_ = np.meshgrid(np.arange(64), np.arange(64), indexing="ij")
    consts[:64, 512:576] = (jj_ <= ii_)
    consts[:64, 576:640] = (jj_ < ii_)
    consts[:64, 640:704] = (jj_ >= ii_)
    consts[:64, 704:768] = (jj_ > ii_)
    sel = np.zeros((NE, NE * 128), f)
    for e in range(NE):
        sel[e, e * 128:(e + 1) * 128] = 1.0
    m = {
        "cols": cols, "consts": consts, "sel": sel,
        "ada_w": np.asarray(inp["ada_w"], f), "router_w": np.asarray(inp["router_w"], f),
        "router_bb": np.ascontiguousarray(np.broadcast_to(np.asarray(inp["router_b"], f)[:, None, :], (DEPTH, 128, NE))),
        "moe_w_gu": np.asarray(inp["moe_w_gu"], f), "moe_w_dn": np.asarray(inp["moe_w_dn"], f), "moe_b_dn": np.asarray(inp["moe_b_dn"], f),
        "final_g": np.ascontiguousarray(_colmat(inp["final_g"])),
    }
    if cfg.mixers[1] and DEPTH >= 2:
        nb_ = DEPTH // 2
        m["gm_w_in"] = np.asarray(inp["gmlp_w_in"], f)
        m["gm_w_out"] = np.asarray(inp["gmlp_w_out"], f)
        m["gm_w_sT"] = np.ascontiguousarray(np.asarray(inp["gmlp_w_s"], f).transpose(0, 1, 3, 2))
        rows = np.concatenate([np.asarray(inp["gmlp_b_in"], f)[:, GW:], np.asarray(inp["gmlp_ln_g"], f), np.asarray(inp["gmlp_ln_b"], f),
                               np.asarray(inp["gmlp_b_s"], f).reshape(nb_, 1024)], axis=1)
        m["gm_rows"] = np.ascontiguousarray(np.broadcast_to(rows[:, None, :], (nb_, 128, rows.shape[1])))
    if cfg.mixers[0]:
        m["gd_w_in"] = np.asarray(inp["gdn_w_in"], f)
        m["gd_w_out"] = np.asarray(inp["gdn_w_out"], f)
    return m


def run(cfg, inp, ncores, trace=False):
    nc = build(cfg)
    shared = _shared_inputs(cfg, inp)
    in_maps = []
    for c in range(ncores):
        m = dict(shared)
        m.update(_prep_inputs(cfg, inp, c, ncores))
        in_maps.append(m)
    res = run_bass_kernel_spmd(nc, in_maps, core_ids=list(range(ncores)), trace=trace)
    outs = []
    run.last = res
    for c in range(ncores):
        yT = res.results[c]["yT"]
        outs.append(np.ascontiguousarray(yT.T).reshape(cfg.NB, cfg.T, D))
    return np.concatenate(outs, 0), res


def kernel(**inputs):
    cfg = Cfg()
    out, _ = run(cfg, inputs, 8)
    return out.astype(np.float32)
```
